# Optimizing a Trainium2 kernel written in Bass

```python
import jax
import jax.numpy as jnp
from jax import lax
import numpy as np

D_MODEL = 1024
BATCH = 2
SEQ = 16384
DEPTH = 2

N_HEADS_MLA = 8
QK_NOPE_DIM = 64
QK_ROPE_DIM = 32
V_HEAD_DIM = 64
Q_LORA_RANK = 384
KV_LORA_RANK = 256
D_MLA = N_HEADS_MLA * V_HEAD_DIM

N_CONV_GROUPS = 4
CONV_GROUP_DIM = 64
D_CONV = N_CONV_GROUPS * CONV_GROUP_DIM
CONV_WIDTH = 3

N_FOURIER_GROUPS = 4
FOURIER_GROUP_DIM = 64
D_FOURIER = N_FOURIER_GROUPS * FOURIER_GROUP_DIM

D_MIX = D_MLA + D_CONV + D_FOURIER
IN_SPLITS = (Q_LORA_RANK, KV_LORA_RANK, QK_ROPE_DIM, D_CONV, D_CONV, D_CONV, D_FOURIER)
D_IN = sum(IN_SPLITS)

N_EXPERT_GROUPS = 4
EXPERTS_PER_GROUP = 8
TOP_K_IN_GROUP = 2
D_EXPERT = 256

Q_BLOCK = 128
ROPE_THETA = 10000.0
LN_EPS = 1e-5
RMS_EPS = 1e-6
DEEPNORM_ALPHA = (2.0 * DEPTH) ** 0.25
DEEPNORM_BETA = (8.0 * DEPTH) ** -0.25

kernel_name = 'hybrid_mla_conv_fourier_hmoe_deepnorm_encoder'


def layer_norm(x, g, b):
    xf = x.astype(jnp.float32)
    mu = jnp.mean(xf, -1, keepdims=True)
    xc = xf - mu
    var = jnp.mean(xc * xc, -1, keepdims=True)
    return (xc * lax.rsqrt(var + LN_EPS) * g.astype(jnp.float32) + b.astype(jnp.float32)).astype(x.dtype)


def rms_norm(x, g):
    xf = x.astype(jnp.float32)
    return (xf * lax.rsqrt(jnp.mean(xf * xf, -1, keepdims=True) + RMS_EPS) * g.astype(jnp.float32)).astype(x.dtype)


def rope_tables(seq_len, dtype):
    inv_freq = 1.0 / (ROPE_THETA ** (jnp.arange(0, QK_ROPE_DIM, 2, dtype=jnp.float32) / QK_ROPE_DIM))
    ang = jnp.arange(seq_len, dtype=jnp.float32)[:, None] * inv_freq[None, :]
    return jnp.cos(ang).astype(dtype), jnp.sin(ang).astype(dtype)


def apply_rope(t, cos, sin):
    t1, t2 = jnp.split(t, 2, axis=-1)
    return jnp.concatenate([t1 * cos - t2 * sin, t1 * sin + t2 * cos], axis=-1)


def mla_attention(c_q, c_kv, k_pe, g_q, g_kv, w_uq, w_ukv, cos, sin):
    bsz, seq, _ = c_q.shape
    q = jnp.einsum('bsr,rhd->bshd', rms_norm(c_q, g_q), w_uq)
    kv = jnp.einsum('bsr,rhd->bshd', rms_norm(c_kv, g_kv), w_ukv)
    q_nope = q[..., :QK_NOPE_DIM]
    q_pe = apply_rope(q[..., QK_NOPE_DIM:], cos[None, :, None, :], sin[None, :, None, :])
    k_nope = kv[..., :QK_NOPE_DIM]
    v = kv[..., QK_NOPE_DIM:]
    k_pe = apply_rope(k_pe, cos[None], sin[None])
    scale = (QK_NOPE_DIM + QK_ROPE_DIM) ** -0.5
    n_blk = seq // Q_BLOCK

    def to_blocks(t):
        return (t * scale).reshape(bsz, n_blk, Q_BLOCK, N_HEADS_MLA, t.shape[-1]).swapaxes(0, 1)

    def attend(blk):
        qn, qp = blk
        s = jnp.einsum('bqhd,bkhd->bhqk', qn, k_nope) + jnp.einsum('bqhr,bkr->bhqk', qp, k_pe)
        p = jax.nn.softmax(s.astype(jnp.float32), axis=-1).astype(v.dtype)
        return jnp.einsum('bhqk,bkhd->bqhd', p, v)

    o = lax.map(attend, (to_blocks(q_nope), to_blocks(q_pe)))
    return o.swapaxes(0, 1).reshape(bsz, seq, D_MLA)


def short_conv_mixer(b_gate, c_gate, h, w_conv):
    seq = h.shape[1]
    pad = CONV_WIDTH // 2
    up = jnp.pad(c_gate * h, ((0, 0), (pad, pad), (0, 0)))
    y = sum(up[:, k:k + seq] * w_conv[k] for k in range(CONV_WIDTH))
    return b_gate * y


def fourier_mixer(f):
    bsz, seq, _ = f.shape
    fg = f.astype(jnp.float32).reshape(bsz, seq, N_FOURIER_GROUPS, FOURIER_GROUP_DIM).transpose(0, 2, 1, 3)
    y = jnp.fft.fft2(fg, norm='ortho').real
    return y.transpose(0, 2, 1, 3).reshape(bsz, seq, D_FOURIER).astype(f.dtype)


def token_mixer(h, w_in, g_q, g_kv, w_uq, w_ukv, w_conv, w_out, cos, sin):
    z = jnp.einsum('bsd,dp->bsp', h, w_in)
    split_points = np.cumsum(IN_SPLITS)[:-1].tolist()
    c_q, c_kv, k_pe, b_gate, c_gate, hc, f = jnp.split(z, split_points, axis=-1)
    o_mla = mla_attention(c_q, c_kv, k_pe, g_q, g_kv, w_uq, w_ukv, cos, sin)
    o_conv = short_conv_mixer(b_gate, c_gate, hc, w_conv)
    o_four = fourier_mixer(f)
    o = jnp.concatenate([o_mla, o_conv, o_four], axis=-1)
    return jnp.einsum('bsm,md->bsd', o, w_out)


def hier_moe(h, w_group, b_group, w_router, b_router, w_gate, w_up, w_down):
    bsz, seq, d = h.shape
    xt = h.reshape(-1, d)
    g_prob = jax.nn.softmax((xt @ w_group + b_group).astype(jnp.float32), axis=-1)
    g_onehot = jax.nn.one_hot(jnp.argmax(g_prob, axis=-1), N_EXPERT_GROUPS, dtype=jnp.float32)
    p_group = jnp.sum(g_prob * g_onehot, -1, keepdims=True)
    e_logits_all = (jnp.einsum('nd,dge->nge', xt, w_router) + b_router).astype(jnp.float32)
    e_logits = jnp.einsum('nge,ng->ne', e_logits_all, g_onehot)
    e_prob = jax.nn.softmax(e_logits, axis=-1)
    top_p, top_i = lax.top_k(e_prob, TOP_K_IN_GROUP)
    top_p = top_p / jnp.sum(top_p, -1, keepdims=True)
    w_in_group = jnp.sum(jax.nn.one_hot(top_i, EXPERTS_PER_GROUP, dtype=jnp.float32) * top_p[..., None], axis=1) * p_group
    combine = (g_onehot[:, :, None] * w_in_group[:, None, :]).astype(h.dtype)
    out = jnp.zeros_like(xt)
    for g in range(N_EXPERT_GROUPS):
        a = jnp.einsum('nd,edf->nef', xt, w_gate[g])
        u = jnp.einsum('nd,edf->nef', xt, w_up[g])
        hid = jax.nn.silu(a) * u * combine[:, g, :, None]
        out = out + jnp.einsum('nef,efd->nd', hid, w_down[g])
    return out.reshape(bsz, seq, d)


def setup_inputs(seed: int = 0) -> dict:
    key = jax.random.key(seed)
    ks = jax.random.split(key, 24)
    f32 = jnp.float32
    L = DEPTH

    def nrm(k, shape, scale):
        return jax.random.normal(k, shape, f32) * scale

    return {
        'x': nrm(ks[0], (BATCH, SEQ, D_MODEL), 1.0),
        'ln_in_g': 1.0 + nrm(ks[1], (D_MODEL,), 0.02),
        'ln_in_b': nrm(ks[2], (D_MODEL,), 0.02),
        'w_in': nrm(ks[3], (L, D_MODEL, D_IN), D_MODEL ** -0.5),
        'g_q': 1.0 + nrm(ks[4], (L, Q_LORA_RANK), 0.02),
        'g_kv': 1.0 + nrm(ks[5], (L, KV_LORA_RANK), 0.02),
        'w_uq': nrm(ks[6], (L, Q_LORA_RANK, N_HEADS_MLA, QK_NOPE_DIM + QK_ROPE_DIM), Q_LORA_RANK ** -0.5),
        'w_ukv': nrm(ks[7], (L, KV_LORA_RANK, N_HEADS_MLA, QK_NOPE_DIM + V_HEAD_DIM), KV_LORA_RANK ** -0.5),
        'w_conv': nrm(ks[8], (L, CONV_WIDTH, D_CONV), CONV_WIDTH ** -0.5),
        'w_out': nrm(ks[9], (L, D_MIX, D_MODEL), DEEPNORM_BETA * D_MIX ** -0.5),
        'ln1_g': 1.0 + nrm(ks[10], (L, D_MODEL), 0.02),
        'ln1_b': nrm(ks[11], (L, D_MODEL), 0.02),
        'w_group': nrm(ks[12], (L, D_MODEL, N_EXPERT_GROUPS), D_MODEL ** -0.5),
        'b_group': nrm(ks[13], (L, N_EXPERT_GROUPS), 0.01),
        'w_router': nrm(ks[14], (L, D_MODEL, N_EXPERT_GROUPS, EXPERTS_PER_GROUP), D_MODEL ** -0.5),
        'b_router': nrm(ks[15], (L, N_EXPERT_GROUPS, EXPERTS_PER_GROUP), 0.01),
        'w_gate': nrm(ks[16], (L, N_EXPERT_GROUPS, EXPERTS_PER_GROUP, D_MODEL, D_EXPERT), D_MODEL ** -0.5),
        'w_up': nrm(ks[17], (L, N_EXPERT_GROUPS, EXPERTS_PER_GROUP, D_MODEL, D_EXPERT), D_MODEL ** -0.5),
        'w_down': nrm(ks[18], (L, N_EXPERT_GROUPS, EXPERTS_PER_GROUP, D_EXPERT, D_MODEL), DEEPNORM_BETA * D_EXPERT ** -0.5),
        'ln2_g': 1.0 + nrm(ks[19], (L, D_MODEL), 0.02),
        'ln2_b': nrm(ks[20], (L, D_MODEL), 0.02),
    }


def reference(x, ln_in_g, ln_in_b, w_in, g_q, g_kv, w_uq, w_ukv, w_conv, w_out, ln1_g, ln1_b,
              w_group, b_group, w_router, b_router, w_gate, w_up, w_down, ln2_g, ln2_b):
    cos, sin = rope_tables(x.shape[1], x.dtype)
    h = layer_norm(x, ln_in_g, ln_in_b)
    for l in range(DEPTH):
        mix = token_mixer(h, w_in[l], g_q[l], g_kv[l], w_uq[l], w_ukv[l], w_conv[l], w_out[l], cos, sin)
        h = layer_norm(DEEPNORM_ALPHA * h + mix, ln1_g[l], ln1_b[l])
        ffn = hier_moe(h, w_group[l], b_group[l], w_router[l], b_router[l], w_gate[l], w_up[l], w_down[l])
        h = layer_norm(DEEPNORM_ALPHA * h + ffn, ln2_g[l], ln2_b[l])
    return h
```

```python
import numpy as np
from concourse.bass_utils import run_bass_kernel_spmd
import contextlib
import concourse.bass as bass
import concourse.mybir as mybir

F32 = mybir.dt.float32
BF16 = mybir.dt.bfloat16
I32 = mybir.dt.int32
AF = mybir.ActivationFunctionType
ALU = mybir.AluOpType
AX = mybir.AxisListType

ENGS = ("pe", "act", "dve", "pool", "sp")


class SemPool:
    NHW = 46
    NDMA = 72

    def __init__(self, nc):
        self.stack = contextlib.ExitStack()
        self.eng = {e: self.stack.enter_context(nc.semaphore(f"sem_{e}")) for e in ENGS}
        self.eng_cnt = {e: 0 for e in ENGS}
        self.dma = [self.stack.enter_context(nc.semaphore(f"sem_d{i}")) for i in range(self.NDMA)]
        self.dma_cnt = [0] * self.NDMA


class Phase:
    def __init__(self, nc, name, pool=None):
        self.nc = nc
        self.name = name
        self.pool = pool if pool is not None else SemPool(nc)
        self.ins = []
        self.stack = contextlib.ExitStack()
        self.nbuf = 0

    def sb(self, shape, dt, name=None):
        self.nbuf += 1
        return self.stack.enter_context(self.nc.sbuf_tensor(f"{self.name}_{name or 'sb'}{self.nbuf}", list(shape), dt))

    def ps(self, shape, dt, name=None):
        self.nbuf += 1
        return self.stack.enter_context(self.nc.psum_tensor(f"{self.name}_{name or 'ps'}{self.nbuf}", list(shape), dt))

    def add(self, eng, fn, reads=(), writes=(), dma=False, semkey=None, inc=16):
        assert eng in ENGS
        reads = tuple(reads)
        writes = tuple(writes)
        if dma and semkey is None:
            sb_w = [k for k in writes if not (isinstance(k, tuple) and k and k[0] == "dram")]
            sb_r = [k for k in reads if not (isinstance(k, tuple) and k and k[0] == "dram")]
            semkey = sb_w[0] if sb_w else (sb_r[0] if sb_r else writes[0])
        self.ins.append(dict(eng=eng, fn=fn, reads=reads, writes=writes, dma=dma, semkey=semkey, inc=inc))

    def dma(self, eng, out, in_, reads=(), writes=(), semkey=None, **kw):
        self.add(eng, lambda e: e.dma_start(out=out, in_=in_, **kw), reads, writes, dma=True, semkey=semkey)

    def allgather(self, src_t, dst_t, reads=(), writes=(), semkey=None):
        self.add("pool", lambda e: e.collective_compute("AllGather", ALU.bypass, replica_groups=[[0, 1, 2, 3], [4, 5, 6, 7]],
                                                        ins=[src_t.ap().opt()], outs=[dst_t.ap().opt()]),
                 reads, writes, dma=True, semkey=semkey, inc=1)

    def interleave(self, fns):
        lists = []
        for fn in fns:
            keep, self.ins = self.ins, []
            fn()
            lists.append(self.ins)
            self.ins = keep
        n = max(len(x) for x in lists)
        for i in range(n):
            for x in lists:
                if i < len(x):
                    self.ins.append(x[i])

    def emit(self):
        nc = self.nc
        ins = self.ins
        n = len(ins)
        last_w = {}
        readers = {}
        deps = [None] * n
        for i, I in enumerate(ins):
            d = {}
            for k in I["reads"]:
                j = last_w.get(k)
                if j is not None:
                    d[j] = d.get(j, 0) | 1
            for k in I["writes"]:
                j = last_w.get(k)
                if j is not None:
                    d[j] = d.get(j, 0) | 2
                for r in readers.get(k, ()):
                    if r != i:
                        d[r] = d.get(r, 0) | 4
            for k in I["reads"]:
                readers.setdefault(k, []).append(i)
            for k in I["writes"]:
                last_w[k] = i
                readers[k] = []
            need = []
            for j, ty in d.items():
                J = ins[j]
                if J["dma"]:
                    need.append(j)
                elif J["eng"] == I["eng"]:
                    if I["eng"] == "pe":
                        continue
                    if I["dma"] or (ty & 3):
                        need.append(j)
                else:
                    need.append(j)
            deps[i] = need
        sig = [False] * n
        for i in range(n):
            for j in deps[i]:
                if not ins[j]["dma"]:
                    sig[j] = True
        pool = self.pool
        cnt = dict(pool.eng_cnt)
        val = [0] * n
        dcount = {}
        dslot = {}
        nhw = nsw = 0
        dma_before = [None] * n
        dma_pos = {}
        dma_cum = {}
        for i, I in enumerate(ins):
            if I["dma"]:
                k = I["semkey"]
                if k not in dslot:
                    if I["eng"] == "pool":
                        nsw += 1
                        dslot[k] = pool.NHW + nsw - 1
                        assert dslot[k] < pool.NDMA, "too many software-DMA semaphores in one phase"
                    else:
                        nhw += 1
                        dslot[k] = nhw - 1
                        assert dslot[k] < pool.NHW, "too many hardware-DMA semaphores in one phase"
                    dcount[k] = pool.dma_cnt[dslot[k]]
                dcount[k] = dcount[k] + I["inc"]
                val[i] = dcount[k]
                dma_pos.setdefault(k, []).append(i)
                dma_cum.setdefault(k, []).append(dcount[k])
            elif sig[i]:
                cnt[I["eng"]] += 1
                val[i] = cnt[I["eng"]]
        esem = pool.eng
        dsem = {k: pool.dma[i] for k, i in dslot.items()}
        base_wait = {("e", e): pool.eng_cnt[e] for e in ENGS}
        for k, i in dslot.items():
            base_wait[("d", k)] = pool.dma_cnt[i]
            pool.dma_cnt[i] = dcount[k]
        pool.eng_cnt = dict(cnt)
        self.n_sems = len(dsem)
        import bisect
        streams = {e: [] for e in ENGS}
        for i, I in enumerate(ins):
            waits = {}
            for j in deps[i]:
                J = ins[j]
                if J["dma"]:
                    k = J["semkey"]
                    c = bisect.bisect_left(dma_pos[k], i)
                    key = ("d", k)
                    waits[key] = max(waits.get(key, 0), dma_cum[k][c - 1])
                else:
                    key = ("e", J["eng"])
                    waits[key] = max(waits.get(key, 0), val[j])
            streams[I["eng"]].append((i, waits))
        final_dma = dict(dcount)

        def run_engine(ename, eobj):
            waited = dict(base_wait)
            for i, waits in streams[ename]:
                I = ins[i]
                for key, v in waits.items():
                    if waited.get(key, 0) >= v:
                        continue
                    waited[key] = v
                    s = dsem[key[1]] if key[0] == "d" else esem[key[1]]
                    eobj.wait_ge(s, v)
                r = I["fn"](eobj)
                if I["dma"]:
                    r.then_inc(dsem[I["semkey"]], I["inc"])
                elif sig[i]:
                    r.then_inc(esem[ename], 1)
            if ename == "sp":
                for k, v in final_dma.items():
                    if waited.get(("d", k), 0) < v:
                        eobj.wait_ge(dsem[k], v)

        with nc.Block() as block:
            @block.tensor
            def _(e):
                run_engine("pe", e)

            @block.scalar
            def _(e):
                run_engine("act", e)

            @block.vector
            def _(e):
                run_engine("dve", e)

            @block.gpsimd
            def _(e):
                run_engine("pool", e)

            @block.sync
            def _(e):
                run_engine("sp", e)
        self.stack.close()


NCORES = 8
BATCH, SEQ, DM, DEPTH = 2, 16384, 1024, 2
TOK = 4096
NST = TOK // 512
NH, DN, DR, DV = 8, 64, 32, 64
QL, KVL = 384, 256
NG, EPG, DE = 4, 8, 256
NE = NG * EPG
LN_EPS, RMS_EPS = 1e-5, 1e-6
ALPHA = (2.0 * DEPTH) ** 0.25
QSCALE = (DN + DR) ** -0.5
C_CQ, C_CKV, C_KPE, C_KPS, C_BG, C_CG, C_HC, C_F, WIN_COLS = 0, 384, 640, 672, 704, 960, 1216, 1472, 1728
GROUPS = [[0, 1, 2, 3], [4, 5, 6, 7]]


def mm(P, out, lhsT, rhs, start, stop, reads, writes):
    P.add("pe", lambda e: e.matmul(out, lhsT, rhs, start=start, stop=stop), reads, writes)


def trn(P, out, in_, ident, reads, writes):
    P.add("pe", lambda e: e.transpose(out=out, in_=in_, identity=ident), reads, writes)


def act(P, out, in_, func, reads, writes, eng="act", **kw):
    P.add(eng, lambda e: e.activation(out=out, in_=in_, func=func, **kw), reads, writes)


def cpy(P, eng, out, in_, reads, writes):
    if eng == "act":
        P.add(eng, lambda e: e.activation(out=out, in_=in_, func=AF.Copy), reads, writes)
    else:
        P.add(eng, lambda e: e.tensor_copy(out=out, in_=in_), reads, writes)


def tt(P, eng, out, in0, in1, op, reads, writes):
    P.add(eng, lambda e: e.tensor_tensor(out=out, in0=in0, in1=in1, op=op), reads, writes)


def ts(P, eng, out, in0, s1, s2, op0, op1, reads, writes):
    if s2 is None:
        P.add(eng, lambda e: e.tensor_scalar(out=out, in0=in0, scalar1=s1, scalar2=None, op0=op0), reads, writes)
    else:
        P.add(eng, lambda e: e.tensor_scalar(out=out, in0=in0, scalar1=s1, scalar2=s2, op0=op0, op1=op1), reads, writes)


def stt(P, eng, out, in0, scalar, in1, op0, op1, reads, writes):
    P.add(eng, lambda e: e.scalar_tensor_tensor(out=out, in0=in0, scalar=scalar, in1=in1, op0=op0, op1=op1), reads, writes)


def mset(P, eng, ap, v, writes):
    P.add(eng, lambda e: e.memset(ap, v), (), writes)


class Rot:
    def __init__(self, bufs, name):
        self.bufs = bufs
        self.name = name
        self.i = 0

    def next(self):
        k = self.i % len(self.bufs)
        self.i += 1
        return self.bufs[k], (self.name, k)


def layer_norm_tile(P, x_ap, xkey, out_ap, okey, g_ap, b_ap, gkeys, tmp, eps_ap, tag):
    st, mv, sd, rs, nb, y = tmp["st"], tmp["mv"], tmp["sd"], tmp["rs"], tmp["nb"], tmp["y"]
    k = lambda s: (tag, s)
    P.add("dve", lambda e: e.bn_stats(out=st[:, 0, :], in_=x_ap[:, 0:512]), [xkey], [k("st0")])
    P.add("dve", lambda e: e.bn_stats(out=st[:, 1, :], in_=x_ap[:, 512:1024]), [xkey], [k("st1")])
    P.add("dve", lambda e: e.bn_aggr(out=mv[:], in_=st[:]), [k("st0"), k("st1")], [k("mv")])
    act(P, sd[:], mv[:, 1:2], AF.Sqrt, [k("mv"), "eps"], [k("sd")], bias=eps_ap, scale=1.0)
    P.add("dve", lambda e: e.reciprocal(out=rs[:], in_=sd[:]), [k("sd")], [k("rs")])
    stt(P, "dve", nb[:], mv[:, 0:1], -1.0, rs[:], ALU.mult, ALU.mult, [k("mv"), k("rs")], [k("nb")])
    act(P, y[:], x_ap, AF.Identity, [xkey, k("nb"), k("rs")], [k("y")], bias=nb[:, 0:1], scale=rs[:, 0:1])
    tt(P, "dve", y[:], y[:], g_ap, ALU.mult, [k("y")] + gkeys, [k("y")])
    tt(P, "dve", out_ap, y[:], b_ap, ALU.add, [k("y")] + gkeys, [okey])


def ln_tmp(P):
    return dict(st=P.sb([128, 2, 6], F32), mv=P.sb([128, 2], F32), sd=P.sb([128, 1], F32), rs=P.sb([128, 1], F32),
                nb=P.sb([128, 1], F32), y=P.sb([128, 1024], F32))


def phase_A(nc, l, d):
    P = Phase(nc, f"A{l}", d.get("pool"))
    identf = P.sb([128, 128], F32)
    ident = P.sb([128, 128], BF16)
    ones = P.sb([128, 128], BF16)
    epsr = P.sb([128, 1], F32)
    epsl = P.sb([128, 1], F32)
    win = P.sb([128, 8, WIN_COLS], BF16)
    wuq = P.sb([128, 3, 2 * NH * 96], BF16)
    wk = P.sb([128, 2, 512], BF16)
    wv = P.sb([128, 2, 512], BF16)
    gq = P.sb([128, 3], F32)
    gkv = P.sb([128, 2], F32)
    stg = [P.sb([128, WIN_COLS], F32) for _ in range(1)]
    lng = P.sb([128, 1024], F32)
    lnb = P.sb([128, 1024], F32)
    vbufs = [P.sb([128, NH, 4, 65], BF16) for _ in range(2)]

    mset(P, "pool", ones[:], 1.0, ["ones"])
    mset(P, "pool", epsr[:], RMS_EPS, ["eps"])
    mset(P, "pool", epsl[:], LN_EPS, ["eps"])
    mset(P, "pool", vbufs[0][:, :, :, 64:65], 1.0, [("vones", 0)])
    mset(P, "pool", vbufs[1][:, :, :, 64:65], 1.0, [("vones", 1)])
    P.dma("sp", identf[:], d["ident"][:, :], writes=["identf"])
    cpy(P, "dve", ident[:], identf[:], ["identf"], ["ident"])
    P.dma("sp", gq[:], d["g_q"][l, :, :], writes=["gq"])
    P.dma("sp", gkv[:], d["g_kv"][l, :, :], writes=["gkv"])
    if l == 0:
        P.dma("sp", lng[:], d["lnp"][0, :, :], writes=["lng"])
        P.dma("sp", lnb[:], d["lnp"][1, :, :], writes=["lnb"])
    si = 0
    for kc in range(8):
        s, sk = stg[0], ("stg", 0)
        si += 1
        P.dma("sp", s[:, :], d["w_in"][l, kc * 128:(kc + 1) * 128, :], writes=[sk])
        cpy(P, "dve" if kc % 2 == 0 else "act", win[:, kc, :], s[:, :], [sk], [("win", kc)])
    for c in range(3):
        s, sk = stg[0], ("stg", 0)
        si += 1
        P.dma("sp", s[:, 0:1536], d["w_uq"][l, c * 128:(c + 1) * 128, :], writes=[sk])
        ts(P, "dve", wuq[:, c, :], s[:, 0:1536], gq[:, c:c + 1], None, ALU.mult, None, [sk, "gq"], [("wuq", c)])
    for c in range(2):
        s, sk = stg[0], ("stg", 0)
        si += 1
        P.dma("sp", s[:, 0:512], d["w_ukv_k"][l, c * 128:(c + 1) * 128, :], writes=[sk])
        P.dma("sp", s[:, 512:1024], d["w_ukv_v"][l, c * 128:(c + 1) * 128, :], writes=[sk])
        ts(P, "dve", wk[:, c, :], s[:, 0:512], gkv[:, c:c + 1], None, ALU.mult, None, [sk, "gkv"], [("wk", c)])
        ts(P, "dve", wv[:, c, :], s[:, 512:1024], gkv[:, c:c + 1], None, ALU.mult, None, [sk, "gkv"], [("wv", c)])
    wkeys = [("win", kc) for kc in range(8)]
    if l == 0:
        zero_slots(P, d)

    xts = [P.sb([128, 4, 1024], F32)] * 2
    hbs = [P.sb([128, 4, 1024], BF16)] * 2
    hTs = [P.sb([128, 8, 512], BF16) for _ in range(2)]
    ropes = [P.sb([128, 2, 512], F32) for _ in range(2)]
    cqTs = [P.sb([128, 3, 512], BF16)] * 2
    sqs = [P.sb([128, 3, 512], BF16)] * 2
    cqns = [P.sb([128, 3, 512], BF16) for _ in range(2)]
    ckvTs = [P.sb([128, 2, 512], BF16)] * 2
    sqks = [P.sb([128, 2, 512], BF16)] * 2
    ckvns = [P.sb([128, 2, 512], BF16) for _ in range(2)]
    sdt = [P.sb([128, 512], F32)] * 2
    rq = [P.sb([128, 512], F32)] * 2
    rk = [P.sb([128, 512], F32)] * 2
    kt1 = [P.sb([32, 512], F32)] * 2
    kt2 = [P.sb([32, 512], F32)] * 2
    krs = [P.sb([32, 512], BF16) for _ in range(2)]
    bgs = [P.sb([128, 2, 512], F32)] * 2
    cgs = [P.sb([128, 2, 512], F32)] * 2
    ups = [P.sb([128, 2, 512], F32)] * 2
    qTs = [P.sb([96, NH, 512], BF16) for _ in range(2)]
    qt1 = Rot([P.sb([96, 512], F32) for _ in range(2)], "qt1")
    qt2 = Rot([P.sb([96, 512], F32) for _ in range(2)], "qt2")
    kTs = [P.sb([128, 4, 512], BF16) for _ in range(2)]
    fs = [P.sb([128, 2, 512], BF16) for _ in range(2)]
    lt = [ln_tmp(P)] * 2 if l == 0 else None
    trp = Rot([P.ps([128, 512], BF16) for _ in range(2)], "trp")
    pb = Rot([P.ps([128, 512], F32) for _ in range(6)], "pb")

    for st in range(NST):
        sl = st % 2
        t0 = st * 512
        xt, hb, hT = xts[sl], hbs[sl], hTs[sl]
        K = lambda s: (s, sl if s in ("rope", "kr") else 0)
        if l == 0:
            P.dma("sp", xt[:, :, :], d["x"][t0:t0 + 512, :].rearrange("(j p) f -> p j f", p=128),
                  writes=[K("xraw")] + [("xt", 0, j) for j in range(4)])
            for j in range(4):
                layer_norm_tile(P, xt[:, j, :], K("xraw"), xt[:, j, :], ("xt", 0, j), lng[:], lnb[:], ["lng", "lnb"], lt[j % 2],
                                epsl[:, 0:1], "lnA")
            xkeys = [("xt", 0, j) for j in range(4)]
            P.dma("pool", d["hbuf"][t0:t0 + 512, :].rearrange("(j p) f -> p j f", p=128), xt[:, :, :], reads=xkeys,
                  semkey=("hst", 0))
        else:
            P.dma("sp", xt[:, :, :], d["hbuf"][t0:t0 + 512, :].rearrange("(j p) f -> p j f", p=128), writes=[K("xraw")])
            xkeys = [K("xraw")]
        P.dma("sp", ropes[sl][:, :, :], d["rope"][:, :, t0:t0 + 512], writes=[K("rope")])
        for j in range(4):
            cpy(P, "act" if j % 2 == 0 else "dve", hb[:, j, :], xt[:, j, :], xkeys if l else [("xt", 0, j)], [("hb", 0, j)])
        for kc in range(8):
            tp, tk = trp.next()
            for j in range(4):
                trn(P, tp[:, j * 128:(j + 1) * 128], hb[:, j, kc * 128:(kc + 1) * 128], ident[:], [("hb", 0, j), "ident"], [tk])
            cpy(P, "act" if kc % 2 == 0 else "dve", hT[:, kc, :], tp[:, :], [tk], [("hT", sl, kc)])
        hkeys = [("hT", sl, kc) for kc in range(8)]

        def zproj(col0, M):
            p, pk = pb.next()
            for kc in range(8):
                mm(P, p[0:M, :], win[:, kc, col0:col0 + M], hT[:, kc, :], kc == 0, kc == 7, [("win", kc), ("hT", sl, kc)], [pk])
            return p, pk

        for c in range(3):
            p, pk = zproj(C_CQ + c * 128, 128)
            cpy(P, "act", cqTs[sl][:, c, :], p[:, :], [pk], [("cqT", 0, c)])
            act(P, sqs[sl][:, c, :], p[:, :], AF.Square, [pk], [("sq", 0, c)])
        p, pk = pb.next()
        for c in range(3):
            mm(P, p[:, :], ones[:, :], sqs[sl][:, c, :], c == 0, c == 2, ["ones", ("sq", 0, c)], [pk])
        act(P, sdt[sl][:, :], p[:, :], AF.Sqrt, [pk, "eps"], [K("sdt")], bias=epsr[:, 0:1], scale=1.0 / QL)
        P.add("dve", lambda e, o=rq[sl], i=sdt[sl]: e.reciprocal(out=o[:, :], in_=i[:, :]), [K("sdt")], [K("rq")])
        for c in range(3):
            tt(P, "dve", cqns[sl][:, c, :], cqTs[sl][:, c, :], rq[sl][:, :], ALU.mult,
               [("cqT", 0, c), K("rq")], [("cqn", sl, c)])
        for c in range(2):
            p, pk = zproj(C_CKV + c * 128, 128)
            cpy(P, "act", ckvTs[sl][:, c, :], p[:, :], [pk], [("ckvT", 0, c)])
            act(P, sqks[sl][:, c, :], p[:, :], AF.Square, [pk], [("sqk", 0, c)])
        p, pk = pb.next()
        for c in range(2):
            mm(P, p[:, :], ones[:, :], sqks[sl][:, c, :], c == 0, c == 1, ["ones", ("sqk", 0, c)], [pk])
        act(P, sdt[sl][:, :], p[:, :], AF.Sqrt, [pk, "eps"], [K("sdt")], bias=epsr[:, 0:1], scale=1.0 / KVL)
        P.add("dve", lambda e, o=rk[sl], i=sdt[sl]: e.reciprocal(out=o[:, :], in_=i[:, :]), [K("sdt")], [K("rk")])
        for c in range(2):
            tt(P, "dve", ckvns[sl][:, c, :], ckvTs[sl][:, c, :], rk[sl][:, :], ALU.mult,
               [("ckvT", 0, c), K("rk")], [("ckvn", sl, c)])
        p1, pk1 = zproj(C_KPE, 32)
        p2, pk2 = zproj(C_KPS, 32)
        tt(P, "dve", kt1[sl][:, :], p1[0:32, :], ropes[sl][0:32, 0, :], ALU.mult, [pk1, K("rope")], [K("kt1")])
        tt(P, "dve", kt2[sl][:, :], p2[0:32, :], ropes[sl][0:32, 1, :], ALU.mult, [pk2, K("rope")], [K("kt2")])
        tt(P, "dve", krs[sl][:, :], kt1[sl][:, :], kt2[sl][:, :], ALU.add, [K("kt1"), K("kt2")], [K("kr")])
        P.dma("pool", d["payKr"][0:32, t0:t0 + 512], krs[sl][:, :], reads=[K("kr")])
        for c in range(2):
            p, pk = zproj(C_BG + c * 128, 128)
            cpy(P, "act", bgs[sl][:, c, :], p[:, :], [pk], [("bg", 0, c)])
        P.dma("pool", d["bgT"][:, t0:t0 + 512].rearrange("(c p) t -> p c t", p=128), bgs[sl][:, :, :],
              reads=[("bg", 0, 0), ("bg", 0, 1)], semkey=("bgst", 0))
        for c in range(2):
            p, pk = zproj(C_CG + c * 128, 128)
            cpy(P, "act", cgs[sl][:, c, :], p[:, :], [pk], [("cg", 0, c)])
        for c in range(2):
            p, pk = zproj(C_HC + c * 128, 128)
            tt(P, "dve", ups[sl][:, c, :], p[:, :], cgs[sl][:, c, :], ALU.mult, [pk, ("cg", 0, c)], [("up", 0, c)])
        P.dma("pool", d["upT"][:, t0:t0 + 512].rearrange("(c p) t -> p c t", p=128), ups[sl][:, :, :],
              reads=[("up", 0, 0), ("up", 0, 1)], semkey=("upst", 0))
        if st == 0:
            P.dma("pool", d["payH"][:, 0:1].rearrange("(c p) x -> p c x", p=128), ups[sl][:, :, 0:1],
                  reads=[("up", 0, 0), ("up", 0, 1)], semkey=("upst", 0), allow_slow_non_contiguous=True)
        if st == NST - 1:
            P.dma("pool", d["payH"][:, 1:2].rearrange("(c p) x -> p c x", p=128), ups[sl][:, :, 511:512],
                  reads=[("up", 0, 0), ("up", 0, 1)], semkey=("upst", 0), allow_slow_non_contiguous=True)
        for q in range(2):
            p, pk = zproj(C_F + q * 128, 128)
            cpy(P, "act" if q % 2 else "dve", fs[sl][:, q, :], p[:, :], [pk], [("f", sl, q)])
            P.dma("pool", d["payF"][q][st * 512:(st + 1) * 512, :].rearrange("(j c) b -> c j b", c=128),
                  fs[sl][:, q, :].rearrange("c (j b) -> c j b", b=128), reads=[("f", sl, q)], semkey=("fst", sl, q))
        def q_head(h):
            pa, pak = pb.next()
            pbb, pbk = pb.next()
            for c in range(3):
                mm(P, pa[0:96, :], wuq[:, c, h * 96:(h + 1) * 96], cqns[sl][:, c, :], c == 0, c == 2,
                   [("wuq", c), ("cqn", sl, c)], [pak])
            for c in range(3):
                mm(P, pbb[0:96, :], wuq[:, c, 768 + h * 96:768 + (h + 1) * 96], cqns[sl][:, c, :], c == 0, c == 2,
                   [("wuq", c), ("cqn", sl, c)], [pbk])
            act(P, qTs[sl][0:64, h, :], pa[0:64, :], AF.Copy, [pak], [("qTn", sl, h)], scale=QSCALE)
            t1, t1k = qt1.next()
            t2, t2k = qt2.next()
            tt(P, "dve", t1[64:96, :], pa[64:96, :], ropes[sl][64:96, 0, :], ALU.mult, [pak, K("rope")], [t1k])
            tt(P, "dve", t2[64:96, :], pbb[64:96, :], ropes[sl][64:96, 1, :], ALU.mult, [pbk, K("rope")], [t2k])
            tt(P, "dve", qTs[sl][64:96, h, :], t1[64:96, :], t2[64:96, :], ALU.add, [t1k, t2k], [("qTr", sl, h)])

        for h in range(0, NH, 2):
            P.interleave([lambda h=h: q_head(h), lambda h=h: q_head(h + 1)])
        P.dma("pool", d["qT"][:, :, t0:t0 + 512].rearrange("h r t -> r h t"), qTs[sl][:, :, :],
              reads=[("qTn", sl, h) for h in range(NH)] + [("qTr", sl, h) for h in range(NH)], semkey=("qst", sl))
        for k2 in range(4):
            p, pk = pb.next()
            for c in range(2):
                mm(P, p[:, :], wk[:, c, k2 * 128:(k2 + 1) * 128], ckvns[sl][:, c, :], c == 0, c == 1,
                   [("wk", c), ("ckvn", sl, c)], [pk])
            cpy(P, "act" if k2 % 2 else "dve", kTs[sl][:, k2, :], p[:, :], [pk], [("kT", sl, k2)])
        for k2 in range(4):
            P.dma("pool", d["payK"][k2][:, t0:t0 + 512], kTs[sl][:, k2, :], reads=[("kT", sl, k2)], semkey=("kst", sl))
        for j in range(4):
            p, pk = pb.next()
            for c in range(2):
                mm(P, p[:, :], ckvns[sl][:, c, j * 128:(j + 1) * 128], wv[:, c, :], c == 0, c == 1,
                   [("wv", c), ("ckvn", sl, c)], [pk])
            cpy(P, "act" if j % 2 else "dve", vbufs[sl][:, :, j, 0:64], p[:, :].rearrange("p (h c) -> p h c", h=NH), [pk],
                [("vb", sl, j)])
        for h in range(NH):
            P.dma("pool", d["payV"][h].rearrange("p (t c) -> p t c", c=65)[:, st * 4:st * 4 + 4, :],
                  vbufs[sl][:, h, :, :], reads=[("vones", sl)] + [("vb", sl, j) for j in range(4)], semkey=("vst", sl))
    P.emit()


KB = 2


def phase_B(nc, l, d):
    P = Phase(nc, f"B{l}", d.get("pool"))
    onesf = P.sb([128, 64], F32)
    mset(P, "pool", onesf[:], 1.0, ["onesf"])
    KTs = [P.sb([96, SEQ], BF16) for _ in range(2)]
    Vs = [P.sb([128, 4, 32, 65], BF16) for _ in range(2)]
    QTs = [P.sb([96, TOK], BF16) for _ in range(2)]
    sps = Rot([P.ps([128, KB, 512], F32) for _ in range(3)], "sp")
    pos = Rot([P.ps([128, 512], F32) for _ in range(1)], "po")
    bcs = Rot([P.ps([64, 512], F32) for _ in range(1)], "bc")
    pts = Rot([P.sb([128, KB, 512], BF16) for _ in range(4)], "pt")
    rsum = Rot([P.sb([128, 512], F32) for _ in range(2)], "rsum")
    rrec = Rot([P.sb([128, 512], F32) for _ in range(2)], "rrec")
    bcsb = Rot([P.sb([64, 512], F32) for _ in range(2)], "bcsb")
    oTs = Rot([P.sb([64, 512], BF16) for _ in range(2)], "oT")

    order = [("Kr", None), ("K", 0), ("V", 0), ("V", 1)]
    for k2 in range(1, 4):
        order += [("K", k2), ("V", 2 * k2), ("V", 2 * k2 + 1)]
    order += [("F", 0), ("F", 1), ("H", None)]
    prev = None
    for kind, i in order:
        name = {"Kr": "Kr", "K": "K", "V": "V", "F": "F", "H": "H"}[kind]
        src = d["pay" + name + "_t"] if i is None else d["pay" + name + "_t"][i]
        dst = d["g" + name + "_t"] if i is None else d["g" + name + "_t"][i]
        P.allgather(src, dst, reads=[prev] if prev else [], writes=[("dram", "g" + name, i), ("ccchain", kind, i)],
                    semkey=("cc", kind, i))
        prev = ("ccchain", kind, i)

    def load_head(h):
        s = h % 2
        for r in range(4):
            P.dma("sp", KTs[s][0:64, r * TOK:(r + 1) * TOK],
                  d["gK"][h // 2][r * 128 + (h % 2) * 64:r * 128 + (h % 2) * 64 + 64, :],
                  reads=[("dram", "gK", h // 2)], writes=[("KTn", s, r)])
            P.dma("sp", KTs[s][64:96, r * TOK:(r + 1) * TOK], d["gKr"][r * 32:(r + 1) * 32, :],
                  reads=[("dram", "gKr", None)], writes=[("KTr", s, r)])
            P.dma("sp", Vs[s][:, r, :, :].rearrange("p t c -> p (t c)"), d["gV"][h][r * 128:(r + 1) * 128, :],
                  reads=[("dram", "gV", h)], writes=[("V", s, r)])
        P.dma("sp", QTs[s][:, :], d["qT"][h, :, :], writes=[("QT", s)])

    wstg = Rot([P.sb([128, 2048], F32) for _ in range(2)], "wstg")
    wrow = Rot([P.sb([128, 6144], BF16) for _ in range(2)], "wrow")

    def precast(ex):
        row, rowk = wrow.next()
        for j, (nm, f) in enumerate((("w_gate", DE), ("w_up", DE), ("w_down", DM))):
            st_, stk = wstg.next()
            P.dma("sp", st_[:, :].rearrange("p (k f) -> p k f", f=f), d[nm][l, ex].rearrange("(k p) f -> p k f", p=128),
                  writes=[stk])
            cpy(P, "pool", row[:, j * 2048:(j + 1) * 2048], st_[:, :], [stk], [(rowk, j)])
        P.dma("sp", d["wbf"][ex * 128:(ex + 1) * 128, :], row[:, :], reads=[(rowk, j) for j in range(3)], semkey=("wrowst", rowk))

    load_head(0)
    nkb = SEQ // 128 // KB
    its = [(h, qt, kb) for h in range(NH) for qt in range(TOK // 512) for kb in range(nkb)]
    state = {}

    def emit_S(it):
        h, qt, kb = it
        s = h % 2
        if qt == 0 and kb == 0 and h + 1 < NH:
            load_head(h + 1)
        if kb == 8 and (h * 8 + qt) % 2 == 0:
            precast((h * 8 + qt) // 2)
        ps, psk = sps.next()
        for u in range(KB):
            kt = kb * KB + u
            r = kt // 32
            mm(P, ps[:, u, :], KTs[s][0:96, kt * 128:(kt + 1) * 128], QTs[s][0:96, qt * 512:(qt + 1) * 512], True, True,
               [("KTn", s, r), ("KTr", s, r), ("QT", s)], [psk])
        state[it] = (ps, psk)

    def emit_rest(it):
        h, qt, kb = it
        s = h % 2
        V = Vs[s]
        ps, psk = state.pop(it)
        if kb == 0:
            state["po"] = pos.next()
        po, pok = state["po"]
        pt, ptk = pts.next()
        act(P, pt[:, :, :], ps[:, :, :], AF.Exp, [psk], [ptk])
        for u in range(KB):
            kt = kb * KB + u
            r, t = kt // 32, kt % 32
            mm(P, po[0:65, :], V[:, r, t, :], pt[:, u, :], kt == 0, kt == SEQ // 128 - 1, [("V", s, r), ptk], [pok])
        if kb == nkb - 1:
            rs_, rsk = rsum.next()
            rr_, rrk = rrec.next()
            bc, bck = bcs.next()
            bs_, bsk = bcsb.next()
            oT, oTk = oTs.next()
            cpy(P, "dve", rs_[64:65, :], po[64:65, :], [pok], [rsk])
            P.add("dve", lambda e, o=rr_, i=rs_: e.reciprocal(out=o[64:65, :], in_=i[64:65, :]), [rsk], [rrk])
            mm(P, bc[0:64, :], onesf[64:65, 0:64], rr_[64:65, :], True, True, ["onesf", rrk], [bck])
            cpy(P, "dve", bs_[:, :], bc[:, :], [bck], [bsk])
            tt(P, "dve", oT[:, :], po[0:64, :], bs_[:, :], ALU.mult, [pok, bsk], [oTk])
            P.dma("sp", d["omixT"][h * 64:(h + 1) * 64, qt * 512:(qt + 1) * 512], oT[:, :], reads=[oTk])

    emit_S(its[0])
    emit_S(its[1])
    for i, it in enumerate(its):
        if i + 2 < len(its):
            emit_S(its[i + 2])
        emit_rest(it)
    P.emit()


def phase_C1(nc, l, d):
    P = Phase(nc, f"C{l}", d.get("pool"))
    wc = P.sb([128, 2, 3], F32)
    sel = P.sb([128, 2, 4], F32)
    hal = P.sb([128, 2, 4, 16], F32)
    tmp = P.sb([128, 2, 4], F32)
    P.dma("sp", wc[:], d["w_conv"][l, :, :, :], writes=["wc"])
    P.dma("sp", sel[:], d["halo_sel"][:, :, :], writes=["sel"])
    for r in range(4):
        P.dma("sp", hal[:, :, r, :], d["gH"][r * 256:(r + 1) * 256, :].rearrange("(c p) x -> p c x", p=128), writes=[("hal", r)])
    halk = [("hal", r) for r in range(4)]
    upx = [P.sb([128, TOK + 2], F32) for _ in range(2)]
    CB = 1024
    bgb = Rot([P.sb([128, CB], F32) for _ in range(2)], "bgb")
    acc = Rot([P.sb([128, CB], F32) for _ in range(2)], "acc")
    ob = Rot([P.sb([128, CB], BF16) for _ in range(2)], "ob")
    for c in range(2):
        u = upx[c]
        P.dma("sp", u[:, 1:TOK + 1], d["upT"][c * 128:(c + 1) * 128, :], writes=[("upx", c)])
        tt(P, "dve", tmp[:, 0, :], hal[:, c, :, 1], sel[:, 0, :], ALU.mult, halk + ["sel"], [("tmpL", c)])
        P.add("dve", lambda e, o=u, i=tmp: e.reduce_sum(out=o[:, 0:1], in_=i[:, 0, :], axis=AX.X), [("tmpL", c)], [("upxL", c)])
        tt(P, "dve", tmp[:, 1, :], hal[:, c, :, 0], sel[:, 1, :], ALU.mult, halk + ["sel"], [("tmpR", c)])
        P.add("dve", lambda e, o=u, i=tmp: e.reduce_sum(out=o[:, TOK + 1:TOK + 2], in_=i[:, 1, :], axis=AX.X), [("tmpR", c)],
              [("upxR", c)])
        ukeys = [("upx", c), ("upxL", c), ("upxR", c)]
        for blk in range(TOK // CB):
            c0 = blk * CB
            b_, bk = bgb.next()
            a_, ak = acc.next()
            o_, ok = ob.next()
            P.dma("sp", b_[:, :], d["bgT"][c * 128:(c + 1) * 128, c0:c0 + CB], writes=[bk])
            eng = "dve"
            ts(P, eng, a_[:, :], u[:, c0:c0 + CB], wc[:, c, 0:1], None, ALU.mult, None, ukeys + ["wc"], [ak])
            stt(P, eng, a_[:, :], u[:, c0 + 1:c0 + 1 + CB], wc[:, c, 1:2], a_[:, :], ALU.mult, ALU.add, ukeys + ["wc", ak], [ak])
            stt(P, eng, a_[:, :], u[:, c0 + 2:c0 + 2 + CB], wc[:, c, 2:3], a_[:, :], ALU.mult, ALU.add, ukeys + ["wc", ak], [ak])
            tt(P, eng, o_[:, :], a_[:, :], b_[:, :], ALU.mult, [ak, bk], [ok])
            P.dma("act", d["omixT"][512 + c * 128:512 + (c + 1) * 128, c0:c0 + CB], o_[:, :], reads=[ok])
    P.emit()


def phase_C2(nc, l, d):
    P = Phase(nc, f"F{l}", d.get("pool"))
    stg = P.sb([128, 128 * 96 // 4], F32)
    T1 = P.sb([128, 256], BF16)
    T2 = P.sb([128, 128, 96], BF16)
    BD = P.sb([128, 2, 128], BF16)
    P.dma("sp", stg[:, 0:256], d["dft1"][:, :], writes=["stg"])
    cpy(P, "dve", T1[:, :], stg[:, 0:256], ["stg"], ["T1"])
    P.dma("sp", stg[:, 0:256], d["dftc"][:, :], writes=["stg"])
    cpy(P, "dve", BD[:, :, :].rearrange("p a b -> p (a b)"), stg[:, 0:256], ["stg"], ["BD"])
    for qq in range(4):
        P.dma("sp", stg[:, :], d["dft2"][:, qq * 32:(qq + 1) * 32, :].rearrange("p a b -> p (a b)"), writes=["stg"])
        cpy(P, "dve" if qq % 2 else "pool", T2[:, qq * 32:(qq + 1) * 32, :].rearrange("p a b -> p (a b)"), stg[:, :], ["stg"],
            [("T2", qq)])
    t2keys = [("T2", qq) for qq in range(4)]
    Fq = P.sb([128, 128, 128], BF16)
    Y1 = P.sb([128, 128, 256], BF16)
    X = P.sb([128, 128, 64], BF16)
    oF = P.sb([128, TOK], BF16)
    ps1 = Rot([P.ps([128, 2, 256], F32) for _ in range(3)], "ps1")
    ps2 = Rot([P.ps([128, 8, 64], F32) for _ in range(3)], "ps2")
    ps3 = Rot([P.ps([128, 512], F32) for _ in range(2)], "ps3")
    for q in range(2):
        src = d["gF"][q].rearrange("(a c) b -> a c b", c=128)
        for part in range(4):
            P.dma("sp", Fq[:, part * 32:(part + 1) * 32, :], src[:, part * 32:(part + 1) * 32, :], reads=[("dram", "gF", q)],
                  writes=[("Fq", part)])
        fkeys = [("Fq", part) for part in range(4)]
        for cp in range(64):
            p, pk = ps1.next()
            for u in range(2):
                c = cp * 2 + u
                mm(P, p[:, u, :], Fq[:, c, :], T1[:, :], True, True, fkeys + ["T1"], [pk])
            cpy(P, "act" if cp % 2 else "dve", Y1[:, cp * 2:cp * 2 + 2, :], p[:, :, :], [pk], [("Y1", cp)])
        y1keys = [("Y1", i) for i in range(64)]
        if "dbgY1" in d and q == 0:
            P.dma("sp", d["dbgY1"][:, :], Y1[:, :, :].rearrange("p a b -> p (a b)"), reads=y1keys, semkey="dbgY1")
            P.dma("sp", d["dbgF"][:, :], Fq[:, :, :].rearrange("p a b -> p (a b)"), reads=fkeys, semkey="dbgF")
        for kg in range(16):
            p, pk = ps2.next()
            for u in range(8):
                k1 = kg * 8 + u
                mm(P, p[:, u, :], Y1[:, :, k1], T2[:, k1, 32:96], True, False, y1keys + t2keys, [pk])
                mm(P, p[:, u, :], Y1[:, :, 128 + k1], T2[:, k1, 0:64], False, True, y1keys + t2keys, [pk])
            cpy(P, "act" if kg % 2 else "dve", X[:, kg * 8:(kg + 1) * 8, :], p[:, :, :], [pk], [("X", kg)])
        if "dbgX" in d and q == 0:
            P.dma("sp", d["dbgX"][:, :], X[:, :, :].rearrange("p a b -> p (a b)"), reads=[("X", i) for i in range(16)], semkey="dbgX")
        for m in range(8):
            p, pk = ps3.next()
            mm(P, p[:, :].rearrange("p (a b) -> p a b", b=32), BD[:, 0, :], X[:, m * 16:(m + 1) * 16, 0:32], True, False,
               [("X", 2 * m), ("X", 2 * m + 1), "BD"], [pk])
            mm(P, p[:, :].rearrange("p (a b) -> p a b", b=32), BD[:, 1, :], X[:, m * 16:(m + 1) * 16, 32:64], False, True,
               [("X", 2 * m), ("X", 2 * m + 1), "BD"], [pk])
            cpy(P, "act" if m % 2 else "dve",
                oF[:, :].rearrange("p (k2 k1) -> p k1 k2", k1=128)[:, m * 16:(m + 1) * 16, :],
                p[:, :].rearrange("p (a b) -> p a b", b=32), [pk], [("oF", m)])
        P.dma("sp", d["omixT"][768 + q * 128:768 + (q + 1) * 128, :], oF[:, :], reads=[("oF", m) for m in range(8)])
    P.emit()


WPAD = 256
NSLOT_T = (2 * TOK + NE * (WPAD - 1)) // 128 + 1
NSLOT = NSLOT_T * 128
BLK = 1024


def zero_slots(P, d):
    z = P.sb([128, DM], BF16)
    mset(P, "pool", z[:, :], 0.0, ["z"])
    for i in range(NSLOT_T):
        P.dma("pool", d["xs"][i * 128:(i + 1) * 128, :], z[:, :], reads=["z"], semkey="zst")


def phase_D(nc, l, d, last):
    phase_Da(nc, l, d)
    phase_Db(nc, l, d, last)


def phase_Da(nc, l, d):
    P = Phase(nc, f"D{l}", d.get("pool"))
    NT = TOK // 128
    NTB = BLK // 128
    identf = P.sb([128, 128], F32)
    ident = P.sb([128, 128], BF16)
    epsl = P.sb([128, 1], F32)
    wout = P.sb([128, 8, DM], BF16)
    wrt = P.sb([128, 8, 36], BF16)
    brt = P.sb([128, 36], F32)
    lnp = P.sb([128, 2, DM], F32)
    sortc = P.sb([128, 128 + NSLOT_T + 1], F32)
    ltri = P.sb([128, 128], BF16)
    ones = P.sb([128, 128], BF16)
    stg = Rot([P.sb([128, 2048], F32) for _ in range(1)], "stg")
    mset(P, "pool", epsl[:], LN_EPS, ["eps"])
    mset(P, "pool", ones[:], 1.0, ["ones"])
    P.dma("sp", identf[:], d["ident"][:, :], writes=["identf"])
    cpy(P, "dve", ident[:], identf[:], ["identf"], ["ident"])
    P.dma("sp", sortc[:], d["sortc"][:, :], writes=["sortc"])
    cpy(P, "dve", ltri[:], sortc[:, 0:128], ["sortc"], ["ltri"])
    tstart = sortc[:, 128:128 + NSLOT_T]
    pidx = sortc[:, 128 + NSLOT_T:128 + NSLOT_T + 1]
    P.dma("sp", brt[:], d["b_rt"][l, :, :], writes=["brt"])
    for i in range(2):
        P.dma("sp", lnp[:, i, :], d["lnp"][2 + 4 * l + i, :, :], writes=[("lnp", i)])
    for kc in range(8):
        s, sk = stg.next()
        P.dma("sp", s[:, 0:1024], d["w_out"][l, kc * 128:(kc + 1) * 128, :], writes=[sk])
        P.dma("sp", s[:, 1024:1060], d["w_rt"][l, kc * 128:(kc + 1) * 128, :], writes=[sk])
        cpy(P, "dve" if kc % 2 else "pool", wout[:, kc, :], s[:, 0:1024], [sk], [("wout", kc)])
        cpy(P, "dve", wrt[:, kc, :], s[:, 1024:1060], [sk], [("wrt", kc)])

    om = P.sb([128, 8, BLK], BF16)
    hts = Rot([P.sb([128, DM], F32) for _ in range(4)], "ht")
    res = Rot([P.sb([128, DM], F32) for _ in range(4)], "res")
    h1s = Rot([P.sb([128, DM], F32) for _ in range(3)], "h1")
    h1b = Rot([P.sb([128, DM], BF16) for _ in range(6)], "h1b")
    h1Ts = Rot([P.sb([128, 8, 128], BF16) for _ in range(2)], "h1T")
    lts = [ln_tmp(P), ln_tmp(P)]
    pb = Rot([P.ps([128, 512], F32) for _ in range(5)], "pb")
    lgps = Rot([P.ps([128, 512], F32) for _ in range(1)], "lgp")
    trp = Rot([P.ps([128, 1024], BF16) for _ in range(2)], "trp")
    M1 = P.sb([128, NT, NE], F32)
    M2 = P.sb([128, NT, NE], F32)
    W1, W2, posi, idxw = d["sbW1"], d["sbW2"], d["sbposi"], d["sbidxw"]
    R = {k: P.sb(shape, F32) for k, shape in dict(
        lg=[128, NTB, 36], m4=[128, NTB], d4=[128, NTB, 4], e4=[128, NTB, 4], s4=[128, NTB], pg=[128, NTB],
        oh=[128, NTB, 4], t48=[128, NTB, 4, 8], el=[128, NTB, 8], m1=[128, NTB], k1=[128, NTB, 8], el2=[128, NTB, 8],
        m2=[128, NTB], k2=[128, NTB, 8], dd=[128, NTB], ee=[128, NTB], p1=[128, NTB], p2=[128, NTB]).items()}

    def rk(n):
        return ("R", n)

    g1, b1 = lnp[:, 0, :], lnp[:, 1, :]
    bc3 = lambda ap, n: ap.unsqueeze(2).broadcast_to([128, NTB, n])
    for nb in range(TOK // BLK):
        tb = nb * BLK
        ts_ = slice(nb * NTB, (nb + 1) * NTB)
        for mc in range(8):
            P.dma("sp", om[:, mc, :], d["omixT"][mc * 128:(mc + 1) * 128, tb:tb + BLK], writes=[("om", mc)])
        lgp, lgk = lgps.next()
        st1 = {}

        def d1_a(t):
            ht, hk = hts.next()
            P.dma("sp", ht[:, :], d["hbuf"][tb + t * 128:tb + (t + 1) * 128, :], writes=[hk])
            r_, rk_ = res.next()
            for half in range(2):
                p, pk = pb.next()
                for mc in range(8):
                    mm(P, p[:, :], om[:, mc, t * 128:(t + 1) * 128], wout[:, mc, half * 512:(half + 1) * 512], mc == 0, mc == 7,
                       [("om", mc), ("wout", mc)], [pk])
                stt(P, "dve", r_[:, half * 512:(half + 1) * 512], ht[:, half * 512:(half + 1) * 512], ALPHA, p[:, :],
                    ALU.mult, ALU.add, [hk, pk], [rk_])
            st1[t] = (r_, rk_)

        def d1_b(t):
            r_, rk_ = st1.pop(t)
            h1, h1k = h1s.next()
            layer_norm_tile(P, r_[:, :], rk_, h1[:, :], h1k, g1, b1, [("lnp", 0), ("lnp", 1)], lts[t % 2], epsl[:, 0:1], ("lnD", t % 2))
            P.dma("act", d["h1buf"][tb + t * 128:tb + (t + 1) * 128, :], h1[:, :], reads=[h1k], writes=[("dram", "h1")],
                  semkey="h1st")
            hb, hbk = h1b.next()
            cpy(P, "act", hb[:, :], h1[:, :], [h1k], [hbk])
            P.dma("act", d["h1b"][tb + t * 128:tb + (t + 1) * 128, :], hb[:, :], reads=[hbk], writes=[("dram", "h1b")],
                  semkey="h1bst")
            tp, tk = trp.next()
            for kc in range(8):
                trn(P, tp[:, kc * 128:(kc + 1) * 128], hb[:, kc * 128:(kc + 1) * 128], ident[:], [hbk, "ident"], [tk])
            hT, hTk = h1Ts.next()
            cpy(P, "dve", hT[:, :, :], tp[:, :].rearrange("p (k t) -> p k t", k=8), [tk], [hTk])
            st1[("hT", t)] = (hT, hTk)

        def d1_c(t):
            hT, hTk = st1.pop(("hT", t))
            for kc in range(8):
                mm(P, lgp[:, t * 36:(t + 1) * 36], hT[:, kc, :], wrt[:, kc, :], kc == 0, kc == 7, [hTk, ("wrt", kc)], [lgk])

        d1_a(0)
        d1_a(1)
        for t in range(0, NTB, 2):
            if t + 2 < NTB:
                d1_a(t + 2)
                d1_a(t + 3)
            P.interleave([lambda t=t: d1_b(t), lambda t=t: d1_b(t + 1)])
            d1_c(t)
            d1_c(t + 1)
        lg = R["lg"]
        tt(P, "dve", lg[:, :, :], lgp[:, 0:NTB * 36].rearrange("p (t c) -> p t c", c=36),
           brt[:, :].unsqueeze(1).broadcast_to([128, NTB, 36]), ALU.add, [lgk, "brt"], [rk("lg")])
        P.add("dve", lambda e: e.reduce_max(out=R["m4"][:, :], in_=lg[:, :, 0:4], axis=AX.X), [rk("lg")], [rk("m4")])
        tt(P, "dve", R["d4"][:, :, :], lg[:, :, 0:4], bc3(R["m4"][:, :], 4), ALU.subtract, [rk("lg"), rk("m4")], [rk("d4")])
        act(P, R["e4"][:, :, :], R["d4"][:, :, :], AF.Exp, [rk("d4")], [rk("e4")])
        P.add("dve", lambda e: e.reduce_sum(out=R["s4"][:, :], in_=R["e4"][:, :, :], axis=AX.X), [rk("e4")], [rk("s4")])
        P.add("dve", lambda e: e.reciprocal(out=R["pg"][:, :], in_=R["s4"][:, :]), [rk("s4")], [rk("pg")])
        ts(P, "dve", R["oh"][:, :, :], R["d4"][:, :, :], 0.0, None, ALU.is_equal, None, [rk("d4")], [rk("oh")])
        tt(P, "dve", R["t48"][:, :, :, :], lg[:, :, 4:36].rearrange("p t (g e) -> p t g e", e=8),
           R["oh"][:, :, :].unsqueeze(3).broadcast_to([128, NTB, 4, 8]), ALU.mult, [rk("lg"), rk("oh")], [rk("t48")])
        P.add("dve", lambda e: e.reduce_sum(out=R["el"][:, :, :], in_=R["t48"][:, :, :, :].rearrange("p t g e -> p t e g"),
                                            axis=AX.X), [rk("t48")], [rk("el")])
        P.add("dve", lambda e: e.reduce_max(out=R["m1"][:, :], in_=R["el"][:, :, :], axis=AX.X), [rk("el")], [rk("m1")])
        tt(P, "dve", R["k1"][:, :, :], R["el"][:, :, :], bc3(R["m1"][:, :], 8), ALU.is_equal, [rk("el"), rk("m1")], [rk("k1")])
        stt(P, "dve", R["el2"][:, :, :], R["k1"][:, :, :], -1.0e30, R["el"][:, :, :], ALU.mult, ALU.add, [rk("k1"), rk("el")],
            [rk("el2")])
        P.add("dve", lambda e: e.reduce_max(out=R["m2"][:, :], in_=R["el2"][:, :, :], axis=AX.X), [rk("el2")], [rk("m2")])
        tt(P, "dve", R["k2"][:, :, :], R["el2"][:, :, :], bc3(R["m2"][:, :], 8), ALU.is_equal, [rk("el2"), rk("m2")], [rk("k2")])
        tt(P, "dve", R["dd"][:, :], R["m2"][:, :], R["m1"][:, :], ALU.subtract, [rk("m1"), rk("m2")], [rk("dd")])
        act(P, R["ee"][:, :], R["dd"][:, :], AF.Exp, [rk("dd")], [rk("ee")])
        ts(P, "dve", R["p2"][:, :], R["ee"][:, :], 1.0, None, ALU.add, None, [rk("ee")], [rk("p2")])
        P.add("dve", lambda e: e.reciprocal(out=R["p1"][:, :], in_=R["p2"][:, :]), [rk("p2")], [rk("p1")])
        tt(P, "dve", W1[:, ts_], R["p1"][:, :], R["pg"][:, :], ALU.mult, [rk("p1"), rk("pg")], [("W1", nb)])
        tt(P, "dve", W2[:, ts_], W1[:, ts_], R["ee"][:, :], ALU.mult, [("W1", nb), rk("ee")], [("W2", nb)])
        ohb = R["oh"][:, :, :].unsqueeze(3).broadcast_to([128, NTB, 4, 8])
        tt(P, "dve", M1[:, ts_, :].rearrange("p t (g e) -> p t g e", e=8), ohb,
           R["k1"][:, :, :].unsqueeze(2).broadcast_to([128, NTB, 4, 8]), ALU.mult, [rk("oh"), rk("k1")], [("M1", nb)])
        tt(P, "dve", M2[:, ts_, :].rearrange("p t (g e) -> p t g e", e=8), ohb,
           R["k2"][:, :, :].unsqueeze(2).broadcast_to([128, NTB, 4, 8]), ALU.mult, [rk("oh"), rk("k2")], [("M2", nb)])
    NBK = TOK // BLK
    mkeys = [("M1", nb) for nb in range(NBK)] + [("M2", nb) for nb in range(NBK)]
    wkeys = [("W1", nb) for nb in range(NBK)] + [("W2", nb) for nb in range(NBK)]

    M12 = P.sb([128, NT * NE], BF16)
    Wn = P.sb([128, NT, NE], F32)
    TA = P.sb([128, NT, NE], F32)
    TB = P.sb([128, NT, NE], F32)
    T0 = P.sb([128, NT, NE], F32)
    SL = P.sb([128, NT, NE], F32)
    ea = P.sb([128, NE], F32)
    eb = P.sb([128, NE], F32)
    pcf = P.sb([128, NE], F32)
    pci = P.sb([128, NE], I32)
    bexc = P.sb([128, NE], F32)
    posf = P.sb([128, 2, NT], F32)
    cmp_ = P.sb([128, NSLOT_T, NE], F32)
    tef = P.sb([128, NSLOT_T], F32)
    PR1 = P.sb([128, NT, NE], F32)
    PR2 = P.sb([128, NT, NE], F32)
    tt(P, "dve", M12[:, :].rearrange("p (t e) -> p t e", e=NE), M1[:, :, :], M2[:, :, :], ALU.add, mkeys, ["M12"])
    for half in range(2):
        p, pk = pb.next()
        mm(P, p[:, :], ltri[:, :], M12[:, half * 512:(half + 1) * 512], True, True, ["ltri", "M12"], [pk])
        cpy(P, "act", Wn[:, half * 16:(half + 1) * 16, :].rearrange("p t e -> p (t e)"), p[:, :], [pk], [("Wn", half)])
        p, pk = pb.next()
        mm(P, p[:, :], ones[:, :], M12[:, half * 512:(half + 1) * 512], True, True, ["ones", "M12"], [pk])
        cpy(P, "dve", T0[:, half * 16:(half + 1) * 16, :].rearrange("p t e -> p (t e)"), p[:, :], [pk], [("T0", half)])
    P.add("dve", lambda e: e.tensor_copy(out=TA[:, :, :], in_=T0[:, :, :]), [("T0", 0), ("T0", 1)], ["TA"])
    cur, curk, oth, othk = TA, "TA", TB, "TB"
    for sft in (1, 2, 4, 8, 16):
        cpy(P, "dve", oth[:, 0:sft, :], cur[:, 0:sft, :], [curk], [othk])
        tt(P, "dve", oth[:, sft:NT, :], cur[:, sft:NT, :], cur[:, 0:NT - sft, :], ALU.add, [curk, othk], [othk])
        cur, curk, oth, othk = oth, othk, cur, curk
    incl, inclk = cur, curk
    cpy(P, "dve", pci[:, :], incl[:, NT - 1, :], [inclk], ["pci"])
    P.add("dve", lambda e: e.tensor_single_scalar(out=pci[:, :], in_=pci[:, :], scalar=WPAD - 1, op=ALU.add), ["pci"], ["pci"])
    P.add("dve", lambda e: e.tensor_single_scalar(out=pci[:, :], in_=pci[:, :], scalar=8, op=ALU.arith_shift_right), ["pci"], ["pci"])
    P.add("dve", lambda e: e.tensor_single_scalar(out=pci[:, :], in_=pci[:, :], scalar=8, op=ALU.logical_shift_left), ["pci"], ["pci"])
    cpy(P, "dve", pcf[:, :], pci[:, :], ["pci"], ["pcf"])
    cpy(P, "dve", ea[:, :], pcf[:, :], ["pcf"], ["ea"])
    c2, c2k, o2, o2k = ea, "ea", eb, "eb"
    for sft in (1, 2, 4, 8, 16):
        cpy(P, "dve", o2[:, 0:sft], c2[:, 0:sft], [c2k], [o2k])
        tt(P, "dve", o2[:, sft:NE], c2[:, sft:NE], c2[:, 0:NE - sft], ALU.add, [c2k, o2k], [o2k])
        c2, c2k, o2, o2k = o2, o2k, c2, c2k
    pend, pendk = c2, c2k
    tt(P, "dve", bexc[:, :], pend[:, :], pcf[:, :], ALU.subtract, [pendk, "pcf"], ["bexc"])
    tt(P, "dve", SL[:, :, :], incl[:, :, :], T0[:, :, :], ALU.subtract, [inclk, ("T0", 0), ("T0", 1)], ["SL"])
    tt(P, "dve", SL[:, :, :], SL[:, :, :], Wn[:, :, :], ALU.add, ["SL", ("Wn", 0), ("Wn", 1)], ["SL"])
    tt(P, "dve", SL[:, :, :], SL[:, :, :], bexc[:, :].unsqueeze(1).broadcast_to([128, NT, NE]), ALU.add, ["SL", "bexc"], ["SL"])
    tt(P, "dve", PR1[:, :, :], SL[:, :, :], M1[:, :, :], ALU.mult, ["SL"] + mkeys, ["PR1"])
    P.add("dve", lambda e: e.reduce_sum(out=posf[:, 0, :], in_=PR1[:, :, :], axis=AX.X), ["PR1"], [("posf", 0)])
    tt(P, "dve", PR2[:, :, :], SL[:, :, :], M2[:, :, :], ALU.mult, ["SL"] + mkeys, ["PR2"])
    P.add("dve", lambda e: e.reduce_sum(out=posf[:, 1, :], in_=PR2[:, :, :], axis=AX.X), ["PR2"], [("posf", 1)])
    cpy(P, "dve", posi[:, :, :], posf[:, :, :], [("posf", 0), ("posf", 1)], ["posi"])
    tt(P, "dve", cmp_[:, :, :], pend[:, :].unsqueeze(1).broadcast_to([128, NSLOT_T, NE]),
       tstart.unsqueeze(2).broadcast_to([128, NSLOT_T, NE]), ALU.is_le, [pendk, "sortc"], ["cmp"])
    P.add("dve", lambda e: e.reduce_sum(out=tef[:, :], in_=cmp_[:, :, :], axis=AX.X), ["cmp"], ["tef"])
    ts(P, "dve", tef[:, :], tef[:, :], float(NE - 1), 128.0, ALU.min, ALU.mult, ["tef"], ["tef"])
    ts(P, "dve", tef[:, :], tef[:, :], pidx, None, ALU.add, None, ["tef", "sortc"], ["tef"])
    cpy(P, "dve", idxw[:, :], tef[:, :], ["tef"], ["idxw"])

    for t in range(0 if "no_scatter" not in d else NT, NT):
        hb, hbk = h1b.next()
        P.dma("sp", hb[:, :], d["h1b"][t * 128:(t + 1) * 128, :], reads=[("dram", "h1b")], writes=[hbk])
        for k in range(2):
            P.add("pool", lambda e, hb=hb, k=k, t=t: e.indirect_dma_start(
                out=d["xs"][:, :], out_offset=bass.IndirectOffsetOnAxis(ap=posi[:, k, t:t + 1], axis=0), in_=hb[:, :], in_offset=None),
                [hbk, "posi"], [("dram", "xs")], dma=True, semkey="xs_sc")
    if "dbg_posi" in d:
        P.dma("sp", d["dbg_posi"][:, :], posi[:, :, :].rearrange("p a b -> p (a b)"), reads=["posi"], semkey="dbg1")
        P.dma("sp", d["dbg_idxw"][:, :], idxw[:, :], reads=["idxw"], semkey="dbg2")
        P.dma("sp", d["dbg_W"][:, 0:NT], W1[:, :], reads=wkeys, semkey="dbg3")
        P.dma("sp", d["dbg_W"][:, NT:2 * NT], W2[:, :], reads=wkeys, semkey="dbg3")
        P.dma("sp", d["dbg_M1"][:, :], M1[:, :, :].rearrange("p a b -> p (a b)"), reads=mkeys, semkey="dbg4")
        P.dma("sp", d["dbg_M2"][:, :], M2[:, :, :].rearrange("p a b -> p (a b)"), reads=mkeys, semkey="dbg4")
    P.emit()


def phase_Db(nc, l, d, last):
    P = Phase(nc, f"E{l}", d.get("pool"))
    NT = TOK // 128
    identf = P.sb([128, 128], F32)
    ident = P.sb([128, 128], BF16)
    epsl = P.sb([128, 1], F32)
    lnp = P.sb([128, 2, DM], F32)
    mset(P, "pool", epsl[:], LN_EPS, ["eps"])
    P.dma("sp", identf[:], d["ident"][:, :], writes=["identf"])
    cpy(P, "dve", ident[:], identf[:], ["identf"], ["ident"])
    for i in range(2):
        P.dma("sp", lnp[:, i, :], d["lnp"][2 + 4 * l + 2 + i, :, :], writes=[("lnp", 2 + i)])
    g2, b2 = lnp[:, 0, :], lnp[:, 1, :]
    W1, W2, posi, idxw = d["sbW1"], d["sbW2"], d["sbposi"], d["sbidxw"]
    wkeys = []
    hts = Rot([P.sb([128, DM], F32) for _ in range(3)], "ht")
    res = Rot([P.sb([128, DM], F32) for _ in range(4)], "res")
    h1s = Rot([P.sb([128, DM], F32) for _ in range(4)], "h1")
    lts = [ln_tmp(P), ln_tmp(P)]
    pb = Rot([P.ps([128, 512], F32) for _ in range(6)], "pb")
    trp = Rot([P.ps([128, 1024], BF16) for _ in range(1)], "trp")
    tp2 = Rot([P.ps([128, 256], BF16) for _ in range(1)], "tp2")

    wsb = Rot([P.sb([128, 6144], BF16) for _ in range(3)], "wsb")
    xts = Rot([P.sb([128, DM], BF16) for _ in range(4)], "xst")
    xTs = Rot([P.sb([128, 8, 128], BF16) for _ in range(3)], "xT")
    sgs = Rot([P.sb([128, 256], F32) for _ in range(3)], "sg")
    hids = Rot([P.sb([128, 256], BF16) for _ in range(3)], "hid")
    hTs = Rot([P.sb([128, 2, 128], BF16) for _ in range(2)], "hidT")
    yos = Rot([P.sb([128, DM], BF16) for _ in range(2)], "yo")
    wcur = {}
    stX = {}

    def stage_x(i):
        if i % (WPAD // 128) == 0:
            w_, wk = wsb.next()
            P.add("pool", lambda e, w_=w_, i=i: e.indirect_dma_start(
                out=w_[:, :], out_offset=None, in_=d["wbf"][:, :], in_offset=bass.IndirectOffsetOnAxis(ap=idxw[:, i:i + 1], axis=0)),
                [], [wk], dma=True)
            wcur["w"] = (w_, wk)
        w_, wk = wcur["w"]
        x_, xk = xts.next()
        P.dma("sp", x_[:, :], d["xs"][i * 128:(i + 1) * 128, :], reads=[("dram", "xs")], writes=[xk])
        tp, tk = trp.next()
        for kc in range(8):
            trn(P, tp[:, kc * 128:(kc + 1) * 128], x_[:, kc * 128:(kc + 1) * 128], ident[:], [xk, "ident"], [tk])
        xT, xTk = xTs.next()
        cpy(P, "dve" if i % 2 else "act", xT[:, :, :], tp[:, :].rearrange("p (k t) -> p k t", k=8), [tk], [xTk])
        pg, pgk = pb.next()
        wgu = w_[:, 0:4096].rearrange("p (g k f) -> p k g f", g=2, k=8)
        for kc in range(8):
            mm(P, pg[:, :].rearrange("p (g f) -> p g f", g=2), xT[:, kc, :], wgu[:, kc, :, :], kc == 0, kc == 7, [xTk, wk], [pgk])
        sg, sgk = sgs.next()
        act(P, sg[:, :], pg[:, 0:256], AF.Silu, [pgk], [sgk])
        hid, hidk = hids.next()
        tt(P, "dve", hid[:, :], sg[:, :], pg[:, 256:512], ALU.mult, [sgk, pgk], [hidk])
        stX[i] = (w_, wk, hid, hidk)

    def stage_y(i):
        w_, wk, hid, hidk = stX.pop(i)
        t2, t2k = tp2.next()
        for fc in range(2):
            trn(P, t2[:, fc * 128:(fc + 1) * 128], hid[:, fc * 128:(fc + 1) * 128], ident[:], [hidk, "ident"], [t2k])
        hT_, hTk_ = hTs.next()
        cpy(P, "act", hT_[:, :, :], t2[:, :].rearrange("p (k t) -> p k t", k=2), [t2k], [hTk_])
        yo, yok = yos.next()
        for half in range(2):
            pd, pdk = pb.next()
            for fc in range(2):
                mm(P, pd[:, :], hT_[:, fc, :], w_[:, 4096 + fc * 1024 + half * 512:4096 + fc * 1024 + (half + 1) * 512], fc == 0, fc == 1,
                   [hTk_, wk], [pdk])
            cpy(P, "dve" if half else "act", yo[:, half * 512:(half + 1) * 512], pd[:, :], [pdk], [(yok, half)])
        P.dma("act", d["ys"][i * 128:(i + 1) * 128, :], yo[:, :], reads=[(yok, 0), (yok, 1)], writes=[("dram", "ys")], semkey="ys_st")

    stage_x(0)
    for i in range(NSLOT_T):
        if i + 1 < NSLOT_T:
            P.interleave([lambda i=i: stage_x(i + 1), lambda i=i: stage_y(i)])
        else:
            stage_y(i)

    y1s = Rot([P.sb([128, DM], BF16) for _ in range(3)], "y1")
    y2s = Rot([P.sb([128, DM], BF16) for _ in range(3)], "y2")
    fs_ = Rot([P.sb([128, DM], F32) for _ in range(2)], "ff")
    st3 = {}

    def d3_a(t):
        ys_ = []
        for k, rot in ((0, y1s), (1, y2s)):
            y_, yk = rot.next()
            P.add("pool", lambda e, y_=y_, k=k, t=t: e.indirect_dma_start(
                out=y_[:, :], out_offset=None, in_=d["ys"][:, :], in_offset=bass.IndirectOffsetOnAxis(ap=posi[:, k, t:t + 1], axis=0)),
                [("dram", "ys")], [yk], dma=True)
            ys_.append((y_, yk))
        h1, h1k = h1s.next()
        P.dma("sp", h1[:, :], d["h1buf"][t * 128:(t + 1) * 128, :], reads=[("dram", "h1")], writes=[h1k])
        f_, fk = fs_.next()
        ts(P, "dve", f_[:, :], ys_[0][0][:, :], W1[:, t:t + 1], None, ALU.mult, None, [ys_[0][1]] + wkeys, [fk])
        stt(P, "dve", f_[:, :], ys_[1][0][:, :], W2[:, t:t + 1], f_[:, :], ALU.mult, ALU.add, [ys_[1][1], fk] + wkeys, [fk])
        r_, rk_ = res.next()
        stt(P, "dve", r_[:, :], h1[:, :], ALPHA, f_[:, :], ALU.mult, ALU.add, [h1k, fk], [rk_])
        st3[t] = (r_, rk_)

    def d3_b(t):
        r_, rk_ = st3.pop(t)
        ht, hk = hts.next()
        layer_norm_tile(P, r_[:, :], rk_, ht[:, :], hk, g2, b2, [("lnp", 2), ("lnp", 3)], lts[t % 2], epsl[:, 0:1], ("lnD", t % 2))
        dst = d["y"] if last else d["hbuf"]
        P.dma("act", dst[t * 128:(t + 1) * 128, :], ht[:, :], reads=[hk])

    d3_a(0)
    d3_a(1)
    for t in range(0, NT, 2):
        if t + 2 < NT:
            d3_a(t + 2)
            d3_a(t + 3)
        P.interleave([lambda t=t: d3_b(t), lambda t=t: d3_b(t + 1)])
    P.emit()


def _rope_tables():
    inv = (1.0 / (10000.0 ** (np.arange(0, DR, 2, dtype=np.float32) / DR))).astype(np.float32)
    ang = np.arange(SEQ, dtype=np.float32)[:, None] * inv[None, :]
    cos = np.cos(ang).astype(np.float32).T
    sin = np.sin(ang).astype(np.float32).T
    c2 = np.concatenate([cos, cos], 0)
    s2 = np.concatenate([-sin, sin], 0)
    out = np.zeros((128, 2, SEQ), np.float32)
    out[0:32, 0], out[0:32, 1] = c2, s2
    out[64:96, 0], out[64:96, 1] = c2 * np.float32(QSCALE), s2 * np.float32(QSCALE)
    return out


def _dft_tables():
    n = np.arange(128, dtype=np.float64)
    ang1 = 2 * np.pi * np.outer(n, n) / 128.0
    dft1 = np.concatenate([np.cos(ang1), -np.sin(ang1)], 1).astype(np.float32)
    c = np.arange(64, dtype=np.float64)
    angc = 2 * np.pi * np.outer(c, c) / 64.0
    sc = 1.0 / np.sqrt(SEQ * 64.0)
    bdc = np.kron(np.eye(2), np.cos(angc)) * sc
    bds = np.kron(np.eye(2), np.sin(angc)) * sc
    dftc = np.concatenate([bdc, bds], 1).astype(np.float32)
    per_core = []
    b = np.arange(128, dtype=np.float64)[:, None, None]
    k1 = np.arange(128, dtype=np.float64)[None, :, None]
    for j in range(4):
        k2 = (32 * j + np.arange(32, dtype=np.float64))[None, None, :]
        ang = 2 * np.pi * ((b * (k1 + 128.0 * k2)) % SEQ) / SEQ
        tr, ti = np.cos(ang), -np.sin(ang)
        per_core.append(np.ascontiguousarray(np.concatenate([-ti, tr, ti], 2).astype(np.float32)))
    return dft1, dftc, per_core


def _prep(inp):
    f = np.float32
    L = DEPTH
    w_in = np.asarray(inp["w_in"], f)
    sp = np.cumsum([0, 384, 256, 32, 256, 256, 256, 256])
    cq, ckv, kpe, bg, cg, hc, ff = [w_in[:, :, sp[i]:sp[i + 1]] for i in range(7)]
    kps = np.concatenate([kpe[:, :, 16:32], kpe[:, :, 0:16]], -1)
    w_in2 = np.ascontiguousarray(np.concatenate([cq, ckv, kpe, kps, bg, cg, hc, ff], -1))
    w_uq = np.asarray(inp["w_uq"], f)
    w_uq_sw = np.concatenate([w_uq[..., 0:64], w_uq[..., 80:96], w_uq[..., 64:80]], -1)
    w_uq2 = np.ascontiguousarray(np.stack([w_uq, w_uq_sw], 2).reshape(L, QL, 2 * NH * 96))
    w_ukv = np.asarray(inp["w_ukv"], f)
    w_k = np.ascontiguousarray(w_ukv[..., 0:64].reshape(L, KVL, NH * 64))
    w_v = np.ascontiguousarray(w_ukv[..., 64:128].reshape(L, KVL, NH * 64))
    g_q = np.ascontiguousarray(np.asarray(inp["g_q"], f).reshape(L, 3, 128).transpose(0, 2, 1))
    g_kv = np.ascontiguousarray(np.asarray(inp["g_kv"], f).reshape(L, 2, 128).transpose(0, 2, 1))
    lnp = np.stack([inp["ln_in_g"], inp["ln_in_b"]] + [inp[k][l] for l in range(L) for k in ("ln1_g", "ln1_b", "ln2_g", "ln2_b")])
    lnp = np.ascontiguousarray(np.broadcast_to(np.asarray(lnp, f)[:, None, :], (2 + 4 * L, 128, DM)))
    rope = _rope_tables()
    dft1, dftc, dft2 = _dft_tables()
    w_conv = np.ascontiguousarray(np.asarray(inp["w_conv"], f).reshape(L, 3, 2, 128).transpose(0, 3, 2, 1))
    w_rt = np.ascontiguousarray(np.concatenate([np.asarray(inp["w_group"], f), np.asarray(inp["w_router"], f).reshape(L, DM, NE)], -1))
    b_rt = np.concatenate([np.asarray(inp["b_group"], f), np.asarray(inp["b_router"], f).reshape(L, NE)], -1)
    b_rt = np.ascontiguousarray(np.broadcast_to(b_rt[:, None, :], (L, 128, 36)))
    sortc = np.zeros((128, 128 + NSLOT_T + 1), f)
    sortc[:, 0:128] = np.triu(np.ones((128, 128), f), 1)
    sortc[:, 128:128 + NSLOT_T] = 128.0 * np.arange(NSLOT_T, dtype=f)[None, :]
    sortc[:, 128 + NSLOT_T] = np.arange(128, dtype=f)
    shared = dict(sortc=sortc, w_out=np.asarray(inp["w_out"], f), w_rt=w_rt, b_rt=b_rt,
                  w_gate=np.asarray(inp["w_gate"], f).reshape(L, NE, DM, DE), w_up=np.asarray(inp["w_up"], f).reshape(L, NE, DM, DE),
                  w_down=np.asarray(inp["w_down"], f).reshape(L, NE, DE, DM),
                  w_in=w_in2, w_uq=w_uq2, w_ukv_k=w_k, w_ukv_v=w_v, g_q=g_q, g_kv=g_kv, lnp=lnp,
                  ident=np.eye(128, dtype=f), dft1=dft1, dftc=dftc, w_conv=w_conv)
    x = np.asarray(inp["x"], f)
    maps = []
    for c in range(NCORES):
        b, j = c // 4, c % 4
        m = dict(shared)
        m["x"] = np.ascontiguousarray(x[b, j * TOK:(j + 1) * TOK])
        m["rope"] = np.ascontiguousarray(rope[:, :, j * TOK:(j + 1) * TOK])
        m["dft2"] = dft2[j]
        hs = np.zeros((128, 2, 4), f)
        if j > 0:
            hs[:, 0, j - 1] = 1.0
        if j < 3:
            hs[:, 1, j + 1] = 1.0
        m["halo_sel"] = hs
        maps.append(m)
    return maps


def build(debug=None, stop_after=None, stop_layers=DEPTH):
    nc = bass.Bass("TRN2", target_bir_lowering=False)
    L = DEPTH
    d = {"pool": SemPool(nc)}

    def inp(name, shape, dt=F32):
        d[name] = nc.dram_tensor(name, list(shape), dt, kind="ExternalInput").ap()

    def scr(name, shape, dt):
        kind = "ExternalOutput" if (debug and name in debug) else None
        t = nc.dram_tensor(name, list(shape), dt, kind=kind) if kind else nc.dram_tensor(name, list(shape), dt)
        d[name + "_t"] = t
        d[name] = t.ap()

    inp("x", [TOK, DM]); inp("rope", [128, 2, TOK]); inp("w_in", [L, DM, WIN_COLS]); inp("w_uq", [L, QL, 2 * NH * 96])
    inp("w_ukv_k", [L, KVL, 512]); inp("w_ukv_v", [L, KVL, 512]); inp("g_q", [L, 128, 3]); inp("g_kv", [L, 128, 2])
    inp("lnp", [2 + 4 * L, 128, DM]); inp("ident", [128, 128])
    inp("dft1", [128, 256]); inp("dftc", [128, 256]); inp("dft2", [128, 128, 96]); inp("w_conv", [L, 128, 2, 3])
    inp("halo_sel", [128, 2, 4]); inp("sortc", [128, 128 + NSLOT_T + 1])
    inp("w_out", [L, DM, DM]); inp("w_rt", [L, DM, 36]); inp("b_rt", [L, 128, 36])
    inp("w_gate", [L, NE, DM, DE]); inp("w_up", [L, NE, DM, DE]); inp("w_down", [L, NE, DE, DM])
    scr("hbuf", [TOK, DM], F32)
    scr("h1buf", [TOK, DM], F32)
    scr("wbf", [NE * 128, 6144], BF16)
    scr("h1b", [TOK, DM], BF16)
    scr("xs", [NSLOT, DM], BF16)
    scr("ys", [NSLOT, DM], BF16)
    scr("qT", [NH, 96, TOK], BF16)
    def scrl(name, n, shape, dt):
        ts_ = [nc.dram_tensor(f"{name}{i}", list(shape), dt) for i in range(n)]
        d[name + "_t"] = ts_
        d[name] = [t.ap() for t in ts_]

    scrl("payK", 4, [128, TOK], BF16); scrl("gK", 4, [4 * 128, TOK], BF16)
    scr("payKr", [32, TOK], BF16); scr("gKr", [4 * 32, TOK], BF16)
    scrl("payV", NH, [128, 32 * 65], BF16); scrl("gV", NH, [4 * 128, 32 * 65], BF16)
    scrl("payF", 2, [TOK, 128], BF16); scrl("gF", 2, [4 * TOK, 128], BF16)
    scr("payH", [256, 16], F32); scr("gH", [4 * 256, 16], F32)
    scr("upT", [256, TOK], F32)
    scr("bgT", [256, TOK], F32)
    scr("omixT", [DM, TOK], BF16)
    d["y"] = nc.dram_tensor("y", [TOK, DM], F32, kind="ExternalOutput").ap()
    pst = contextlib.ExitStack()
    d["_pst"] = pst
    d["sbW1"] = pst.enter_context(nc.sbuf_tensor("p_W1", [128, TOK // 128], F32))
    d["sbW2"] = pst.enter_context(nc.sbuf_tensor("p_W2", [128, TOK // 128], F32))
    d["sbposi"] = pst.enter_context(nc.sbuf_tensor("p_posi", [128, 2, TOK // 128], I32))
    d["sbidxw"] = pst.enter_context(nc.sbuf_tensor("p_idxw", [128, NSLOT_T], I32))
    if debug and "dbgF" in debug:
        d["dbgY1"] = nc.dram_tensor("dbgY1", [128, 256 * 128], BF16, kind="ExternalOutput").ap()
        d["dbgF"] = nc.dram_tensor("dbgF", [128, 128 * 128], BF16, kind="ExternalOutput").ap()
        d["dbgX"] = nc.dram_tensor("dbgX", [128, 128 * 64], BF16, kind="ExternalOutput").ap()
    for l in range(stop_layers):
        phase_A(nc, l, d)
        if stop_after == "A":
            break
        phase_B(nc, l, d)
        if stop_after == "B":
            break
        phase_C1(nc, l, d)
        phase_C2(nc, l, d)
        if stop_after == "C":
            break
        phase_D(nc, l, d, last=(l == DEPTH - 1))
    return nc


def kernel(**inputs):
    maps = _prep(inputs)
    nc = build()
    res = run_bass_kernel_spmd(nc, maps, core_ids=list(range(NCORES)))
    out = np.empty((BATCH, SEQ, DM), np.float32)
    for c in range(NCORES):
        out[c // 4, (c % 4) * TOK:(c % 4 + 1) * TOK] = res.results[c]["y"]
    return out
```

```python
import numpy as np
from concourse.bass_utils import run_bass_kernel_spmd
import contextlib
import concourse.bass as bass
import concourse.mybir as mybir

F32 = mybir.dt.float32
BF16 = mybir.dt.bfloat16
I32 = mybir.dt.int32
AF = mybir.ActivationFunctionType
ALU = mybir.AluOpType
AX = mybir.AxisListType

ENGS = ("pe", "act", "dve", "pool", "sp")


class SemPool:
    NHW = 46
    NDMA = 72

    def __init__(self, nc):
        self.stack = contextlib.ExitStack()
        self.eng = {e: self.stack.enter_context(nc.semaphore(f"sem_{e}")) for e in ENGS}
        self.eng_cnt = {e: 0 for e in ENGS}
        self.dma = [self.stack.enter_context(nc.semaphore(f"sem_d{i}")) for i in range(self.NDMA)]
        self.dma_cnt = [0] * self.NDMA


class Phase:
    def __init__(self, nc, name, pool=None):
        self.nc = nc
        self.name = name
        self.pool = pool if pool is not None else SemPool(nc)
        self.ins = []
        self.stack = contextlib.ExitStack()
        self.nbuf = 0

    def sb(self, shape, dt, name=None):
        self.nbuf += 1
        return self.stack.enter_context(self.nc.sbuf_tensor(f"{self.name}_{name or 'sb'}{self.nbuf}", list(shape), dt))

    def ps(self, shape, dt, name=None):
        self.nbuf += 1
        return self.stack.enter_context(self.nc.psum_tensor(f"{self.name}_{name or 'ps'}{self.nbuf}", list(shape), dt))

    def add(self, eng, fn, reads=(), writes=(), dma=False, semkey=None, inc=16):
        assert eng in ENGS
        reads = tuple(reads)
        writes = tuple(writes)
        if dma and semkey is None:
            sb_w = [k for k in writes if not (isinstance(k, tuple) and k and k[0] == "dram")]
            sb_r = [k for k in reads if not (isinstance(k, tuple) and k and k[0] == "dram")]
            semkey = sb_w[0] if sb_w else (sb_r[0] if sb_r else writes[0])
        self.ins.append(dict(eng=eng, fn=fn, reads=reads, writes=writes, dma=dma, semkey=semkey, inc=inc))

    def dma(self, eng, out, in_, reads=(), writes=(), semkey=None, **kw):
        self.add(eng, lambda e: e.dma_start(out=out, in_=in_, **kw), reads, writes, dma=True, semkey=semkey)

    def allgather(self, src_t, dst_t, reads=(), writes=(), semkey=None):
        self.add("pool", lambda e: e.collective_compute("AllGather", ALU.bypass, replica_groups=[[0, 1, 2, 3], [4, 5, 6, 7]],
                                                        ins=[src_t.ap().opt()], outs=[dst_t.ap().opt()]),
                 reads, writes, dma=True, semkey=semkey, inc=1)

    def interleave(self, fns):
        lists = []
        for fn in fns:
            keep, self.ins = self.ins, []
            fn()
            lists.append(self.ins)
            self.ins = keep
        n = max(len(x) for x in lists)
        for i in range(n):
            for x in lists:
                if i < len(x):
                    self.ins.append(x[i])

    def emit(self):
        nc = self.nc
        ins = self.ins
        n = len(ins)
        last_w = {}
        readers = {}
        deps = [None] * n
        for i, I in enumerate(ins):
            d = {}
            for k in I["reads"]:
                j = last_w.get(k)
                if j is not None:
                    d[j] = d.get(j, 0) | 1
            for k in I["writes"]:
                j = last_w.get(k)
                if j is not None:
                    d[j] = d.get(j, 0) | 2
                for r in readers.get(k, ()):
                    if r != i:
                        d[r] = d.get(r, 0) | 4
            for k in I["reads"]:
                readers.setdefault(k, []).append(i)
            for k in I["writes"]:
                last_w[k] = i
                readers[k] = []
            need = []
            for j, ty in d.items():
                J = ins[j]
                if J["dma"]:
                    need.append(j)
                elif J["eng"] == I["eng"]:
                    if I["eng"] == "pe":
                        continue
                    if I["dma"] or (ty & 3):
                        need.append(j)
                else:
                    need.append(j)
            deps[i] = need
        sig = [False] * n
        for i in range(n):
            for j in deps[i]:
                if not ins[j]["dma"]:
                    sig[j] = True
        pool = self.pool
        cnt = dict(pool.eng_cnt)
        val = [0] * n
        dcount = {}
        dslot = {}
        nhw = nsw = 0
        dma_before = [None] * n
        dma_pos = {}
        dma_cum = {}
        for i, I in enumerate(ins):
            if I["dma"]:
                k = I["semkey"]
                if k not in dslot:
                    if I["eng"] == "pool":
                        nsw += 1
                        dslot[k] = pool.NHW + nsw - 1
                        assert dslot[k] < pool.NDMA, "too many software-DMA semaphores in one phase"
                    else:
                        nhw += 1
                        dslot[k] = nhw - 1
                        assert dslot[k] < pool.NHW, "too many hardware-DMA semaphores in one phase"
                    dcount[k] = pool.dma_cnt[dslot[k]]
                dcount[k] = dcount[k] + I["inc"]
                val[i] = dcount[k]
                dma_pos.setdefault(k, []).append(i)
                dma_cum.setdefault(k, []).append(dcount[k])
            elif sig[i]:
                cnt[I["eng"]] += 1
                val[i] = cnt[I["eng"]]
        esem = pool.eng
        dsem = {k: pool.dma[i] for k, i in dslot.items()}
        base_wait = {("e", e): pool.eng_cnt[e] for e in ENGS}
        for k, i in dslot.items():
            base_wait[("d", k)] = pool.dma_cnt[i]
            pool.dma_cnt[i] = dcount[k]
        pool.eng_cnt = dict(cnt)
        self.n_sems = len(dsem)
        import bisect
        streams = {e: [] for e in ENGS}
        for i, I in enumerate(ins):
            waits = {}
            for j in deps[i]:
                J = ins[j]
                if J["dma"]:
                    k = J["semkey"]
                    c = bisect.bisect_left(dma_pos[k], i)
                    key = ("d", k)
                    waits[key] = max(waits.get(key, 0), dma_cum[k][c - 1])
                else:
                    key = ("e", J["eng"])
                    waits[key] = max(waits.get(key, 0), val[j])
            streams[I["eng"]].append((i, waits))
        final_dma = dict(dcount)

        def run_engine(ename, eobj):
            waited = dict(base_wait)
            for i, waits in streams[ename]:
                I = ins[i]
                for key, v in waits.items():
                    if waited.get(key, 0) >= v:
                        continue
                    waited[key] = v
                    s = dsem[key[1]] if key[0] == "d" else esem[key[1]]
                    eobj.wait_ge(s, v)
                r = I["fn"](eobj)
                if I["dma"]:
                    r.then_inc(dsem[I["semkey"]], I["inc"])
                elif sig[i]:
                    r.then_inc(esem[ename], 1)
            if ename == "sp":
                for k, v in final_dma.items():
                    if waited.get(("d", k), 0) < v:
                        eobj.wait_ge(dsem[k], v)

        with nc.Block() as block:
            @block.tensor
            def _(e):
                run_engine("pe", e)

            @block.scalar
            def _(e):
                run_engine("act", e)

            @block.vector
            def _(e):
                run_engine("dve", e)

            @block.gpsimd
            def _(e):
                run_engine("pool", e)

            @block.sync
            def _(e):
                run_engine("sp", e)
        self.stack.close()


NCORES = 8
BATCH, SEQ, DM, DEPTH = 2, 16384, 1024, 2
TOK = 4096
NST = TOK // 512
NH, DN, DR, DV = 8, 64, 32, 64
QL, KVL = 384, 256
NG, EPG, DE = 4, 8, 256
NE = NG * EPG
LN_EPS, RMS_EPS = 1e-5, 1e-6
ALPHA = (2.0 * DEPTH) ** 0.25
QSCALE = (DN + DR) ** -0.5
C_CQ, C_CKV, C_KPE, C_KPS, C_BG, C_CG, C_HC, C_F, WIN_COLS = 0, 384, 640, 672, 704, 960, 1216, 1472, 1728
GROUPS = [[0, 1, 2, 3], [4, 5, 6, 7]]


def mm(P, out, lhsT, rhs, start, stop, reads, writes):
    P.add("pe", lambda e: e.matmul(out, lhsT, rhs, start=start, stop=stop), reads, writes)


def trn(P, out, in_, ident, reads, writes):
    P.add("pe", lambda e: e.transpose(out=out, in_=in_, identity=ident), reads, writes)


def act(P, out, in_, func, reads, writes, eng="act", **kw):
    P.add(eng, lambda e: e.activation(out=out, in_=in_, func=func, **kw), reads, writes)


def cpy(P, eng, out, in_, reads, writes):
    if eng == "act":
        P.add(eng, lambda e: e.activation(out=out, in_=in_, func=AF.Copy), reads, writes)
    else:
        P.add(eng, lambda e: e.tensor_copy(out=out, in_=in_), reads, writes)


def tt(P, eng, out, in0, in1, op, reads, writes):
    P.add(eng, lambda e: e.tensor_tensor(out=out, in0=in0, in1=in1, op=op), reads, writes)


def ts(P, eng, out, in0, s1, s2, op0, op1, reads, writes):
    if s2 is None:
        P.add(eng, lambda e: e.tensor_scalar(out=out, in0=in0, scalar1=s1, scalar2=None, op0=op0), reads, writes)
    else:
        P.add(eng, lambda e: e.tensor_scalar(out=out, in0=in0, scalar1=s1, scalar2=s2, op0=op0, op1=op1), reads, writes)


def stt(P, eng, out, in0, scalar, in1, op0, op1, reads, writes):
    P.add(eng, lambda e: e.scalar_tensor_tensor(out=out, in0=in0, scalar=scalar, in1=in1, op0=op0, op1=op1), reads, writes)


def mset(P, eng, ap, v, writes):
    P.add(eng, lambda e: e.memset(ap, v), (), writes)


class Rot:
    def __init__(self, bufs, name):
        self.bufs = bufs
        self.name = name
        self.i = 0

    def next(self):
        k = self.i % len(self.bufs)
        self.i += 1
        return self.bufs[k], (self.name, k)


def layer_norm_tile(P, x_ap, xkey, out_ap, okey, g_ap, b_ap, gkeys, tmp, eps_ap, tag):
    st, mv, sd, rs, nb, y = tmp["st"], tmp["mv"], tmp["sd"], tmp["rs"], tmp["nb"], tmp["y"]
    k = lambda s: (tag, s)
    P.add("dve", lambda e: e.bn_stats(out=st[:, 0, :], in_=x_ap[:, 0:512]), [xkey], [k("st0")])
    P.add("dve", lambda e: e.bn_stats(out=st[:, 1, :], in_=x_ap[:, 512:1024]), [xkey], [k("st1")])
    P.add("dve", lambda e: e.bn_aggr(out=mv[:], in_=st[:]), [k("st0"), k("st1")], [k("mv")])
    act(P, sd[:], mv[:, 1:2], AF.Sqrt, [k("mv"), "eps"], [k("sd")], bias=eps_ap, scale=1.0)
    P.add("dve", lambda e: e.reciprocal(out=rs[:], in_=sd[:]), [k("sd")], [k("rs")])
    stt(P, "dve", nb[:], mv[:, 0:1], -1.0, rs[:], ALU.mult, ALU.mult, [k("mv"), k("rs")], [k("nb")])
    act(P, y[:], x_ap, AF.Identity, [xkey, k("nb"), k("rs")], [k("y")], bias=nb[:, 0:1], scale=rs[:, 0:1])
    tt(P, "dve", y[:], y[:], g_ap, ALU.mult, [k("y")] + gkeys, [k("y")])
    tt(P, "dve", out_ap, y[:], b_ap, ALU.add, [k("y")] + gkeys, [okey])


def ln_tmp(P):
    return dict(st=P.sb([128, 2, 6], F32), mv=P.sb([128, 2], F32), sd=P.sb([128, 1], F32), rs=P.sb([128, 1], F32),
                nb=P.sb([128, 1], F32), y=P.sb([128, 1024], F32))


def phase_A(nc, l, d):
    P = Phase(nc, f"A{l}", d.get("pool"))
    identf = P.sb([128, 128], F32)
    ident = P.sb([128, 128], BF16)
    ones = P.sb([128, 128], BF16)
    epsr = P.sb([128, 1], F32)
    epsl = P.sb([128, 1], F32)
    win = P.sb([128, 8, WIN_COLS], BF16)
    wuq = P.sb([128, 3, 2 * NH * 96], BF16)
    wk = P.sb([128, 2, 512], BF16)
    wv = P.sb([128, 2, 512], BF16)
    gq = P.sb([128, 3], F32)
    gkv = P.sb([128, 2], F32)
    stg = [P.sb([128, WIN_COLS], F32) for _ in range(1)]
    lng = P.sb([128, 1024], F32)
    lnb = P.sb([128, 1024], F32)
    vbufs = [P.sb([128, NH, 4, 65], BF16) for _ in range(2)]

    mset(P, "pool", ones[:], 1.0, ["ones"])
    mset(P, "pool", epsr[:], RMS_EPS, ["eps"])
    mset(P, "pool", epsl[:], LN_EPS, ["eps"])
    mset(P, "pool", vbufs[0][:, :, :, 64:65], 1.0, [("vones", 0)])
    mset(P, "pool", vbufs[1][:, :, :, 64:65], 1.0, [("vones", 1)])
    P.dma("sp", identf[:], d["ident"][:, :], writes=["identf"])
    cpy(P, "dve", ident[:], identf[:], ["identf"], ["ident"])
    P.dma("sp", gq[:], d["g_q"][l, :, :], writes=["gq"])
    P.dma("sp", gkv[:], d["g_kv"][l, :, :], writes=["gkv"])
    if l == 0:
        P.dma("sp", lng[:], d["lnp"][0, :, :], writes=["lng"])
        P.dma("sp", lnb[:], d["lnp"][1, :, :], writes=["lnb"])
    si = 0
    for kc in range(8):
        s, sk = stg[0], ("stg", 0)
        si += 1
        P.dma("sp", s[:, :], d["w_in"][l, kc * 128:(kc + 1) * 128, :], writes=[sk])
        cpy(P, "dve" if kc % 2 == 0 else "act", win[:, kc, :], s[:, :], [sk], [("win", kc)])
    for c in range(3):
        s, sk = stg[0], ("stg", 0)
        si += 1
        P.dma("sp", s[:, 0:1536], d["w_uq"][l, c * 128:(c + 1) * 128, :], writes=[sk])
        ts(P, "dve", wuq[:, c, :], s[:, 0:1536], gq[:, c:c + 1], None, ALU.mult, None, [sk, "gq"], [("wuq", c)])
    for c in range(2):
        s, sk = stg[0], ("stg", 0)
        si += 1
        P.dma("sp", s[:, 0:512], d["w_ukv_k"][l, c * 128:(c + 1) * 128, :], writes=[sk])
        P.dma("sp", s[:, 512:1024], d["w_ukv_v"][l, c * 128:(c + 1) * 128, :], writes=[sk])
        ts(P, "dve", wk[:, c, :], s[:, 0:512], gkv[:, c:c + 1], None, ALU.mult, None, [sk, "gkv"], [("wk", c)])
        ts(P, "dve", wv[:, c, :], s[:, 512:1024], gkv[:, c:c + 1], None, ALU.mult, None, [sk, "gkv"], [("wv", c)])
    wkeys = [("win", kc) for kc in range(8)]
    if l == 0:
        zero_slots(P, d)

    xts = [P.sb([128, 4, 1024], F32)] * 2
    hbs = [P.sb([128, 4, 1024], BF16)] * 2
    hTs = [P.sb([128, 8, 512], BF16) for _ in range(2)]
    ropes = [P.sb([128, 2, 512], F32) for _ in range(2)]
    cqTs = [P.sb([128, 3, 512], BF16)] * 2
    sqs = [P.sb([128, 3, 512], BF16)] * 2
    cqns = [P.sb([128, 3, 512], BF16) for _ in range(2)]
    ckvTs = [P.sb([128, 2, 512], BF16)] * 2
    sqks = [P.sb([128, 2, 512], BF16)] * 2
    ckvns = [P.sb([128, 2, 512], BF16) for _ in range(2)]
    sdt = [P.sb([128, 512], F32)] * 2
    rq = [P.sb([128, 512], F32)] * 2
    rk = [P.sb([128, 512], F32)] * 2
    kt1 = [P.sb([32, 512], F32)] * 2
    kt2 = [P.sb([32, 512], F32)] * 2
    krs = [P.sb([32, 512], BF16) for _ in range(2)]
    bgs = [P.sb([128, 2, 512], F32)] * 2
    cgs = [P.sb([128, 2, 512], F32)] * 2
    ups = [P.sb([128, 2, 512], F32)] * 2
    qTs = [P.sb([96, NH, 512], BF16) for _ in range(2)]
    qt1 = Rot([P.sb([96, 512], F32) for _ in range(2)], "qt1")
    qt2 = Rot([P.sb([96, 512], F32) for _ in range(2)], "qt2")
    kTs = [P.sb([128, 4, 512], BF16) for _ in range(2)]
    fs = [P.sb([128, 2, 512], BF16) for _ in range(2)]
    lt = [ln_tmp(P)] * 2 if l == 0 else None
    trp = Rot([P.ps([128, 512], BF16) for _ in range(2)], "trp")
    pb = Rot([P.ps([128, 512], F32) for _ in range(6)], "pb")

    for st in range(NST):
        sl = st % 2
        t0 = st * 512
        xt, hb, hT = xts[sl], hbs[sl], hTs[sl]
        K = lambda s: (s, sl if s in ("rope", "kr") else 0)
        if l == 0:
            P.dma("sp", xt[:, :, :], d["x"][t0:t0 + 512, :].rearrange("(j p) f -> p j f", p=128),
                  writes=[K("xraw")] + [("xt", 0, j) for j in range(4)])
            for j in range(4):
                layer_norm_tile(P, xt[:, j, :], K("xraw"), xt[:, j, :], ("xt", 0, j), lng[:], lnb[:], ["lng", "lnb"], lt[j % 2],
                                epsl[:, 0:1], "lnA")
            xkeys = [("xt", 0, j) for j in range(4)]
            P.dma("pool", d["hbuf"][t0:t0 + 512, :].rearrange("(j p) f -> p j f", p=128), xt[:, :, :], reads=xkeys,
                  semkey=("hst", 0))
        else:
            P.dma("sp", xt[:, :, :], d["hbuf"][t0:t0 + 512, :].rearrange("(j p) f -> p j f", p=128), writes=[K("xraw")])
            xkeys = [K("xraw")]
        P.dma("sp", ropes[sl][:, :, :], d["rope"][:, :, t0:t0 + 512], writes=[K("rope")])
        for j in range(4):
            cpy(P, "act" if j % 2 == 0 else "dve", hb[:, j, :], xt[:, j, :], xkeys if l else [("xt", 0, j)], [("hb", 0, j)])
        for kc in range(8):
            tp, tk = trp.next()
            for j in range(4):
                trn(P, tp[:, j * 128:(j + 1) * 128], hb[:, j, kc * 128:(kc + 1) * 128], ident[:], [("hb", 0, j), "ident"], [tk])
            cpy(P, "act" if kc % 2 == 0 else "dve", hT[:, kc, :], tp[:, :], [tk], [("hT", sl, kc)])
        hkeys = [("hT", sl, kc) for kc in range(8)]

        def zproj(col0, M):
            p, pk = pb.next()
            for kc in range(8):
                mm(P, p[0:M, :], win[:, kc, col0:col0 + M], hT[:, kc, :], kc == 0, kc == 7, [("win", kc), ("hT", sl, kc)], [pk])
            return p, pk

        def cq_chunk(c):
            p, pk = zproj(C_CQ + c * 128, 128)
            cpy(P, "act", cqTs[sl][:, c, :], p[:, :], [pk], [("cqT", 0, c)])
            act(P, sqs[sl][:, c, :], p[:, :], AF.Square, [pk], [("sq", 0, c)])

        P.interleave([lambda c=c: cq_chunk(c) for c in range(3)])
        p, pk = pb.next()
        for c in range(3):
            mm(P, p[:, :], ones[:, :], sqs[sl][:, c, :], c == 0, c == 2, ["ones", ("sq", 0, c)], [pk])
        act(P, sdt[sl][:, :], p[:, :], AF.Sqrt, [pk, "eps"], [K("sdt")], bias=epsr[:, 0:1], scale=1.0 / QL)
        P.add("dve", lambda e, o=rq[sl], i=sdt[sl]: e.reciprocal(out=o[:, :], in_=i[:, :]), [K("sdt")], [K("rq")])
        for c in range(3):
            tt(P, "dve", cqns[sl][:, c, :], cqTs[sl][:, c, :], rq[sl][:, :], ALU.mult,
               [("cqT", 0, c), K("rq")], [("cqn", sl, c)])
        def ckv_chunk(c):
            p, pk = zproj(C_CKV + c * 128, 128)
            cpy(P, "act", ckvTs[sl][:, c, :], p[:, :], [pk], [("ckvT", 0, c)])
            act(P, sqks[sl][:, c, :], p[:, :], AF.Square, [pk], [("sqk", 0, c)])

        P.interleave([lambda c=c: ckv_chunk(c) for c in range(2)])
        p, pk = pb.next()
        for c in range(2):
            mm(P, p[:, :], ones[:, :], sqks[sl][:, c, :], c == 0, c == 1, ["ones", ("sqk", 0, c)], [pk])
        act(P, sdt[sl][:, :], p[:, :], AF.Sqrt, [pk, "eps"], [K("sdt")], bias=epsr[:, 0:1], scale=1.0 / KVL)
        P.add("dve", lambda e, o=rk[sl], i=sdt[sl]: e.reciprocal(out=o[:, :], in_=i[:, :]), [K("sdt")], [K("rk")])
        for c in range(2):
            tt(P, "dve", ckvns[sl][:, c, :], ckvTs[sl][:, c, :], rk[sl][:, :], ALU.mult,
               [("ckvT", 0, c), K("rk")], [("ckvn", sl, c)])
        kp = {}
        P.interleave([lambda: kp.__setitem__(0, zproj(C_KPE, 32)), lambda: kp.__setitem__(1, zproj(C_KPS, 32))])
        (p1, pk1), (p2, pk2) = kp[0], kp[1]
        tt(P, "dve", kt1[sl][:, :], p1[0:32, :], ropes[sl][0:32, 0, :], ALU.mult, [pk1, K("rope")], [K("kt1")])
        tt(P, "dve", kt2[sl][:, :], p2[0:32, :], ropes[sl][0:32, 1, :], ALU.mult, [pk2, K("rope")], [K("kt2")])
        tt(P, "dve", krs[sl][:, :], kt1[sl][:, :], kt2[sl][:, :], ALU.add, [K("kt1"), K("kt2")], [K("kr")])
        P.dma("pool", d["payKr"][0:32, t0:t0 + 512], krs[sl][:, :], reads=[K("kr")])
        def bg_chunk(c):
            p, pk = zproj(C_BG + c * 128, 128)
            cpy(P, "act", bgs[sl][:, c, :], p[:, :], [pk], [("bg", 0, c)])

        P.interleave([lambda c=c: bg_chunk(c) for c in range(2)])
        P.dma("pool", d["bgT"][:, t0:t0 + 512].rearrange("(c p) t -> p c t", p=128), bgs[sl][:, :, :],
              reads=[("bg", 0, 0), ("bg", 0, 1)], semkey=("bgst", 0))
        def cg_chunk(c):
            p, pk = zproj(C_CG + c * 128, 128)
            cpy(P, "act", cgs[sl][:, c, :], p[:, :], [pk], [("cg", 0, c)])

        def hc_chunk(c):
            p, pk = zproj(C_HC + c * 128, 128)
            tt(P, "dve", ups[sl][:, c, :], p[:, :], cgs[sl][:, c, :], ALU.mult, [pk, ("cg", 0, c)], [("up", 0, c)])

        P.interleave([lambda c=c: cg_chunk(c) for c in range(2)])
        P.interleave([lambda c=c: hc_chunk(c) for c in range(2)])
        P.dma("pool", d["upT"][:, t0:t0 + 512].rearrange("(c p) t -> p c t", p=128), ups[sl][:, :, :],
              reads=[("up", 0, 0), ("up", 0, 1)], semkey=("upst", 0))
        if st == 0:
            P.dma("pool", d["payH"][:, 0:1].rearrange("(c p) x -> p c x", p=128), ups[sl][:, :, 0:1],
                  reads=[("up", 0, 0), ("up", 0, 1)], semkey=("upst", 0), allow_slow_non_contiguous=True)
        if st == NST - 1:
            P.dma("pool", d["payH"][:, 1:2].rearrange("(c p) x -> p c x", p=128), ups[sl][:, :, 511:512],
                  reads=[("up", 0, 0), ("up", 0, 1)], semkey=("upst", 0), allow_slow_non_contiguous=True)
        def f_chunk(q):
            p, pk = zproj(C_F + q * 128, 128)
            cpy(P, "act" if q % 2 else "dve", fs[sl][:, q, :], p[:, :], [pk], [("f", sl, q)])
            P.dma("pool", d["payF"][q][st * 512:(st + 1) * 512, :].rearrange("(j c) b -> c j b", c=128),
                  fs[sl][:, q, :].rearrange("c (j b) -> c j b", b=128), reads=[("f", sl, q)], semkey=("fst", sl, q))

        P.interleave([lambda q=q: f_chunk(q) for q in range(2)])
        def q_head(h):
            pa, pak = pb.next()
            pbb, pbk = pb.next()
            for c in range(3):
                mm(P, pa[0:96, :], wuq[:, c, h * 96:(h + 1) * 96], cqns[sl][:, c, :], c == 0, c == 2,
                   [("wuq", c), ("cqn", sl, c)], [pak])
                mm(P, pbb[0:96, :], wuq[:, c, 768 + h * 96:768 + (h + 1) * 96], cqns[sl][:, c, :], c == 0, c == 2,
                   [("wuq", c), ("cqn", sl, c)], [pbk])
            act(P, qTs[sl][0:64, h, :], pa[0:64, :], AF.Copy, [pak], [("qTn", sl, h)], scale=QSCALE)
            t1, t1k = qt1.next()
            t2, t2k = qt2.next()
            tt(P, "dve", t1[64:96, :], pa[64:96, :], ropes[sl][64:96, 0, :], ALU.mult, [pak, K("rope")], [t1k])
            tt(P, "dve", t2[64:96, :], pbb[64:96, :], ropes[sl][64:96, 1, :], ALU.mult, [pbk, K("rope")], [t2k])
            tt(P, "dve", qTs[sl][64:96, h, :], t1[64:96, :], t2[64:96, :], ALU.add, [t1k, t2k], [("qTr", sl, h)])

        for h in range(NH):
            q_head(h)
        P.dma("pool", d["qT"][:, :, t0:t0 + 512].rearrange("h r t -> r h t"), qTs[sl][:, :, :],
              reads=[("qTn", sl, h) for h in range(NH)] + [("qTr", sl, h) for h in range(NH)], semkey=("qst", sl))
        def k_chunk(k2):
            p, pk = pb.next()
            for c in range(2):
                mm(P, p[:, :], wk[:, c, k2 * 128:(k2 + 1) * 128], ckvns[sl][:, c, :], c == 0, c == 1,
                   [("wk", c), ("ckvn", sl, c)], [pk])
            cpy(P, "act" if k2 % 2 else "dve", kTs[sl][:, k2, :], p[:, :], [pk], [("kT", sl, k2)])

        P.interleave([lambda k2=k2: k_chunk(k2) for k2 in range(4)])
        for k2 in range(4):
            P.dma("pool", d["payK"][k2][:, t0:t0 + 512], kTs[sl][:, k2, :], reads=[("kT", sl, k2)], semkey=("kst", sl))
        def v_chunk(j):
            p, pk = pb.next()
            for c in range(2):
                mm(P, p[:, :], ckvns[sl][:, c, j * 128:(j + 1) * 128], wv[:, c, :], c == 0, c == 1,
                   [("wv", c), ("ckvn", sl, c)], [pk])
            cpy(P, "act" if j % 2 else "dve", vbufs[sl][:, :, j, 0:64], p[:, :].rearrange("p (h c) -> p h c", h=NH), [pk],
                [("vb", sl, j)])

        P.interleave([lambda j=j: v_chunk(j) for j in range(4)])
        for h in range(NH):
            P.dma("pool", d["payV"][h].rearrange("p (t c) -> p t c", c=65)[:, st * 4:st * 4 + 4, :],
                  vbufs[sl][:, h, :, :], reads=[("vones", sl)] + [("vb", sl, j) for j in range(4)], semkey=("vst", sl))
    P.emit()


KB = 2


def phase_B(nc, l, d):
    P = Phase(nc, f"B{l}", d.get("pool"))
    onesf = P.sb([128, 64], F32)
    mset(P, "pool", onesf[:], 1.0, ["onesf"])
    KTs = [P.sb([96, SEQ], BF16) for _ in range(2)]
    Vs = [P.sb([128, 4, 32, 65], BF16) for _ in range(2)]
    QTs = [P.sb([96, TOK], BF16) for _ in range(2)]
    sps = Rot([P.ps([128, KB, 512], F32) for _ in range(3)], "sp")
    pos = Rot([P.ps([128, 512], F32) for _ in range(1)], "po")
    bcs = Rot([P.ps([64, 512], F32) for _ in range(1)], "bc")
    pts = Rot([P.sb([128, KB, 512], BF16) for _ in range(4)], "pt")
    rsum = Rot([P.sb([128, 512], F32) for _ in range(2)], "rsum")
    rrec = Rot([P.sb([128, 512], F32) for _ in range(2)], "rrec")
    bcsb = Rot([P.sb([64, 512], F32) for _ in range(2)], "bcsb")
    oTs = Rot([P.sb([64, 512], BF16) for _ in range(2)], "oT")

    order = [("Kr", None), ("K", 0), ("V", 0), ("V", 1)]
    for k2 in range(1, 4):
        order += [("K", k2), ("V", 2 * k2), ("V", 2 * k2 + 1)]
    order += [("F", 0), ("F", 1), ("H", None)]
    prev = None
    for kind, i in order:
        name = {"Kr": "Kr", "K": "K", "V": "V", "F": "F", "H": "H"}[kind]
        src = d["pay" + name + "_t"] if i is None else d["pay" + name + "_t"][i]
        dst = d["g" + name + "_t"] if i is None else d["g" + name + "_t"][i]
        P.allgather(src, dst, reads=[prev] if prev else [], writes=[("dram", "g" + name, i), ("ccchain", kind, i)],
                    semkey=("cc", kind, i))
        prev = ("ccchain", kind, i)

    def load_head(h):
        s = h % 2
        for r in range(4):
            P.dma("sp", KTs[s][0:64, r * TOK:(r + 1) * TOK],
                  d["gK"][h // 2][r * 128 + (h % 2) * 64:r * 128 + (h % 2) * 64 + 64, :],
                  reads=[("dram", "gK", h // 2)], writes=[("KTn", s, r)])
            P.dma("sp", KTs[s][64:96, r * TOK:(r + 1) * TOK], d["gKr"][r * 32:(r + 1) * 32, :],
                  reads=[("dram", "gKr", None)], writes=[("KTr", s, r)])
            P.dma("sp", Vs[s][:, r, :, :].rearrange("p t c -> p (t c)"), d["gV"][h][r * 128:(r + 1) * 128, :],
                  reads=[("dram", "gV", h)], writes=[("V", s, r)])
        P.dma("sp", QTs[s][:, :], d["qT"][h, :, :], writes=[("QT", s)])

    wstg = Rot([P.sb([128, 2048], F32) for _ in range(2)], "wstg")
    wrow = Rot([P.sb([128, 6144], BF16) for _ in range(2)], "wrow")

    def precast(ex):
        row, rowk = wrow.next()
        for j, (nm, f) in enumerate((("w_gate", DE), ("w_up", DE), ("w_down", DM))):
            st_, stk = wstg.next()
            P.dma("sp", st_[:, :].rearrange("p (k f) -> p k f", f=f), d[nm][l, ex].rearrange("(k p) f -> p k f", p=128),
                  writes=[stk])
            cpy(P, "pool", row[:, j * 2048:(j + 1) * 2048], st_[:, :], [stk], [(rowk, j)])
        P.dma("sp", d["wbf"][ex * 128:(ex + 1) * 128, :], row[:, :], reads=[(rowk, j) for j in range(3)], semkey=("wrowst", rowk))

    load_head(0)
    nkb = SEQ // 128 // KB
    its = [(h, qt, kb) for h in range(NH) for qt in range(TOK // 512) for kb in range(nkb)]
    state = {}

    def emit_S(it):
        h, qt, kb = it
        s = h % 2
        if qt == 0 and kb == 0 and h + 1 < NH:
            load_head(h + 1)
        if kb == 8 and (h * 8 + qt) % 2 == 0:
            precast((h * 8 + qt) // 2)
        ps, psk = sps.next()
        for u in range(KB):
            kt = kb * KB + u
            r = kt // 32
            mm(P, ps[:, u, :], KTs[s][0:96, kt * 128:(kt + 1) * 128], QTs[s][0:96, qt * 512:(qt + 1) * 512], True, True,
               [("KTn", s, r), ("KTr", s, r), ("QT", s)], [psk])
        state[it] = (ps, psk)

    def emit_rest(it):
        h, qt, kb = it
        s = h % 2
        V = Vs[s]
        ps, psk = state.pop(it)
        if kb == 0:
            state["po"] = pos.next()
        po, pok = state["po"]
        pt, ptk = pts.next()
        act(P, pt[:, :, :], ps[:, :, :], AF.Exp, [psk], [ptk])
        for u in range(KB):
            kt = kb * KB + u
            r, t = kt // 32, kt % 32
            mm(P, po[0:65, :], V[:, r, t, :], pt[:, u, :], kt == 0, kt == SEQ // 128 - 1, [("V", s, r), ptk], [pok])
        if kb == nkb - 1:
            rs_, rsk = rsum.next()
            rr_, rrk = rrec.next()
            bc, bck = bcs.next()
            bs_, bsk = bcsb.next()
            oT, oTk = oTs.next()
            cpy(P, "dve", rs_[64:65, :], po[64:65, :], [pok], [rsk])
            P.add("dve", lambda e, o=rr_, i=rs_: e.reciprocal(out=o[64:65, :], in_=i[64:65, :]), [rsk], [rrk])
            mm(P, bc[0:64, :], onesf[64:65, 0:64], rr_[64:65, :], True, True, ["onesf", rrk], [bck])
            cpy(P, "dve", bs_[:, :], bc[:, :], [bck], [bsk])
            tt(P, "dve", oT[:, :], po[0:64, :], bs_[:, :], ALU.mult, [pok, bsk], [oTk])
            P.dma("sp", d["omixT"][h * 64:(h + 1) * 64, qt * 512:(qt + 1) * 512], oT[:, :], reads=[oTk])

    emit_S(its[0])
    emit_S(its[1])
    for i, it in enumerate(its):
        if i + 2 < len(its):
            emit_S(its[i + 2])
        emit_rest(it)
    P.emit()


def phase_C1(nc, l, d):
    P = Phase(nc, f"C{l}", d.get("pool"))
    wc = P.sb([128, 2, 3], F32)
    sel = P.sb([128, 2, 4], F32)
    hal = P.sb([128, 2, 4, 16], F32)
    tmp = P.sb([128, 2, 4], F32)
    P.dma("sp", wc[:], d["w_conv"][l, :, :, :], writes=["wc"])
    P.dma("sp", sel[:], d["halo_sel"][:, :, :], writes=["sel"])
    for r in range(4):
        P.dma("sp", hal[:, :, r, :], d["gH"][r * 256:(r + 1) * 256, :].rearrange("(c p) x -> p c x", p=128), writes=[("hal", r)])
    halk = [("hal", r) for r in range(4)]
    upx = [P.sb([128, TOK + 2], F32) for _ in range(2)]
    CB = 1024
    bgb = Rot([P.sb([128, CB], F32) for _ in range(2)], "bgb")
    acc = Rot([P.sb([128, CB], F32) for _ in range(2)], "acc")
    ob = Rot([P.sb([128, CB], BF16) for _ in range(2)], "ob")
    for c in range(2):
        u = upx[c]
        P.dma("sp", u[:, 1:TOK + 1], d["upT"][c * 128:(c + 1) * 128, :], writes=[("upx", c)])
        tt(P, "dve", tmp[:, 0, :], hal[:, c, :, 1], sel[:, 0, :], ALU.mult, halk + ["sel"], [("tmpL", c)])
        P.add("dve", lambda e, o=u, i=tmp: e.reduce_sum(out=o[:, 0:1], in_=i[:, 0, :], axis=AX.X), [("tmpL", c)], [("upxL", c)])
        tt(P, "dve", tmp[:, 1, :], hal[:, c, :, 0], sel[:, 1, :], ALU.mult, halk + ["sel"], [("tmpR", c)])
        P.add("dve", lambda e, o=u, i=tmp: e.reduce_sum(out=o[:, TOK + 1:TOK + 2], in_=i[:, 1, :], axis=AX.X), [("tmpR", c)],
              [("upxR", c)])
        ukeys = [("upx", c), ("upxL", c), ("upxR", c)]
        for blk in range(TOK // CB):
            c0 = blk * CB
            b_, bk = bgb.next()
            a_, ak = acc.next()
            o_, ok = ob.next()
            P.dma("sp", b_[:, :], d["bgT"][c * 128:(c + 1) * 128, c0:c0 + CB], writes=[bk])
            eng = "dve"
            ts(P, eng, a_[:, :], u[:, c0:c0 + CB], wc[:, c, 0:1], None, ALU.mult, None, ukeys + ["wc"], [ak])
            stt(P, eng, a_[:, :], u[:, c0 + 1:c0 + 1 + CB], wc[:, c, 1:2], a_[:, :], ALU.mult, ALU.add, ukeys + ["wc", ak], [ak])
            stt(P, eng, a_[:, :], u[:, c0 + 2:c0 + 2 + CB], wc[:, c, 2:3], a_[:, :], ALU.mult, ALU.add, ukeys + ["wc", ak], [ak])
            tt(P, eng, o_[:, :], a_[:, :], b_[:, :], ALU.mult, [ak, bk], [ok])
            P.dma("act", d["omixT"][512 + c * 128:512 + (c + 1) * 128, c0:c0 + CB], o_[:, :], reads=[ok])
    P.emit()


def phase_C2(nc, l, d):
    P = Phase(nc, f"F{l}", d.get("pool"))
    stg = P.sb([128, 128 * 96 // 4], F32)
    T1 = P.sb([128, 256], BF16)
    T2 = P.sb([128, 128, 96], BF16)
    BD = P.sb([128, 2, 128], BF16)
    P.dma("sp", stg[:, 0:256], d["dft1"][:, :], writes=["stg"])
    cpy(P, "dve", T1[:, :], stg[:, 0:256], ["stg"], ["T1"])
    P.dma("sp", stg[:, 0:256], d["dftc"][:, :], writes=["stg"])
    cpy(P, "dve", BD[:, :, :].rearrange("p a b -> p (a b)"), stg[:, 0:256], ["stg"], ["BD"])
    for qq in range(4):
        P.dma("sp", stg[:, :], d["dft2"][:, qq * 32:(qq + 1) * 32, :].rearrange("p a b -> p (a b)"), writes=["stg"])
        cpy(P, "dve" if qq % 2 else "pool", T2[:, qq * 32:(qq + 1) * 32, :].rearrange("p a b -> p (a b)"), stg[:, :], ["stg"],
            [("T2", qq)])
    t2keys = [("T2", qq) for qq in range(4)]
    Fq = P.sb([128, 128, 128], BF16)
    Y1 = P.sb([128, 128, 256], BF16)
    X = P.sb([128, 128, 64], BF16)
    oF = P.sb([128, TOK], BF16)
    ps1 = Rot([P.ps([128, 2, 256], F32) for _ in range(3)], "ps1")
    ps2 = Rot([P.ps([128, 8, 64], F32) for _ in range(3)], "ps2")
    ps3 = Rot([P.ps([128, 512], F32) for _ in range(2)], "ps3")
    for q in range(2):
        src = d["gF"][q].rearrange("(a c) b -> a c b", c=128)
        for part in range(4):
            P.dma("sp", Fq[:, part * 32:(part + 1) * 32, :], src[:, part * 32:(part + 1) * 32, :], reads=[("dram", "gF", q)],
                  writes=[("Fq", part)])
        fkeys = [("Fq", part) for part in range(4)]
        for cp in range(64):
            p, pk = ps1.next()
            for u in range(2):
                c = cp * 2 + u
                mm(P, p[:, u, :], Fq[:, c, :], T1[:, :], True, True, fkeys + ["T1"], [pk])
            cpy(P, "act" if cp % 2 else "dve", Y1[:, cp * 2:cp * 2 + 2, :], p[:, :, :], [pk], [("Y1", cp)])
        y1keys = [("Y1", i) for i in range(64)]
        if "dbgY1" in d and q == 0:
            P.dma("sp", d["dbgY1"][:, :], Y1[:, :, :].rearrange("p a b -> p (a b)"), reads=y1keys, semkey="dbgY1")
            P.dma("sp", d["dbgF"][:, :], Fq[:, :, :].rearrange("p a b -> p (a b)"), reads=fkeys, semkey="dbgF")
        for kg in range(16):
            p, pk = ps2.next()
            for u in range(8):
                k1 = kg * 8 + u
                mm(P, p[:, u, :], Y1[:, :, k1], T2[:, k1, 32:96], True, False, y1keys + t2keys, [pk])
                mm(P, p[:, u, :], Y1[:, :, 128 + k1], T2[:, k1, 0:64], False, True, y1keys + t2keys, [pk])
            cpy(P, "act" if kg % 2 else "dve", X[:, kg * 8:(kg + 1) * 8, :], p[:, :, :], [pk], [("X", kg)])
        if "dbgX" in d and q == 0:
            P.dma("sp", d["dbgX"][:, :], X[:, :, :].rearrange("p a b -> p (a b)"), reads=[("X", i) for i in range(16)], semkey="dbgX")
        for m in range(8):
            p, pk = ps3.next()
            mm(P, p[:, :].rearrange("p (a b) -> p a b", b=32), BD[:, 0, :], X[:, m * 16:(m + 1) * 16, 0:32], True, False,
               [("X", 2 * m), ("X", 2 * m + 1), "BD"], [pk])
            mm(P, p[:, :].rearrange("p (a b) -> p a b", b=32), BD[:, 1, :], X[:, m * 16:(m + 1) * 16, 32:64], False, True,
               [("X", 2 * m), ("X", 2 * m + 1), "BD"], [pk])
            cpy(P, "act" if m % 2 else "dve",
                oF[:, :].rearrange("p (k2 k1) -> p k1 k2", k1=128)[:, m * 16:(m + 1) * 16, :],
                p[:, :].rearrange("p (a b) -> p a b", b=32), [pk], [("oF", m)])
        P.dma("sp", d["omixT"][768 + q * 128:768 + (q + 1) * 128, :], oF[:, :], reads=[("oF", m) for m in range(8)])
    P.emit()


WPAD = 256
NSLOT_T = (2 * TOK + NE * (WPAD - 1)) // 128 + 1
NSLOT = NSLOT_T * 128
BLK = 1024


def zero_slots(P, d):
    z = P.sb([128, DM], BF16)
    mset(P, "pool", z[:, :], 0.0, ["z"])
    for i in range(NSLOT_T):
        P.dma("pool", d["xs"][i * 128:(i + 1) * 128, :], z[:, :], reads=["z"], semkey="zst")


def phase_D(nc, l, d, last):
    phase_Da(nc, l, d)
    phase_Db(nc, l, d, last)


def phase_Da(nc, l, d):
    P = Phase(nc, f"D{l}", d.get("pool"))
    NT = TOK // 128
    NTB = BLK // 128
    identf = P.sb([128, 128], F32)
    ident = P.sb([128, 128], BF16)
    epsl = P.sb([128, 1], F32)
    wout = P.sb([128, 8, DM], BF16)
    wrt = P.sb([128, 8, 36], BF16)
    brt = P.sb([128, 36], F32)
    lnp = P.sb([128, 2, DM], F32)
    sortc = P.sb([128, 128 + NSLOT_T + 1], F32)
    ltri = P.sb([128, 128], BF16)
    ones = P.sb([128, 128], BF16)
    stg = Rot([P.sb([128, 2048], F32) for _ in range(1)], "stg")
    mset(P, "pool", epsl[:], LN_EPS, ["eps"])
    mset(P, "pool", ones[:], 1.0, ["ones"])
    P.dma("sp", identf[:], d["ident"][:, :], writes=["identf"])
    cpy(P, "dve", ident[:], identf[:], ["identf"], ["ident"])
    P.dma("sp", sortc[:], d["sortc"][:, :], writes=["sortc"])
    cpy(P, "dve", ltri[:], sortc[:, 0:128], ["sortc"], ["ltri"])
    tstart = sortc[:, 128:128 + NSLOT_T]
    pidx = sortc[:, 128 + NSLOT_T:128 + NSLOT_T + 1]
    P.dma("sp", brt[:], d["b_rt"][l, :, :], writes=["brt"])
    for i in range(2):
        P.dma("sp", lnp[:, i, :], d["lnp"][2 + 4 * l + i, :, :], writes=[("lnp", i)])
    for kc in range(8):
        s, sk = stg.next()
        P.dma("sp", s[:, 0:1024], d["w_out"][l, kc * 128:(kc + 1) * 128, :], writes=[sk])
        P.dma("sp", s[:, 1024:1060], d["w_rt"][l, kc * 128:(kc + 1) * 128, :], writes=[sk])
        cpy(P, "dve" if kc % 2 else "pool", wout[:, kc, :], s[:, 0:1024], [sk], [("wout", kc)])
        cpy(P, "dve", wrt[:, kc, :], s[:, 1024:1060], [sk], [("wrt", kc)])

    om = P.sb([128, 8, BLK], BF16)
    hts = Rot([P.sb([128, DM], F32) for _ in range(4)], "ht")
    res = Rot([P.sb([128, DM], F32) for _ in range(4)], "res")
    h1s = Rot([P.sb([128, DM], F32) for _ in range(3)], "h1")
    h1b = Rot([P.sb([128, DM], BF16) for _ in range(6)], "h1b")
    h1Ts = Rot([P.sb([128, 8, 128], BF16) for _ in range(2)], "h1T")
    lts = [ln_tmp(P), ln_tmp(P)]
    pb = Rot([P.ps([128, 512], F32) for _ in range(5)], "pb")
    lgps = Rot([P.ps([128, 512], F32) for _ in range(1)], "lgp")
    trp = Rot([P.ps([128, 1024], BF16) for _ in range(2)], "trp")
    M1 = P.sb([128, NT, NE], F32)
    M2 = P.sb([128, NT, NE], F32)
    W1, W2, posi, idxw = d["sbW1"], d["sbW2"], d["sbposi"], d["sbidxw"]
    R = {k: P.sb(shape, F32) for k, shape in dict(
        lg=[128, NTB, 36], m4=[128, NTB], d4=[128, NTB, 4], e4=[128, NTB, 4], s4=[128, NTB], pg=[128, NTB],
        oh=[128, NTB, 4], t48=[128, NTB, 4, 8], el=[128, NTB, 8], m1=[128, NTB], k1=[128, NTB, 8], el2=[128, NTB, 8],
        m2=[128, NTB], k2=[128, NTB, 8], dd=[128, NTB], ee=[128, NTB], p1=[128, NTB], p2=[128, NTB]).items()}

    def rk(n):
        return ("R", n)

    g1, b1 = lnp[:, 0, :], lnp[:, 1, :]
    bc3 = lambda ap, n: ap.unsqueeze(2).broadcast_to([128, NTB, n])
    for nb in range(TOK // BLK):
        tb = nb * BLK
        ts_ = slice(nb * NTB, (nb + 1) * NTB)
        for mc in range(8):
            P.dma("sp", om[:, mc, :], d["omixT"][mc * 128:(mc + 1) * 128, tb:tb + BLK], writes=[("om", mc)])
        lgp, lgk = lgps.next()
        st1 = {}

        def d1_a(t):
            ht, hk = hts.next()
            P.dma("sp", ht[:, :], d["hbuf"][tb + t * 128:tb + (t + 1) * 128, :], writes=[hk])
            r_, rk_ = res.next()
            def mix_half(half):
                p, pk = pb.next()
                for mc in range(8):
                    mm(P, p[:, :], om[:, mc, t * 128:(t + 1) * 128], wout[:, mc, half * 512:(half + 1) * 512], mc == 0, mc == 7,
                       [("om", mc), ("wout", mc)], [pk])
                stt(P, "dve", r_[:, half * 512:(half + 1) * 512], ht[:, half * 512:(half + 1) * 512], ALPHA, p[:, :],
                    ALU.mult, ALU.add, [hk, pk], [rk_])

            P.interleave([lambda: mix_half(0), lambda: mix_half(1)])
            st1[t] = (r_, rk_)

        def d1_b(t):
            r_, rk_ = st1.pop(t)
            h1, h1k = h1s.next()
            layer_norm_tile(P, r_[:, :], rk_, h1[:, :], h1k, g1, b1, [("lnp", 0), ("lnp", 1)], lts[t % 2], epsl[:, 0:1], ("lnD", t % 2))
            P.dma("act", d["h1buf"][tb + t * 128:tb + (t + 1) * 128, :], h1[:, :], reads=[h1k], writes=[("dram", "h1")],
                  semkey="h1st")
            hb, hbk = h1b.next()
            cpy(P, "act", hb[:, :], h1[:, :], [h1k], [hbk])
            P.dma("act", d["h1b"][tb + t * 128:tb + (t + 1) * 128, :], hb[:, :], reads=[hbk], writes=[("dram", "h1b")],
                  semkey="h1bst")
            tp, tk = trp.next()
            for kc in range(8):
                trn(P, tp[:, kc * 128:(kc + 1) * 128], hb[:, kc * 128:(kc + 1) * 128], ident[:], [hbk, "ident"], [tk])
            hT, hTk = h1Ts.next()
            cpy(P, "dve", hT[:, :, :], tp[:, :].rearrange("p (k t) -> p k t", k=8), [tk], [hTk])
            st1[("hT", t)] = (hT, hTk)

        def d1_c(t):
            hT, hTk = st1.pop(("hT", t))
            for kc in range(8):
                mm(P, lgp[:, t * 36:(t + 1) * 36], hT[:, kc, :], wrt[:, kc, :], kc == 0, kc == 7, [hTk, ("wrt", kc)], [lgk])

        d1_a(0)
        d1_a(1)
        for t in range(0, NTB, 2):
            if t + 2 < NTB:
                d1_a(t + 2)
                d1_a(t + 3)
            P.interleave([lambda t=t: d1_b(t), lambda t=t: d1_b(t + 1)])
            d1_c(t)
            d1_c(t + 1)
        lg = R["lg"]
        tt(P, "dve", lg[:, :, :], lgp[:, 0:NTB * 36].rearrange("p (t c) -> p t c", c=36),
           brt[:, :].unsqueeze(1).broadcast_to([128, NTB, 36]), ALU.add, [lgk, "brt"], [rk("lg")])
        P.add("dve", lambda e: e.reduce_max(out=R["m4"][:, :], in_=lg[:, :, 0:4], axis=AX.X), [rk("lg")], [rk("m4")])
        tt(P, "dve", R["d4"][:, :, :], lg[:, :, 0:4], bc3(R["m4"][:, :], 4), ALU.subtract, [rk("lg"), rk("m4")], [rk("d4")])
        act(P, R["e4"][:, :, :], R["d4"][:, :, :], AF.Exp, [rk("d4")], [rk("e4")])
        P.add("dve", lambda e: e.reduce_sum(out=R["s4"][:, :], in_=R["e4"][:, :, :], axis=AX.X), [rk("e4")], [rk("s4")])
        P.add("dve", lambda e: e.reciprocal(out=R["pg"][:, :], in_=R["s4"][:, :]), [rk("s4")], [rk("pg")])
        ts(P, "dve", R["oh"][:, :, :], R["d4"][:, :, :], 0.0, None, ALU.is_equal, None, [rk("d4")], [rk("oh")])
        tt(P, "dve", R["t48"][:, :, :, :], lg[:, :, 4:36].rearrange("p t (g e) -> p t g e", e=8),
           R["oh"][:, :, :].unsqueeze(3).broadcast_to([128, NTB, 4, 8]), ALU.mult, [rk("lg"), rk("oh")], [rk("t48")])
        P.add("dve", lambda e: e.reduce_sum(out=R["el"][:, :, :], in_=R["t48"][:, :, :, :].rearrange("p t g e -> p t e g"),
                                            axis=AX.X), [rk("t48")], [rk("el")])
        P.add("dve", lambda e: e.reduce_max(out=R["m1"][:, :], in_=R["el"][:, :, :], axis=AX.X), [rk("el")], [rk("m1")])
        tt(P, "dve", R["k1"][:, :, :], R["el"][:, :, :], bc3(R["m1"][:, :], 8), ALU.is_equal, [rk("el"), rk("m1")], [rk("k1")])
        stt(P, "dve", R["el2"][:, :, :], R["k1"][:, :, :], -1.0e30, R["el"][:, :, :], ALU.mult, ALU.add, [rk("k1"), rk("el")],
            [rk("el2")])
        P.add("dve", lambda e: e.reduce_max(out=R["m2"][:, :], in_=R["el2"][:, :, :], axis=AX.X), [rk("el2")], [rk("m2")])
        tt(P, "dve", R["k2"][:, :, :], R["el2"][:, :, :], bc3(R["m2"][:, :], 8), ALU.is_equal, [rk("el2"), rk("m2")], [rk("k2")])
        tt(P, "dve", R["dd"][:, :], R["m2"][:, :], R["m1"][:, :], ALU.subtract, [rk("m1"), rk("m2")], [rk("dd")])
        act(P, R["ee"][:, :], R["dd"][:, :], AF.Exp, [rk("dd")], [rk("ee")])
        ts(P, "dve", R["p2"][:, :], R["ee"][:, :], 1.0, None, ALU.add, None, [rk("ee")], [rk("p2")])
        P.add("dve", lambda e: e.reciprocal(out=R["p1"][:, :], in_=R["p2"][:, :]), [rk("p2")], [rk("p1")])
        tt(P, "dve", W1[:, ts_], R["p1"][:, :], R["pg"][:, :], ALU.mult, [rk("p1"), rk("pg")], [("W1", nb)])
        tt(P, "dve", W2[:, ts_], W1[:, ts_], R["ee"][:, :], ALU.mult, [("W1", nb), rk("ee")], [("W2", nb)])
        ohb = R["oh"][:, :, :].unsqueeze(3).broadcast_to([128, NTB, 4, 8])
        tt(P, "dve", M1[:, ts_, :].rearrange("p t (g e) -> p t g e", e=8), ohb,
           R["k1"][:, :, :].unsqueeze(2).broadcast_to([128, NTB, 4, 8]), ALU.mult, [rk("oh"), rk("k1")], [("M1", nb)])
        tt(P, "dve", M2[:, ts_, :].rearrange("p t (g e) -> p t g e", e=8), ohb,
           R["k2"][:, :, :].unsqueeze(2).broadcast_to([128, NTB, 4, 8]), ALU.mult, [rk("oh"), rk("k2")], [("M2", nb)])
    NBK = TOK // BLK
    mkeys = [("M1", nb) for nb in range(NBK)] + [("M2", nb) for nb in range(NBK)]
    wkeys = [("W1", nb) for nb in range(NBK)] + [("W2", nb) for nb in range(NBK)]

    M12 = P.sb([128, NT * NE], BF16)
    Wn = P.sb([128, NT, NE], F32)
    TA = P.sb([128, NT, NE], F32)
    TB = P.sb([128, NT, NE], F32)
    T0 = P.sb([128, NT, NE], F32)
    SL = P.sb([128, NT, NE], F32)
    ea = P.sb([128, NE], F32)
    eb = P.sb([128, NE], F32)
    pcf = P.sb([128, NE], F32)
    pci = P.sb([128, NE], I32)
    bexc = P.sb([128, NE], F32)
    posf = P.sb([128, 2, NT], F32)
    cmp_ = P.sb([128, NSLOT_T, NE], F32)
    tef = P.sb([128, NSLOT_T], F32)
    PR1 = P.sb([128, NT, NE], F32)
    PR2 = P.sb([128, NT, NE], F32)
    tt(P, "dve", M12[:, :].rearrange("p (t e) -> p t e", e=NE), M1[:, :, :], M2[:, :, :], ALU.add, mkeys, ["M12"])
    for half in range(2):
        p, pk = pb.next()
        mm(P, p[:, :], ltri[:, :], M12[:, half * 512:(half + 1) * 512], True, True, ["ltri", "M12"], [pk])
        cpy(P, "act", Wn[:, half * 16:(half + 1) * 16, :].rearrange("p t e -> p (t e)"), p[:, :], [pk], [("Wn", half)])
        p, pk = pb.next()
        mm(P, p[:, :], ones[:, :], M12[:, half * 512:(half + 1) * 512], True, True, ["ones", "M12"], [pk])
        cpy(P, "dve", T0[:, half * 16:(half + 1) * 16, :].rearrange("p t e -> p (t e)"), p[:, :], [pk], [("T0", half)])
    P.add("dve", lambda e: e.tensor_copy(out=TA[:, :, :], in_=T0[:, :, :]), [("T0", 0), ("T0", 1)], ["TA"])
    cur, curk, oth, othk = TA, "TA", TB, "TB"
    for sft in (1, 2, 4, 8, 16):
        cpy(P, "dve", oth[:, 0:sft, :], cur[:, 0:sft, :], [curk], [othk])
        tt(P, "dve", oth[:, sft:NT, :], cur[:, sft:NT, :], cur[:, 0:NT - sft, :], ALU.add, [curk, othk], [othk])
        cur, curk, oth, othk = oth, othk, cur, curk
    incl, inclk = cur, curk
    cpy(P, "dve", pci[:, :], incl[:, NT - 1, :], [inclk], ["pci"])
    P.add("dve", lambda e: e.tensor_single_scalar(out=pci[:, :], in_=pci[:, :], scalar=WPAD - 1, op=ALU.add), ["pci"], ["pci"])
    P.add("dve", lambda e: e.tensor_single_scalar(out=pci[:, :], in_=pci[:, :], scalar=8, op=ALU.arith_shift_right), ["pci"], ["pci"])
    P.add("dve", lambda e: e.tensor_single_scalar(out=pci[:, :], in_=pci[:, :], scalar=8, op=ALU.logical_shift_left), ["pci"], ["pci"])
    cpy(P, "dve", pcf[:, :], pci[:, :], ["pci"], ["pcf"])
    cpy(P, "dve", ea[:, :], pcf[:, :], ["pcf"], ["ea"])
    c2, c2k, o2, o2k = ea, "ea", eb, "eb"
    for sft in (1, 2, 4, 8, 16):
        cpy(P, "dve", o2[:, 0:sft], c2[:, 0:sft], [c2k], [o2k])
        tt(P, "dve", o2[:, sft:NE], c2[:, sft:NE], c2[:, 0:NE - sft], ALU.add, [c2k, o2k], [o2k])
        c2, c2k, o2, o2k = o2, o2k, c2, c2k
    pend, pendk = c2, c2k
    tt(P, "dve", bexc[:, :], pend[:, :], pcf[:, :], ALU.subtract, [pendk, "pcf"], ["bexc"])
    tt(P, "dve", SL[:, :, :], incl[:, :, :], T0[:, :, :], ALU.subtract, [inclk, ("T0", 0), ("T0", 1)], ["SL"])
    tt(P, "dve", SL[:, :, :], SL[:, :, :], Wn[:, :, :], ALU.add, ["SL", ("Wn", 0), ("Wn", 1)], ["SL"])
    tt(P, "dve", SL[:, :, :], SL[:, :, :], bexc[:, :].unsqueeze(1).broadcast_to([128, NT, NE]), ALU.add, ["SL", "bexc"], ["SL"])
    tt(P, "dve", PR1[:, :, :], SL[:, :, :], M1[:, :, :], ALU.mult, ["SL"] + mkeys, ["PR1"])
    P.add("dve", lambda e: e.reduce_sum(out=posf[:, 0, :], in_=PR1[:, :, :], axis=AX.X), ["PR1"], [("posf", 0)])
    tt(P, "dve", PR2[:, :, :], SL[:, :, :], M2[:, :, :], ALU.mult, ["SL"] + mkeys, ["PR2"])
    P.add("dve", lambda e: e.reduce_sum(out=posf[:, 1, :], in_=PR2[:, :, :], axis=AX.X), ["PR2"], [("posf", 1)])
    cpy(P, "dve", posi[:, :, :], posf[:, :, :], [("posf", 0), ("posf", 1)], ["posi"])
    tt(P, "dve", cmp_[:, :, :], pend[:, :].unsqueeze(1).broadcast_to([128, NSLOT_T, NE]),
       tstart.unsqueeze(2).broadcast_to([128, NSLOT_T, NE]), ALU.is_le, [pendk, "sortc"], ["cmp"])
    P.add("dve", lambda e: e.reduce_sum(out=tef[:, :], in_=cmp_[:, :, :], axis=AX.X), ["cmp"], ["tef"])
    ts(P, "dve", tef[:, :], tef[:, :], float(NE - 1), 128.0, ALU.min, ALU.mult, ["tef"], ["tef"])
    ts(P, "dve", tef[:, :], tef[:, :], pidx, None, ALU.add, None, ["tef", "sortc"], ["tef"])
    cpy(P, "dve", idxw[:, :], tef[:, :], ["tef"], ["idxw"])

    for t in range(0 if "no_scatter" not in d else NT, NT):
        hb, hbk = h1b.next()
        P.dma("sp", hb[:, :], d["h1b"][t * 128:(t + 1) * 128, :], reads=[("dram", "h1b")], writes=[hbk])
        for k in range(2):
            P.add("pool", lambda e, hb=hb, k=k, t=t: e.indirect_dma_start(
                out=d["xs"][:, :], out_offset=bass.IndirectOffsetOnAxis(ap=posi[:, k, t:t + 1], axis=0), in_=hb[:, :], in_offset=None),
                [hbk, "posi"], [("dram", "xs")], dma=True, semkey="xs_sc")
    if "dbg_posi" in d:
        P.dma("sp", d["dbg_posi"][:, :], posi[:, :, :].rearrange("p a b -> p (a b)"), reads=["posi"], semkey="dbg1")
        P.dma("sp", d["dbg_idxw"][:, :], idxw[:, :], reads=["idxw"], semkey="dbg2")
        P.dma("sp", d["dbg_W"][:, 0:NT], W1[:, :], reads=wkeys, semkey="dbg3")
        P.dma("sp", d["dbg_W"][:, NT:2 * NT], W2[:, :], reads=wkeys, semkey="dbg3")
        P.dma("sp", d["dbg_M1"][:, :], M1[:, :, :].rearrange("p a b -> p (a b)"), reads=mkeys, semkey="dbg4")
        P.dma("sp", d["dbg_M2"][:, :], M2[:, :, :].rearrange("p a b -> p (a b)"), reads=mkeys, semkey="dbg4")
    P.emit()


def phase_Db(nc, l, d, last):
    P = Phase(nc, f"E{l}", d.get("pool"))
    NT = TOK // 128
    identf = P.sb([128, 128], F32)
    ident = P.sb([128, 128], BF16)
    epsl = P.sb([128, 1], F32)
    lnp = P.sb([128, 2, DM], F32)
    mset(P, "pool", epsl[:], LN_EPS, ["eps"])
    P.dma("sp", identf[:], d["ident"][:, :], writes=["identf"])
    cpy(P, "dve", ident[:], identf[:], ["identf"], ["ident"])
    for i in range(2):
        P.dma("sp", lnp[:, i, :], d["lnp"][2 + 4 * l + 2 + i, :, :], writes=[("lnp", 2 + i)])
    g2, b2 = lnp[:, 0, :], lnp[:, 1, :]
    W1, W2, posi, idxw = d["sbW1"], d["sbW2"], d["sbposi"], d["sbidxw"]
    wkeys = []
    hts = Rot([P.sb([128, DM], F32) for _ in range(3)], "ht")
    res = Rot([P.sb([128, DM], F32) for _ in range(4)], "res")
    h1s = Rot([P.sb([128, DM], F32) for _ in range(4)], "h1")
    lts = [ln_tmp(P), ln_tmp(P)]
    pb = Rot([P.ps([128, 512], F32) for _ in range(6)], "pb")
    trp = Rot([P.ps([128, 1024], BF16) for _ in range(1)], "trp")
    tp2 = Rot([P.ps([128, 256], BF16) for _ in range(1)], "tp2")

    wsb = Rot([P.sb([128, 6144], BF16) for _ in range(3)], "wsb")
    xts = Rot([P.sb([128, DM], BF16) for _ in range(4)], "xst")
    xTs = Rot([P.sb([128, 8, 128], BF16) for _ in range(3)], "xT")
    sgs = Rot([P.sb([128, 256], F32) for _ in range(3)], "sg")
    hids = Rot([P.sb([128, 256], BF16) for _ in range(3)], "hid")
    hTs = Rot([P.sb([128, 2, 128], BF16) for _ in range(2)], "hidT")
    yos = Rot([P.sb([128, DM], BF16) for _ in range(2)], "yo")
    wcur = {}
    stX = {}

    def stage_x(i):
        if i % (WPAD // 128) == 0:
            w_, wk = wsb.next()
            P.add("pool", lambda e, w_=w_, i=i: e.indirect_dma_start(
                out=w_[:, :], out_offset=None, in_=d["wbf"][:, :], in_offset=bass.IndirectOffsetOnAxis(ap=idxw[:, i:i + 1], axis=0)),
                [], [wk], dma=True)
            wcur["w"] = (w_, wk)
        w_, wk = wcur["w"]
        x_, xk = xts.next()
        P.dma("sp", x_[:, :], d["xs"][i * 128:(i + 1) * 128, :], reads=[("dram", "xs")], writes=[xk])
        tp, tk = trp.next()
        for kc in range(8):
            trn(P, tp[:, kc * 128:(kc + 1) * 128], x_[:, kc * 128:(kc + 1) * 128], ident[:], [xk, "ident"], [tk])
        xT, xTk = xTs.next()
        cpy(P, "dve" if i % 2 else "act", xT[:, :, :], tp[:, :].rearrange("p (k t) -> p k t", k=8), [tk], [xTk])
        pg, pgk = pb.next()
        wgu = w_[:, 0:4096].rearrange("p (g k f) -> p k g f", g=2, k=8)
        for kc in range(8):
            mm(P, pg[:, :].rearrange("p (g f) -> p g f", g=2), xT[:, kc, :], wgu[:, kc, :, :], kc == 0, kc == 7, [xTk, wk], [pgk])
        sg, sgk = sgs.next()
        act(P, sg[:, :], pg[:, 0:256], AF.Silu, [pgk], [sgk])
        hid, hidk = hids.next()
        tt(P, "dve", hid[:, :], sg[:, :], pg[:, 256:512], ALU.mult, [sgk, pgk], [hidk])
        stX[i] = (w_, wk, hid, hidk)

    def stage_y(i):
        w_, wk, hid, hidk = stX.pop(i)
        t2, t2k = tp2.next()
        for fc in range(2):
            trn(P, t2[:, fc * 128:(fc + 1) * 128], hid[:, fc * 128:(fc + 1) * 128], ident[:], [hidk, "ident"], [t2k])
        hT_, hTk_ = hTs.next()
        cpy(P, "act", hT_[:, :, :], t2[:, :].rearrange("p (k t) -> p k t", k=2), [t2k], [hTk_])
        yo, yok = yos.next()
        for half in range(2):
            pd, pdk = pb.next()
            for fc in range(2):
                mm(P, pd[:, :], hT_[:, fc, :], w_[:, 4096 + fc * 1024 + half * 512:4096 + fc * 1024 + (half + 1) * 512], fc == 0, fc == 1,
                   [hTk_, wk], [pdk])
            cpy(P, "dve" if half else "act", yo[:, half * 512:(half + 1) * 512], pd[:, :], [pdk], [(yok, half)])
        P.dma("act", d["ys"][i * 128:(i + 1) * 128, :], yo[:, :], reads=[(yok, 0), (yok, 1)], writes=[("dram", "ys")], semkey="ys_st")

    stage_x(0)
    for i in range(NSLOT_T):
        if i + 1 < NSLOT_T:
            stage_x(i + 1)
        stage_y(i)

    y1s = Rot([P.sb([128, DM], BF16) for _ in range(3)], "y1")
    y2s = Rot([P.sb([128, DM], BF16) for _ in range(3)], "y2")
    fs_ = Rot([P.sb([128, DM], F32) for _ in range(2)], "ff")
    st3 = {}

    def d3_a(t):
        ys_ = []
        for k, rot in ((0, y1s), (1, y2s)):
            y_, yk = rot.next()
            P.add("pool", lambda e, y_=y_, k=k, t=t: e.indirect_dma_start(
                out=y_[:, :], out_offset=None, in_=d["ys"][:, :], in_offset=bass.IndirectOffsetOnAxis(ap=posi[:, k, t:t + 1], axis=0)),
                [("dram", "ys")], [yk], dma=True)
            ys_.append((y_, yk))
        h1, h1k = h1s.next()
        P.dma("sp", h1[:, :], d["h1buf"][t * 128:(t + 1) * 128, :], reads=[("dram", "h1")], writes=[h1k])
        f_, fk = fs_.next()
        ts(P, "dve", f_[:, :], ys_[0][0][:, :], W1[:, t:t + 1], None, ALU.mult, None, [ys_[0][1]] + wkeys, [fk])
        stt(P, "dve", f_[:, :], ys_[1][0][:, :], W2[:, t:t + 1], f_[:, :], ALU.mult, ALU.add, [ys_[1][1], fk] + wkeys, [fk])
        r_, rk_ = res.next()
        stt(P, "dve", r_[:, :], h1[:, :], ALPHA, f_[:, :], ALU.mult, ALU.add, [h1k, fk], [rk_])
        st3[t] = (r_, rk_)

    def d3_b(t):
        r_, rk_ = st3.pop(t)
        ht, hk = hts.next()
        layer_norm_tile(P, r_[:, :], rk_, ht[:, :], hk, g2, b2, [("lnp", 2), ("lnp", 3)], lts[t % 2], epsl[:, 0:1], ("lnD", t % 2))
        dst = d["y"] if last else d["hbuf"]
        P.dma("act", dst[t * 128:(t + 1) * 128, :], ht[:, :], reads=[hk])

    d3_a(0)
    d3_a(1)
    for t in range(0, NT, 2):
        if t + 2 < NT:
            d3_a(t + 2)
            d3_a(t + 3)
        P.interleave([lambda t=t: d3_b(t), lambda t=t: d3_b(t + 1)])
    P.emit()


def _rope_tables():
    inv = (1.0 / (10000.0 ** (np.arange(0, DR, 2, dtype=np.float32) / DR))).astype(np.float32)
    ang = np.arange(SEQ, dtype=np.float32)[:, None] * inv[None, :]
    cos = np.cos(ang).astype(np.float32).T
    sin = np.sin(ang).astype(np.float32).T
    c2 = np.concatenate([cos, cos], 0)
    s2 = np.concatenate([-sin, sin], 0)
    out = np.zeros((128, 2, SEQ), np.float32)
    out[0:32, 0], out[0:32, 1] = c2, s2
    out[64:96, 0], out[64:96, 1] = c2 * np.float32(QSCALE), s2 * np.float32(QSCALE)
    return out


def _dft_tables():
    n = np.arange(128, dtype=np.float64)
    ang1 = 2 * np.pi * np.outer(n, n) / 128.0
    dft1 = np.concatenate([np.cos(ang1), -np.sin(ang1)], 1).astype(np.float32)
    c = np.arange(64, dtype=np.float64)
    angc = 2 * np.pi * np.outer(c, c) / 64.0
    sc = 1.0 / np.sqrt(SEQ * 64.0)
    bdc = np.kron(np.eye(2), np.cos(angc)) * sc
    bds = np.kron(np.eye(2), np.sin(angc)) * sc
    dftc = np.concatenate([bdc, bds], 1).astype(np.float32)
    per_core = []
    b = np.arange(128, dtype=np.float64)[:, None, None]
    k1 = np.arange(128, dtype=np.float64)[None, :, None]
    for j in range(4):
        k2 = (32 * j + np.arange(32, dtype=np.float64))[None, None, :]
        ang = 2 * np.pi * ((b * (k1 + 128.0 * k2)) % SEQ) / SEQ
        tr, ti = np.cos(ang), -np.sin(ang)
        per_core.append(np.ascontiguousarray(np.concatenate([-ti, tr, ti], 2).astype(np.float32)))
    return dft1, dftc, per_core


def _prep(inp):
    f = np.float32
    L = DEPTH
    w_in = np.asarray(inp["w_in"], f)
    sp = np.cumsum([0, 384, 256, 32, 256, 256, 256, 256])
    cq, ckv, kpe, bg, cg, hc, ff = [w_in[:, :, sp[i]:sp[i + 1]] for i in range(7)]
    kps = np.concatenate([kpe[:, :, 16:32], kpe[:, :, 0:16]], -1)
    w_in2 = np.ascontiguousarray(np.concatenate([cq, ckv, kpe, kps, bg, cg, hc, ff], -1))
    w_uq = np.asarray(inp["w_uq"], f)
    w_uq_sw = np.concatenate([w_uq[..., 0:64], w_uq[..., 80:96], w_uq[..., 64:80]], -1)
    w_uq2 = np.ascontiguousarray(np.stack([w_uq, w_uq_sw], 2).reshape(L, QL, 2 * NH * 96))
    w_ukv = np.asarray(inp["w_ukv"], f)
    w_k = np.ascontiguousarray(w_ukv[..., 0:64].reshape(L, KVL, NH * 64))
    w_v = np.ascontiguousarray(w_ukv[..., 64:128].reshape(L, KVL, NH * 64))
    g_q = np.ascontiguousarray(np.asarray(inp["g_q"], f).reshape(L, 3, 128).transpose(0, 2, 1))
    g_kv = np.ascontiguousarray(np.asarray(inp["g_kv"], f).reshape(L, 2, 128).transpose(0, 2, 1))
    lnp = np.stack([inp["ln_in_g"], inp["ln_in_b"]] + [inp[k][l] for l in range(L) for k in ("ln1_g", "ln1_b", "ln2_g", "ln2_b")])
    lnp = np.ascontiguousarray(np.broadcast_to(np.asarray(lnp, f)[:, None, :], (2 + 4 * L, 128, DM)))
    rope = _rope_tables()
    dft1, dftc, dft2 = _dft_tables()
    w_conv = np.ascontiguousarray(np.asarray(inp["w_conv"], f).reshape(L, 3, 2, 128).transpose(0, 3, 2, 1))
    w_rt = np.ascontiguousarray(np.concatenate([np.asarray(inp["w_group"], f), np.asarray(inp["w_router"], f).reshape(L, DM, NE)], -1))
    b_rt = np.concatenate([np.asarray(inp["b_group"], f), np.asarray(inp["b_router"], f).reshape(L, NE)], -1)
    b_rt = np.ascontiguousarray(np.broadcast_to(b_rt[:, None, :], (L, 128, 36)))
    sortc = np.zeros((128, 128 + NSLOT_T + 1), f)
    sortc[:, 0:128] = np.triu(np.ones((128, 128), f), 1)
    sortc[:, 128:128 + NSLOT_T] = 128.0 * np.arange(NSLOT_T, dtype=f)[None, :]
    sortc[:, 128 + NSLOT_T] = np.arange(128, dtype=f)
    shared = dict(sortc=sortc, w_out=np.asarray(inp["w_out"], f), w_rt=w_rt, b_rt=b_rt,
                  w_gate=np.asarray(inp["w_gate"], f).reshape(L, NE, DM, DE), w_up=np.asarray(inp["w_up"], f).reshape(L, NE, DM, DE),
                  w_down=np.asarray(inp["w_down"], f).reshape(L, NE, DE, DM),
                  w_in=w_in2, w_uq=w_uq2, w_ukv_k=w_k, w_ukv_v=w_v, g_q=g_q, g_kv=g_kv, lnp=lnp,
                  ident=np.eye(128, dtype=f), dft1=dft1, dftc=dftc, w_conv=w_conv)
    x = np.asarray(inp["x"], f)
    maps = []
    for c in range(NCORES):
        b, j = c // 4, c % 4
        m = dict(shared)
        m["x"] = np.ascontiguousarray(x[b, j * TOK:(j + 1) * TOK])
        m["rope"] = np.ascontiguousarray(rope[:, :, j * TOK:(j + 1) * TOK])
        m["dft2"] = dft2[j]
        hs = np.zeros((128, 2, 4), f)
        if j > 0:
            hs[:, 0, j - 1] = 1.0
        if j < 3:
            hs[:, 1, j + 1] = 1.0
        m["halo_sel"] = hs
        maps.append(m)
    return maps


def build(debug=None, stop_after=None, stop_layers=DEPTH):
    nc = bass.Bass("TRN2", target_bir_lowering=False)
    L = DEPTH
    d = {"pool": SemPool(nc)}

    def inp(name, shape, dt=F32):
        d[name] = nc.dram_tensor(name, list(shape), dt, kind="ExternalInput").ap()

    def scr(name, shape, dt):
        kind = "ExternalOutput" if (debug and name in debug) else None
        t = nc.dram_tensor(name, list(shape), dt, kind=kind) if kind else nc.dram_tensor(name, list(shape), dt)
        d[name + "_t"] = t
        d[name] = t.ap()

    inp("x", [TOK, DM]); inp("rope", [128, 2, TOK]); inp("w_in", [L, DM, WIN_COLS]); inp("w_uq", [L, QL, 2 * NH * 96])
    inp("w_ukv_k", [L, KVL, 512]); inp("w_ukv_v", [L, KVL, 512]); inp("g_q", [L, 128, 3]); inp("g_kv", [L, 128, 2])
    inp("lnp", [2 + 4 * L, 128, DM]); inp("ident", [128, 128])
    inp("dft1", [128, 256]); inp("dftc", [128, 256]); inp("dft2", [128, 128, 96]); inp("w_conv", [L, 128, 2, 3])
    inp("halo_sel", [128, 2, 4]); inp("sortc", [128, 128 + NSLOT_T + 1])
    inp("w_out", [L, DM, DM]); inp("w_rt", [L, DM, 36]); inp("b_rt", [L, 128, 36])
    inp("w_gate", [L, NE, DM, DE]); inp("w_up", [L, NE, DM, DE]); inp("w_down", [L, NE, DE, DM])
    scr("hbuf", [TOK, DM], F32)
    scr("h1buf", [TOK, DM], F32)
    scr("wbf", [NE * 128, 6144], BF16)
    scr("h1b", [TOK, DM], BF16)
    scr("xs", [NSLOT, DM], BF16)
    scr("ys", [NSLOT, DM], BF16)
    scr("qT", [NH, 96, TOK], BF16)
    def scrl(name, n, shape, dt):
        ts_ = [nc.dram_tensor(f"{name}{i}", list(shape), dt) for i in range(n)]
        d[name + "_t"] = ts_
        d[name] = [t.ap() for t in ts_]

    scrl("payK", 4, [128, TOK], BF16); scrl("gK", 4, [4 * 128, TOK], BF16)
    scr("payKr", [32, TOK], BF16); scr("gKr", [4 * 32, TOK], BF16)
    scrl("payV", NH, [128, 32 * 65], BF16); scrl("gV", NH, [4 * 128, 32 * 65], BF16)
    scrl("payF", 2, [TOK, 128], BF16); scrl("gF", 2, [4 * TOK, 128], BF16)
    scr("payH", [256, 16], F32); scr("gH", [4 * 256, 16], F32)
    scr("upT", [256, TOK], F32)
    scr("bgT", [256, TOK], F32)
    scr("omixT", [DM, TOK], BF16)
    d["y"] = nc.dram_tensor("y", [TOK, DM], F32, kind="ExternalOutput").ap()
    pst = contextlib.ExitStack()
    d["_pst"] = pst
    d["sbW1"] = pst.enter_context(nc.sbuf_tensor("p_W1", [128, TOK // 128], F32))
    d["sbW2"] = pst.enter_context(nc.sbuf_tensor("p_W2", [128, TOK // 128], F32))
    d["sbposi"] = pst.enter_context(nc.sbuf_tensor("p_posi", [128, 2, TOK // 128], I32))
    d["sbidxw"] = pst.enter_context(nc.sbuf_tensor("p_idxw", [128, NSLOT_T], I32))
    if debug and "dbgF" in debug:
        d["dbgY1"] = nc.dram_tensor("dbgY1", [128, 256 * 128], BF16, kind="ExternalOutput").ap()
        d["dbgF"] = nc.dram_tensor("dbgF", [128, 128 * 128], BF16, kind="ExternalOutput").ap()
        d["dbgX"] = nc.dram_tensor("dbgX", [128, 128 * 64], BF16, kind="ExternalOutput").ap()
    for l in range(stop_layers):
        phase_A(nc, l, d)
        if stop_after == "A":
            break
        phase_B(nc, l, d)
        if stop_after == "B":
            break
        phase_C1(nc, l, d)
        phase_C2(nc, l, d)
        if stop_after == "C":
            break
        phase_D(nc, l, d, last=(l == DEPTH - 1))
    return nc


def kernel(**inputs):
    maps = _prep(inputs)
    nc = build()
    res = run_bass_kernel_spmd(nc, maps, core_ids=list(range(NCORES)))
    out = np.empty((BATCH, SEQ, DM), np.float32)
    for c in range(NCORES):
        out[c // 4, (c % 4) * TOK:(c % 4 + 1) * TOK] = res.results[c]["y"]
    return out
```

```python
import numpy as np
from concourse.bass_utils import run_bass_kernel_spmd
import contextlib
import concourse.bass as bass
import concourse.mybir as mybir

F32 = mybir.dt.float32
BF16 = mybir.dt.bfloat16
I32 = mybir.dt.int32
AF = mybir.ActivationFunctionType
ALU = mybir.AluOpType
AX = mybir.AxisListType

ENGS = ("pe", "act", "dve", "pool", "sp")


class SemPool:
    NHW = 46
    NDMA = 72

    def __init__(self, nc):
        self.stack = contextlib.ExitStack()
        self.eng = {e: self.stack.enter_context(nc.semaphore(f"sem_{e}")) for e in ENGS}
        self.eng_cnt = {e: 0 for e in ENGS}
        self.dma = [self.stack.enter_context(nc.semaphore(f"sem_d{i}")) for i in range(self.NDMA)]
        self.dma_cnt = [0] * self.NDMA


class Phase:
    def __init__(self, nc, name, pool=None):
        self.nc = nc
        self.name = name
        self.pool = pool if pool is not None else SemPool(nc)
        self.ins = []
        self.stack = contextlib.ExitStack()
        self.nbuf = 0

    def sb(self, shape, dt, name=None):
        self.nbuf += 1
        return self.stack.enter_context(self.nc.sbuf_tensor(f"{self.name}_{name or 'sb'}{self.nbuf}", list(shape), dt))

    def ps(self, shape, dt, name=None):
        self.nbuf += 1
        return self.stack.enter_context(self.nc.psum_tensor(f"{self.name}_{name or 'ps'}{self.nbuf}", list(shape), dt))

    def add(self, eng, fn, reads=(), writes=(), dma=False, semkey=None, inc=16):
        assert eng in ENGS
        reads = tuple(reads)
        writes = tuple(writes)
        if dma and semkey is None:
            sb_w = [k for k in writes if not (isinstance(k, tuple) and k and k[0] == "dram")]
            sb_r = [k for k in reads if not (isinstance(k, tuple) and k and k[0] == "dram")]
            semkey = sb_w[0] if sb_w else (sb_r[0] if sb_r else writes[0])
        self.ins.append(dict(eng=eng, fn=fn, reads=reads, writes=writes, dma=dma, semkey=semkey, inc=inc))

    def dma(self, eng, out, in_, reads=(), writes=(), semkey=None, **kw):
        self.add(eng, lambda e: e.dma_start(out=out, in_=in_, **kw), reads, writes, dma=True, semkey=semkey)

    def allgather(self, src_t, dst_t, reads=(), writes=(), semkey=None):
        self.add("pool", lambda e: e.collective_compute("AllGather", ALU.bypass, replica_groups=[[0, 1, 2, 3], [4, 5, 6, 7]],
                                                        ins=[src_t.ap().opt()], outs=[dst_t.ap().opt()]),
                 reads, writes, dma=True, semkey=semkey, inc=1)

    def interleave(self, fns):
        lists = []
        for fn in fns:
            keep, self.ins = self.ins, []
            fn()
            lists.append(self.ins)
            self.ins = keep
        n = max(len(x) for x in lists)
        for i in range(n):
            for x in lists:
                if i < len(x):
                    self.ins.append(x[i])

    def emit(self):
        nc = self.nc
        ins = self.ins
        n = len(ins)
        last_w = {}
        readers = {}
        deps = [None] * n
        for i, I in enumerate(ins):
            d = {}
            for k in I["reads"]:
                j = last_w.get(k)
                if j is not None:
                    d[j] = d.get(j, 0) | 1
            for k in I["writes"]:
                j = last_w.get(k)
                if j is not None:
                    d[j] = d.get(j, 0) | 2
                for r in readers.get(k, ()):
                    if r != i:
                        d[r] = d.get(r, 0) | 4
            for k in I["reads"]:
                readers.setdefault(k, []).append(i)
            for k in I["writes"]:
                last_w[k] = i
                readers[k] = []
            need = []
            for j, ty in d.items():
                J = ins[j]
                if J["dma"]:
                    need.append(j)
                elif J["eng"] == I["eng"]:
                    if I["eng"] == "pe":
                        continue
                    if I["dma"] or (ty & 3):
                        need.append(j)
                else:
                    need.append(j)
            deps[i] = need
        sig = [False] * n
        for i in range(n):
            for j in deps[i]:
                if not ins[j]["dma"]:
                    sig[j] = True
        pool = self.pool
        cnt = dict(pool.eng_cnt)
        val = [0] * n
        dcount = {}
        dslot = {}
        nhw = nsw = 0
        dma_before = [None] * n
        dma_pos = {}
        dma_cum = {}
        for i, I in enumerate(ins):
            if I["dma"]:
                k = I["semkey"]
                if k not in dslot:
                    if I["eng"] == "pool":
                        nsw += 1
                        dslot[k] = pool.NHW + nsw - 1
                        assert dslot[k] < pool.NDMA, "too many software-DMA semaphores in one phase"
                    else:
                        nhw += 1
                        dslot[k] = nhw - 1
                        assert dslot[k] < pool.NHW, "too many hardware-DMA semaphores in one phase"
                    dcount[k] = pool.dma_cnt[dslot[k]]
                dcount[k] = dcount[k] + I["inc"]
                val[i] = dcount[k]
                dma_pos.setdefault(k, []).append(i)
                dma_cum.setdefault(k, []).append(dcount[k])
            elif sig[i]:
                cnt[I["eng"]] += 1
                val[i] = cnt[I["eng"]]
        esem = pool.eng
        dsem = {k: pool.dma[i] for k, i in dslot.items()}
        base_wait = {("e", e): pool.eng_cnt[e] for e in ENGS}
        for k, i in dslot.items():
            base_wait[("d", k)] = pool.dma_cnt[i]
            pool.dma_cnt[i] = dcount[k]
        pool.eng_cnt = dict(cnt)
        self.n_sems = len(dsem)
        import bisect
        streams = {e: [] for e in ENGS}
        for i, I in enumerate(ins):
            waits = {}
            for j in deps[i]:
                J = ins[j]
                if J["dma"]:
                    k = J["semkey"]
                    c = bisect.bisect_left(dma_pos[k], i)
                    key = ("d", k)
                    waits[key] = max(waits.get(key, 0), dma_cum[k][c - 1])
                else:
                    key = ("e", J["eng"])
                    waits[key] = max(waits.get(key, 0), val[j])
            streams[I["eng"]].append((i, waits))
        final_dma = dict(dcount)

        def run_engine(ename, eobj):
            waited = dict(base_wait)
            for i, waits in streams[ename]:
                I = ins[i]
                for key, v in waits.items():
                    if waited.get(key, 0) >= v:
                        continue
                    waited[key] = v
                    s = dsem[key[1]] if key[0] == "d" else esem[key[1]]
                    eobj.wait_ge(s, v)
                r = I["fn"](eobj)
                if I["dma"]:
                    r.then_inc(dsem[I["semkey"]], I["inc"])
                elif sig[i]:
                    r.then_inc(esem[ename], 1)
            if ename == "sp":
                for k, v in final_dma.items():
                    if waited.get(("d", k), 0) < v:
                        eobj.wait_ge(dsem[k], v)

        with nc.Block() as block:
            @block.tensor
            def _(e):
                run_engine("pe", e)

            @block.scalar
            def _(e):
                run_engine("act", e)

            @block.vector
            def _(e):
                run_engine("dve", e)

            @block.gpsimd
            def _(e):
                run_engine("pool", e)

            @block.sync
            def _(e):
                run_engine("sp", e)
        self.stack.close()


NCORES = 8
BATCH, SEQ, DM, DEPTH = 2, 16384, 1024, 2
TOK = 4096
NST = TOK // 512
NH, DN, DR, DV = 8, 64, 32, 64
QL, KVL = 384, 256
NG, EPG, DE = 4, 8, 256
NE = NG * EPG
LN_EPS, RMS_EPS = 1e-5, 1e-6
ALPHA = (2.0 * DEPTH) ** 0.25
QSCALE = (DN + DR) ** -0.5
C_CQ, C_CKV, C_KPE, C_KPS, C_BG, C_CG, C_HC, C_F, WIN_COLS = 0, 384, 640, 672, 704, 960, 1216, 1472, 1728
GROUPS = [[0, 1, 2, 3], [4, 5, 6, 7]]


def mm(P, out, lhsT, rhs, start, stop, reads, writes):
    P.add("pe", lambda e: e.matmul(out, lhsT, rhs, start=start, stop=stop), reads, writes)


def trn(P, out, in_, ident, reads, writes):
    P.add("pe", lambda e: e.transpose(out=out, in_=in_, identity=ident), reads, writes)


def act(P, out, in_, func, reads, writes, eng="act", **kw):
    P.add(eng, lambda e: e.activation(out=out, in_=in_, func=func, **kw), reads, writes)


def cpy(P, eng, out, in_, reads, writes):
    if eng == "act":
        P.add(eng, lambda e: e.activation(out=out, in_=in_, func=AF.Copy), reads, writes)
    else:
        P.add(eng, lambda e: e.tensor_copy(out=out, in_=in_), reads, writes)


def tt(P, eng, out, in0, in1, op, reads, writes):
    P.add(eng, lambda e: e.tensor_tensor(out=out, in0=in0, in1=in1, op=op), reads, writes)


def ts(P, eng, out, in0, s1, s2, op0, op1, reads, writes):
    if s2 is None:
        P.add(eng, lambda e: e.tensor_scalar(out=out, in0=in0, scalar1=s1, scalar2=None, op0=op0), reads, writes)
    else:
        P.add(eng, lambda e: e.tensor_scalar(out=out, in0=in0, scalar1=s1, scalar2=s2, op0=op0, op1=op1), reads, writes)


def stt(P, eng, out, in0, scalar, in1, op0, op1, reads, writes):
    P.add(eng, lambda e: e.scalar_tensor_tensor(out=out, in0=in0, scalar=scalar, in1=in1, op0=op0, op1=op1), reads, writes)


def mset(P, eng, ap, v, writes):
    P.add(eng, lambda e: e.memset(ap, v), (), writes)


class Rot:
    def __init__(self, bufs, name):
        self.bufs = bufs
        self.name = name
        self.i = 0

    def next(self):
        k = self.i % len(self.bufs)
        self.i += 1
        return self.bufs[k], (self.name, k)


def layer_norm_tile(P, x_ap, xkey, out_ap, okey, g_ap, b_ap, gkeys, tmp, eps_ap, tag):
    st, mv, sd, rs, nb, y = tmp["st"], tmp["mv"], tmp["sd"], tmp["rs"], tmp["nb"], tmp["y"]
    k = lambda s: (tag, s)
    P.add("dve", lambda e: e.bn_stats(out=st[:, 0, :], in_=x_ap[:, 0:512]), [xkey], [k("st0")])
    P.add("dve", lambda e: e.bn_stats(out=st[:, 1, :], in_=x_ap[:, 512:1024]), [xkey], [k("st1")])
    P.add("dve", lambda e: e.bn_aggr(out=mv[:], in_=st[:]), [k("st0"), k("st1")], [k("mv")])
    act(P, sd[:], mv[:, 1:2], AF.Sqrt, [k("mv"), "eps"], [k("sd")], bias=eps_ap, scale=1.0)
    P.add("dve", lambda e: e.reciprocal(out=rs[:], in_=sd[:]), [k("sd")], [k("rs")])
    stt(P, "dve", nb[:], mv[:, 0:1], -1.0, rs[:], ALU.mult, ALU.mult, [k("mv"), k("rs")], [k("nb")])
    act(P, y[:], x_ap, AF.Identity, [xkey, k("nb"), k("rs")], [k("y")], bias=nb[:, 0:1], scale=rs[:, 0:1])
    tt(P, "dve", y[:], y[:], g_ap, ALU.mult, [k("y")] + gkeys, [k("y")])
    tt(P, "dve", out_ap, y[:], b_ap, ALU.add, [k("y")] + gkeys, [okey])


def ln_tmp(P):
    return dict(st=P.sb([128, 2, 6], F32), mv=P.sb([128, 2], F32), sd=P.sb([128, 1], F32), rs=P.sb([128, 1], F32),
                nb=P.sb([128, 1], F32), y=P.sb([128, 1024], F32))


def phase_A(nc, l, d):
    P = Phase(nc, f"A{l}", d.get("pool"))
    identf = P.sb([128, 128], F32)
    ident = P.sb([128, 128], BF16)
    ones = P.sb([128, 128], BF16)
    epsr = P.sb([128, 1], F32)
    epsl = P.sb([128, 1], F32)
    win = P.sb([128, 8, WIN_COLS], BF16)
    wuq = P.sb([128, 3, 2 * NH * 96], BF16)
    wk = P.sb([128, 2, 512], BF16)
    wv = P.sb([128, 2, 512], BF16)
    gq = P.sb([128, 3], F32)
    gkv = P.sb([128, 2], F32)
    stg = [P.sb([128, WIN_COLS], F32) for _ in range(1)]
    lng = P.sb([128, 1024], F32)
    lnb = P.sb([128, 1024], F32)
    vbufs = [P.sb([128, NH, 4, 65], BF16) for _ in range(2)]

    mset(P, "pool", ones[:], 1.0, ["ones"])
    mset(P, "pool", epsr[:], RMS_EPS, ["eps"])
    mset(P, "pool", epsl[:], LN_EPS, ["eps"])
    mset(P, "pool", vbufs[0][:, :, :, 64:65], 1.0, [("vones", 0)])
    mset(P, "pool", vbufs[1][:, :, :, 64:65], 1.0, [("vones", 1)])
    P.dma("sp", identf[:], d["ident"][:, :], writes=["identf"])
    cpy(P, "dve", ident[:], identf[:], ["identf"], ["ident"])
    P.dma("sp", gq[:], d["g_q"][l, :, :], writes=["gq"])
    P.dma("sp", gkv[:], d["g_kv"][l, :, :], writes=["gkv"])
    if l == 0:
        P.dma("sp", lng[:], d["lnp"][0, :, :], writes=["lng"])
        P.dma("sp", lnb[:], d["lnp"][1, :, :], writes=["lnb"])
    si = 0
    for kc in range(8):
        s, sk = stg[0], ("stg", 0)
        si += 1
        P.dma("sp", s[:, :], d["w_in"][l, kc * 128:(kc + 1) * 128, :], writes=[sk])
        cpy(P, "dve" if kc % 2 == 0 else "act", win[:, kc, :], s[:, :], [sk], [("win", kc)])
    for c in range(3):
        s, sk = stg[0], ("stg", 0)
        si += 1
        P.dma("sp", s[:, 0:1536], d["w_uq"][l, c * 128:(c + 1) * 128, :], writes=[sk])
        ts(P, "dve", wuq[:, c, :], s[:, 0:1536], gq[:, c:c + 1], None, ALU.mult, None, [sk, "gq"], [("wuq", c)])
    for c in range(2):
        s, sk = stg[0], ("stg", 0)
        si += 1
        P.dma("sp", s[:, 0:512], d["w_ukv_k"][l, c * 128:(c + 1) * 128, :], writes=[sk])
        P.dma("sp", s[:, 512:1024], d["w_ukv_v"][l, c * 128:(c + 1) * 128, :], writes=[sk])
        ts(P, "dve", wk[:, c, :], s[:, 0:512], gkv[:, c:c + 1], None, ALU.mult, None, [sk, "gkv"], [("wk", c)])
        ts(P, "dve", wv[:, c, :], s[:, 512:1024], gkv[:, c:c + 1], None, ALU.mult, None, [sk, "gkv"], [("wv", c)])
    wkeys = [("win", kc) for kc in range(8)]
    if l == 0:
        zero_slots(P, d)

    xts = [P.sb([128, 4, 1024], F32)] * 2
    hbs = [P.sb([128, 4, 1024], BF16)] * 2
    hTs = [P.sb([128, 8, 512], BF16) for _ in range(2)]
    ropes = [P.sb([128, 2, 512], F32) for _ in range(2)]
    cqTs = [P.sb([128, 3, 512], BF16)] * 2
    sqs = [P.sb([128, 3, 512], BF16)] * 2
    cqns = [P.sb([128, 3, 512], BF16) for _ in range(2)]
    ckvTs = [P.sb([128, 2, 512], BF16)] * 2
    sqks = [P.sb([128, 2, 512], BF16)] * 2
    ckvns = [P.sb([128, 2, 512], BF16) for _ in range(2)]
    sdt = [P.sb([128, 512], F32)] * 2
    rq = [P.sb([128, 512], F32)] * 2
    rk = [P.sb([128, 512], F32)] * 2
    kt1 = [P.sb([32, 512], F32)] * 2
    kt2 = [P.sb([32, 512], F32)] * 2
    krs = [P.sb([32, 512], BF16) for _ in range(2)]
    bgs = [P.sb([128, 2, 512], F32)] * 2
    cgs = [P.sb([128, 2, 512], F32)] * 2
    ups = [P.sb([128, 2, 512], F32)] * 2
    qTs = [P.sb([96, NH, 512], BF16) for _ in range(2)]
    qt1 = Rot([P.sb([96, 512], F32) for _ in range(2)], "qt1")
    qt2 = Rot([P.sb([96, 512], F32) for _ in range(2)], "qt2")
    kTs = [P.sb([128, 4, 512], BF16) for _ in range(2)]
    fs = [P.sb([128, 2, 512], BF16) for _ in range(2)]
    lt = [ln_tmp(P)] * 2 if l == 0 else None
    trp = Rot([P.ps([128, 512], BF16) for _ in range(2)], "trp")
    pb = Rot([P.ps([128, 512], F32) for _ in range(6)], "pb")

    for st in range(NST):
        sl = st % 2
        t0 = st * 512
        xt, hb, hT = xts[sl], hbs[sl], hTs[sl]
        K = lambda s: (s, sl if s in ("rope", "kr") else 0)
        if l == 0:
            P.dma("sp", xt[:, :, :], d["x"][t0:t0 + 512, :].rearrange("(j p) f -> p j f", p=128),
                  writes=[K("xraw")] + [("xt", 0, j) for j in range(4)])
            for j in range(4):
                layer_norm_tile(P, xt[:, j, :], K("xraw"), xt[:, j, :], ("xt", 0, j), lng[:], lnb[:], ["lng", "lnb"], lt[j % 2],
                                epsl[:, 0:1], "lnA")
            xkeys = [("xt", 0, j) for j in range(4)]
            P.dma("pool", d["hbuf"][t0:t0 + 512, :].rearrange("(j p) f -> p j f", p=128), xt[:, :, :], reads=xkeys,
                  semkey=("hst", 0))
        else:
            P.dma("sp", xt[:, :, :], d["hbuf"][t0:t0 + 512, :].rearrange("(j p) f -> p j f", p=128), writes=[K("xraw")])
            xkeys = [K("xraw")]
        P.dma("sp", ropes[sl][:, :, :], d["rope"][:, :, t0:t0 + 512], writes=[K("rope")])
        for j in range(4):
            cpy(P, "act" if j % 2 == 0 else "dve", hb[:, j, :], xt[:, j, :], xkeys if l else [("xt", 0, j)], [("hb", 0, j)])
        for kc in range(8):
            tp, tk = trp.next()
            for j in range(4):
                trn(P, tp[:, j * 128:(j + 1) * 128], hb[:, j, kc * 128:(kc + 1) * 128], ident[:], [("hb", 0, j), "ident"], [tk])
            cpy(P, "act" if kc % 2 == 0 else "dve", hT[:, kc, :], tp[:, :], [tk], [("hT", sl, kc)])
        hkeys = [("hT", sl, kc) for kc in range(8)]

        def zproj(col0, M):
            p, pk = pb.next()
            for kc in range(8):
                mm(P, p[0:M, :], win[:, kc, col0:col0 + M], hT[:, kc, :], kc == 0, kc == 7, [("win", kc), ("hT", sl, kc)], [pk])
            return p, pk

        def cq_chunk(c):
            p, pk = zproj(C_CQ + c * 128, 128)
            cpy(P, "act", cqTs[sl][:, c, :], p[:, :], [pk], [("cqT", 0, c)])
            act(P, sqs[sl][:, c, :], p[:, :], AF.Square, [pk], [("sq", 0, c)])

        P.interleave([lambda c=c: cq_chunk(c) for c in range(3)])
        p, pk = pb.next()
        for c in range(3):
            mm(P, p[:, :], ones[:, :], sqs[sl][:, c, :], c == 0, c == 2, ["ones", ("sq", 0, c)], [pk])
        act(P, sdt[sl][:, :], p[:, :], AF.Sqrt, [pk, "eps"], [K("sdt")], bias=epsr[:, 0:1], scale=1.0 / QL)
        P.add("dve", lambda e, o=rq[sl], i=sdt[sl]: e.reciprocal(out=o[:, :], in_=i[:, :]), [K("sdt")], [K("rq")])
        for c in range(3):
            tt(P, "dve", cqns[sl][:, c, :], cqTs[sl][:, c, :], rq[sl][:, :], ALU.mult,
               [("cqT", 0, c), K("rq")], [("cqn", sl, c)])
        def ckv_chunk(c):
            p, pk = zproj(C_CKV + c * 128, 128)
            cpy(P, "act", ckvTs[sl][:, c, :], p[:, :], [pk], [("ckvT", 0, c)])
            act(P, sqks[sl][:, c, :], p[:, :], AF.Square, [pk], [("sqk", 0, c)])

        P.interleave([lambda c=c: ckv_chunk(c) for c in range(2)])
        p, pk = pb.next()
        for c in range(2):
            mm(P, p[:, :], ones[:, :], sqks[sl][:, c, :], c == 0, c == 1, ["ones", ("sqk", 0, c)], [pk])
        act(P, sdt[sl][:, :], p[:, :], AF.Sqrt, [pk, "eps"], [K("sdt")], bias=epsr[:, 0:1], scale=1.0 / KVL)
        P.add("dve", lambda e, o=rk[sl], i=sdt[sl]: e.reciprocal(out=o[:, :], in_=i[:, :]), [K("sdt")], [K("rk")])
        for c in range(2):
            tt(P, "dve", ckvns[sl][:, c, :], ckvTs[sl][:, c, :], rk[sl][:, :], ALU.mult,
               [("ckvT", 0, c), K("rk")], [("ckvn", sl, c)])
        kp = {}
        P.interleave([lambda: kp.__setitem__(0, zproj(C_KPE, 32)), lambda: kp.__setitem__(1, zproj(C_KPS, 32))])
        (p1, pk1), (p2, pk2) = kp[0], kp[1]
        tt(P, "dve", kt1[sl][:, :], p1[0:32, :], ropes[sl][0:32, 0, :], ALU.mult, [pk1, K("rope")], [K("kt1")])
        tt(P, "dve", kt2[sl][:, :], p2[0:32, :], ropes[sl][0:32, 1, :], ALU.mult, [pk2, K("rope")], [K("kt2")])
        tt(P, "dve", krs[sl][:, :], kt1[sl][:, :], kt2[sl][:, :], ALU.add, [K("kt1"), K("kt2")], [K("kr")])
        P.dma("pool", d["payKr"][0:32, t0:t0 + 512], krs[sl][:, :], reads=[K("kr")])
        def bg_chunk(c):
            p, pk = zproj(C_BG + c * 128, 128)
            cpy(P, "act", bgs[sl][:, c, :], p[:, :], [pk], [("bg", 0, c)])

        P.interleave([lambda c=c: bg_chunk(c) for c in range(2)])
        P.dma("pool", d["bgT"][:, t0:t0 + 512].rearrange("(c p) t -> p c t", p=128), bgs[sl][:, :, :],
              reads=[("bg", 0, 0), ("bg", 0, 1)], semkey=("bgst", 0))
        def cg_chunk(c):
            p, pk = zproj(C_CG + c * 128, 128)
            cpy(P, "act", cgs[sl][:, c, :], p[:, :], [pk], [("cg", 0, c)])

        def hc_chunk(c):
            p, pk = zproj(C_HC + c * 128, 128)
            tt(P, "dve", ups[sl][:, c, :], p[:, :], cgs[sl][:, c, :], ALU.mult, [pk, ("cg", 0, c)], [("up", 0, c)])

        P.interleave([lambda c=c: cg_chunk(c) for c in range(2)])
        P.interleave([lambda c=c: hc_chunk(c) for c in range(2)])
        P.dma("pool", d["upT"][:, t0:t0 + 512].rearrange("(c p) t -> p c t", p=128), ups[sl][:, :, :],
              reads=[("up", 0, 0), ("up", 0, 1)], semkey=("upst", 0))
        if st == 0:
            P.dma("pool", d["payH"][:, 0:1].rearrange("(c p) x -> p c x", p=128), ups[sl][:, :, 0:1],
                  reads=[("up", 0, 0), ("up", 0, 1)], semkey=("upst", 0), allow_slow_non_contiguous=True)
        if st == NST - 1:
            P.dma("pool", d["payH"][:, 1:2].rearrange("(c p) x -> p c x", p=128), ups[sl][:, :, 511:512],
                  reads=[("up", 0, 0), ("up", 0, 1)], semkey=("upst", 0), allow_slow_non_contiguous=True)
        def f_chunk(q):
            p, pk = zproj(C_F + q * 128, 128)
            cpy(P, "act" if q % 2 else "dve", fs[sl][:, q, :], p[:, :], [pk], [("f", sl, q)])
            P.dma("pool", d["payF"][q][st * 512:(st + 1) * 512, :].rearrange("(j c) b -> c j b", c=128),
                  fs[sl][:, q, :].rearrange("c (j b) -> c j b", b=128), reads=[("f", sl, q)], semkey=("fst", sl, q))

        P.interleave([lambda q=q: f_chunk(q) for q in range(2)])
        def q_head(h):
            pa, pak = pb.next()
            pbb, pbk = pb.next()
            for c in range(3):
                mm(P, pa[0:96, :], wuq[:, c, h * 96:(h + 1) * 96], cqns[sl][:, c, :], c == 0, c == 2,
                   [("wuq", c), ("cqn", sl, c)], [pak])
                mm(P, pbb[0:96, :], wuq[:, c, 768 + h * 96:768 + (h + 1) * 96], cqns[sl][:, c, :], c == 0, c == 2,
                   [("wuq", c), ("cqn", sl, c)], [pbk])
            act(P, qTs[sl][0:64, h, :], pa[0:64, :], AF.Copy, [pak], [("qTn", sl, h)], scale=QSCALE)
            t1, t1k = qt1.next()
            t2, t2k = qt2.next()
            tt(P, "dve", t1[64:96, :], pa[64:96, :], ropes[sl][64:96, 0, :], ALU.mult, [pak, K("rope")], [t1k])
            tt(P, "dve", t2[64:96, :], pbb[64:96, :], ropes[sl][64:96, 1, :], ALU.mult, [pbk, K("rope")], [t2k])
            tt(P, "dve", qTs[sl][64:96, h, :], t1[64:96, :], t2[64:96, :], ALU.add, [t1k, t2k], [("qTr", sl, h)])

        for h in range(NH):
            q_head(h)
        P.dma("pool", d["qT"][:, :, t0:t0 + 512].rearrange("h r t -> r h t"), qTs[sl][:, :, :],
              reads=[("qTn", sl, h) for h in range(NH)] + [("qTr", sl, h) for h in range(NH)], semkey=("qst", sl))
        def k_chunk(k2):
            p, pk = pb.next()
            for c in range(2):
                mm(P, p[:, :], wk[:, c, k2 * 128:(k2 + 1) * 128], ckvns[sl][:, c, :], c == 0, c == 1,
                   [("wk", c), ("ckvn", sl, c)], [pk])
            cpy(P, "act" if k2 % 2 else "dve", kTs[sl][:, k2, :], p[:, :], [pk], [("kT", sl, k2)])

        P.interleave([lambda k2=k2: k_chunk(k2) for k2 in range(4)])
        for k2 in range(4):
            P.dma("pool", d["payK"][k2][:, t0:t0 + 512], kTs[sl][:, k2, :], reads=[("kT", sl, k2)], semkey=("kst", sl))
        def v_chunk(j):
            p, pk = pb.next()
            for c in range(2):
                mm(P, p[:, :], ckvns[sl][:, c, j * 128:(j + 1) * 128], wv[:, c, :], c == 0, c == 1,
                   [("wv", c), ("ckvn", sl, c)], [pk])
            cpy(P, "act" if j % 2 else "dve", vbufs[sl][:, :, j, 0:64], p[:, :].rearrange("p (h c) -> p h c", h=NH), [pk],
                [("vb", sl, j)])

        P.interleave([lambda j=j: v_chunk(j) for j in range(4)])
        for h in range(NH):
            P.dma("pool", d["payV"][h].rearrange("p (t c) -> p t c", c=65)[:, st * 4:st * 4 + 4, :],
                  vbufs[sl][:, h, :, :], reads=[("vones", sl)] + [("vb", sl, j) for j in range(4)], semkey=("vst", sl))
    P.emit()


KB = 2


def phase_B(nc, l, d):
    P = Phase(nc, f"B{l}", d.get("pool"))
    onesf = P.sb([128, 64], F32)
    mset(P, "pool", onesf[:], 1.0, ["onesf"])
    KTs = [P.sb([96, SEQ], BF16) for _ in range(2)]
    Vs = [P.sb([128, 4, 32, 65], BF16) for _ in range(2)]
    QTs = [P.sb([96, TOK], BF16) for _ in range(2)]
    sps = Rot([P.ps([128, KB, 512], F32) for _ in range(3)], "sp")
    pos = Rot([P.ps([128, 512], F32) for _ in range(1)], "po")
    bcs = Rot([P.ps([64, 512], F32) for _ in range(1)], "bc")
    pts = Rot([P.sb([128, KB, 512], BF16) for _ in range(4)], "pt")
    rsum = Rot([P.sb([128, 512], F32) for _ in range(2)], "rsum")
    rrec = Rot([P.sb([128, 512], F32) for _ in range(2)], "rrec")
    bcsb = Rot([P.sb([64, 512], F32) for _ in range(2)], "bcsb")
    oTs = Rot([P.sb([64, 512], BF16) for _ in range(2)], "oT")

    order = [("Kr", None), ("K", 0), ("V", 0), ("V", 1)]
    for k2 in range(1, 4):
        order += [("K", k2), ("V", 2 * k2), ("V", 2 * k2 + 1)]
    order += [("F", 0), ("F", 1), ("H", None)]
    prev = None
    for kind, i in order:
        name = {"Kr": "Kr", "K": "K", "V": "V", "F": "F", "H": "H"}[kind]
        src = d["pay" + name + "_t"] if i is None else d["pay" + name + "_t"][i]
        dst = d["g" + name + "_t"] if i is None else d["g" + name + "_t"][i]
        P.allgather(src, dst, reads=[prev] if prev else [], writes=[("dram", "g" + name, i), ("ccchain", kind, i)],
                    semkey=("cc", kind, i))
        prev = ("ccchain", kind, i)

    def load_head(h):
        s = h % 2
        for r in range(4):
            P.dma("sp", KTs[s][0:64, r * TOK:(r + 1) * TOK],
                  d["gK"][h // 2][r * 128 + (h % 2) * 64:r * 128 + (h % 2) * 64 + 64, :],
                  reads=[("dram", "gK", h // 2)], writes=[("KTn", s, r)])
            P.dma("sp", KTs[s][64:96, r * TOK:(r + 1) * TOK], d["gKr"][r * 32:(r + 1) * 32, :],
                  reads=[("dram", "gKr", None)], writes=[("KTr", s, r)])
            P.dma("sp", Vs[s][:, r, :, :].rearrange("p t c -> p (t c)"), d["gV"][h][r * 128:(r + 1) * 128, :],
                  reads=[("dram", "gV", h)], writes=[("V", s, r)])
        P.dma("sp", QTs[s][:, :], d["qT"][h, :, :], writes=[("QT", s)])

    wstg = Rot([P.sb([128, 2048], F32) for _ in range(2)], "wstg")
    wrow = Rot([P.sb([128, 6144], BF16) for _ in range(2)], "wrow")

    def precast(ex):
        row, rowk = wrow.next()
        for j, (nm, f) in enumerate((("w_gate", DE), ("w_up", DE), ("w_down", DM))):
            st_, stk = wstg.next()
            P.dma("sp", st_[:, :].rearrange("p (k f) -> p k f", f=f), d[nm][l, ex].rearrange("(k p) f -> p k f", p=128),
                  writes=[stk])
            cpy(P, "dve", row[:, j * 2048:(j + 1) * 2048], st_[:, :], [stk], [(rowk, j)])
        P.dma("sp", d["wbf"][ex * 128:(ex + 1) * 128, :], row[:, :], reads=[(rowk, j) for j in range(3)], semkey=("wrowst", rowk))

    load_head(0)
    nkb = SEQ // 128 // KB
    its = [(h, qt, kb) for h in range(NH) for qt in range(TOK // 512) for kb in range(nkb)]
    state = {}

    def emit_S(it):
        h, qt, kb = it
        s = h % 2
        if qt == 0 and kb == 0 and h + 1 < NH:
            load_head(h + 1)
        if kb == 8 and (h * 8 + qt) % 2 == 0:
            precast((h * 8 + qt) // 2)
        ps, psk = sps.next()
        for u in range(KB):
            kt = kb * KB + u
            r = kt // 32
            mm(P, ps[:, u, :], KTs[s][0:96, kt * 128:(kt + 1) * 128], QTs[s][0:96, qt * 512:(qt + 1) * 512], True, True,
               [("KTn", s, r), ("KTr", s, r), ("QT", s)], [psk])
        state[it] = (ps, psk)

    def emit_rest(it):
        h, qt, kb = it
        s = h % 2
        V = Vs[s]
        ps, psk = state.pop(it)
        if kb == 0:
            state["po"] = pos.next()
        po, pok = state["po"]
        pt, ptk = pts.next()
        act(P, pt[:, :, :], ps[:, :, :], AF.Exp, [psk], [ptk])
        for u in range(KB):
            kt = kb * KB + u
            r, t = kt // 32, kt % 32
            mm(P, po[0:65, :], V[:, r, t, :], pt[:, u, :], kt == 0, kt == SEQ // 128 - 1, [("V", s, r), ptk], [pok])
        if kb == nkb - 1:
            rs_, rsk = rsum.next()
            rr_, rrk = rrec.next()
            bc, bck = bcs.next()
            bs_, bsk = bcsb.next()
            oT, oTk = oTs.next()
            cpy(P, "dve", rs_[64:65, :], po[64:65, :], [pok], [rsk])
            P.add("dve", lambda e, o=rr_, i=rs_: e.reciprocal(out=o[64:65, :], in_=i[64:65, :]), [rsk], [rrk])
            mm(P, bc[0:64, :], onesf[64:65, 0:64], rr_[64:65, :], True, True, ["onesf", rrk], [bck])
            cpy(P, "dve", bs_[:, :], bc[:, :], [bck], [bsk])
            tt(P, "dve", oT[:, :], po[0:64, :], bs_[:, :], ALU.mult, [pok, bsk], [oTk])
            P.dma("sp", d["omixT"][h * 64:(h + 1) * 64, qt * 512:(qt + 1) * 512], oT[:, :], reads=[oTk])

    emit_S(its[0])
    emit_S(its[1])
    for i, it in enumerate(its):
        if i + 2 < len(its):
            emit_S(its[i + 2])
        emit_rest(it)
    P.emit()


def phase_C1(nc, l, d):
    P = Phase(nc, f"C{l}", d.get("pool"))
    wc = P.sb([128, 2, 3], F32)
    sel = P.sb([128, 2, 4], F32)
    hal = P.sb([128, 2, 4, 16], F32)
    tmp = P.sb([128, 2, 4], F32)
    P.dma("sp", wc[:], d["w_conv"][l, :, :, :], writes=["wc"])
    P.dma("sp", sel[:], d["halo_sel"][:, :, :], writes=["sel"])
    for r in range(4):
        P.dma("sp", hal[:, :, r, :], d["gH"][r * 256:(r + 1) * 256, :].rearrange("(c p) x -> p c x", p=128), writes=[("hal", r)])
    halk = [("hal", r) for r in range(4)]
    upx = [P.sb([128, TOK + 2], F32) for _ in range(2)]
    CB = 1024
    bgb = Rot([P.sb([128, CB], F32) for _ in range(2)], "bgb")
    acc = Rot([P.sb([128, CB], F32) for _ in range(2)], "acc")
    ob = Rot([P.sb([128, CB], BF16) for _ in range(2)], "ob")
    for c in range(2):
        u = upx[c]
        P.dma("sp", u[:, 1:TOK + 1], d["upT"][c * 128:(c + 1) * 128, :], writes=[("upx", c)])
        tt(P, "dve", tmp[:, 0, :], hal[:, c, :, 1], sel[:, 0, :], ALU.mult, halk + ["sel"], [("tmpL", c)])
        P.add("dve", lambda e, o=u, i=tmp: e.reduce_sum(out=o[:, 0:1], in_=i[:, 0, :], axis=AX.X), [("tmpL", c)], [("upxL", c)])
        tt(P, "dve", tmp[:, 1, :], hal[:, c, :, 0], sel[:, 1, :], ALU.mult, halk + ["sel"], [("tmpR", c)])
        P.add("dve", lambda e, o=u, i=tmp: e.reduce_sum(out=o[:, TOK + 1:TOK + 2], in_=i[:, 1, :], axis=AX.X), [("tmpR", c)],
              [("upxR", c)])
        ukeys = [("upx", c), ("upxL", c), ("upxR", c)]
        for blk in range(TOK // CB):
            c0 = blk * CB
            b_, bk = bgb.next()
            a_, ak = acc.next()
            o_, ok = ob.next()
            P.dma("sp", b_[:, :], d["bgT"][c * 128:(c + 1) * 128, c0:c0 + CB], writes=[bk])
            eng = "dve"
            ts(P, eng, a_[:, :], u[:, c0:c0 + CB], wc[:, c, 0:1], None, ALU.mult, None, ukeys + ["wc"], [ak])
            stt(P, eng, a_[:, :], u[:, c0 + 1:c0 + 1 + CB], wc[:, c, 1:2], a_[:, :], ALU.mult, ALU.add, ukeys + ["wc", ak], [ak])
            stt(P, eng, a_[:, :], u[:, c0 + 2:c0 + 2 + CB], wc[:, c, 2:3], a_[:, :], ALU.mult, ALU.add, ukeys + ["wc", ak], [ak])
            tt(P, eng, o_[:, :], a_[:, :], b_[:, :], ALU.mult, [ak, bk], [ok])
            P.dma("act", d["omixT"][512 + c * 128:512 + (c + 1) * 128, c0:c0 + CB], o_[:, :], reads=[ok])
    P.emit()


def phase_C2(nc, l, d):
    P = Phase(nc, f"F{l}", d.get("pool"))
    stg = P.sb([128, 128 * 96 // 4], F32)
    T1 = P.sb([128, 256], BF16)
    T2 = P.sb([128, 128, 96], BF16)
    BD = P.sb([128, 2, 128], BF16)
    P.dma("sp", stg[:, 0:256], d["dft1"][:, :], writes=["stg"])
    cpy(P, "dve", T1[:, :], stg[:, 0:256], ["stg"], ["T1"])
    P.dma("sp", stg[:, 0:256], d["dftc"][:, :], writes=["stg"])
    cpy(P, "dve", BD[:, :, :].rearrange("p a b -> p (a b)"), stg[:, 0:256], ["stg"], ["BD"])
    for qq in range(4):
        P.dma("sp", stg[:, :], d["dft2"][:, qq * 32:(qq + 1) * 32, :].rearrange("p a b -> p (a b)"), writes=["stg"])
        cpy(P, "dve" if qq % 2 else "pool", T2[:, qq * 32:(qq + 1) * 32, :].rearrange("p a b -> p (a b)"), stg[:, :], ["stg"],
            [("T2", qq)])
    t2keys = [("T2", qq) for qq in range(4)]
    Fq = P.sb([128, 128, 128], BF16)
    Y1 = P.sb([128, 128, 256], BF16)
    X = P.sb([128, 128, 64], BF16)
    oF = P.sb([128, TOK], BF16)
    ps1 = Rot([P.ps([128, 2, 256], F32) for _ in range(3)], "ps1")
    ps2 = Rot([P.ps([128, 8, 64], F32) for _ in range(3)], "ps2")
    ps3 = Rot([P.ps([128, 512], F32) for _ in range(2)], "ps3")
    for q in range(2):
        src = d["gF"][q].rearrange("(a c) b -> a c b", c=128)
        for part in range(4):
            P.dma("sp", Fq[:, part * 32:(part + 1) * 32, :], src[:, part * 32:(part + 1) * 32, :], reads=[("dram", "gF", q)],
                  writes=[("Fq", part)])
        fkeys = [("Fq", part) for part in range(4)]
        for cp in range(64):
            p, pk = ps1.next()
            for u in range(2):
                c = cp * 2 + u
                mm(P, p[:, u, :], Fq[:, c, :], T1[:, :], True, True, fkeys + ["T1"], [pk])
            cpy(P, "act" if cp % 2 else "dve", Y1[:, cp * 2:cp * 2 + 2, :], p[:, :, :], [pk], [("Y1", cp)])
        y1keys = [("Y1", i) for i in range(64)]
        if "dbgY1" in d and q == 0:
            P.dma("sp", d["dbgY1"][:, :], Y1[:, :, :].rearrange("p a b -> p (a b)"), reads=y1keys, semkey="dbgY1")
            P.dma("sp", d["dbgF"][:, :], Fq[:, :, :].rearrange("p a b -> p (a b)"), reads=fkeys, semkey="dbgF")
        for kg in range(16):
            p, pk = ps2.next()
            for u in range(8):
                k1 = kg * 8 + u
                mm(P, p[:, u, :], Y1[:, :, k1], T2[:, k1, 32:96], True, False, y1keys + t2keys, [pk])
                mm(P, p[:, u, :], Y1[:, :, 128 + k1], T2[:, k1, 0:64], False, True, y1keys + t2keys, [pk])
            cpy(P, "act" if kg % 2 else "dve", X[:, kg * 8:(kg + 1) * 8, :], p[:, :, :], [pk], [("X", kg)])
        if "dbgX" in d and q == 0:
            P.dma("sp", d["dbgX"][:, :], X[:, :, :].rearrange("p a b -> p (a b)"), reads=[("X", i) for i in range(16)], semkey="dbgX")
        for m in range(8):
            p, pk = ps3.next()
            mm(P, p[:, :].rearrange("p (a b) -> p a b", b=32), BD[:, 0, :], X[:, m * 16:(m + 1) * 16, 0:32], True, False,
               [("X", 2 * m), ("X", 2 * m + 1), "BD"], [pk])
            mm(P, p[:, :].rearrange("p (a b) -> p a b", b=32), BD[:, 1, :], X[:, m * 16:(m + 1) * 16, 32:64], False, True,
               [("X", 2 * m), ("X", 2 * m + 1), "BD"], [pk])
            cpy(P, "act" if m % 2 else "dve",
                oF[:, :].rearrange("p (k2 k1) -> p k1 k2", k1=128)[:, m * 16:(m + 1) * 16, :],
                p[:, :].rearrange("p (a b) -> p a b", b=32), [pk], [("oF", m)])
        P.dma("sp", d["omixT"][768 + q * 128:768 + (q + 1) * 128, :], oF[:, :], reads=[("oF", m) for m in range(8)])
    P.emit()


WPAD = 256
NSLOT_T = (2 * TOK + NE * (WPAD - 1)) // 128 + 1
NSLOT = NSLOT_T * 128
BLK = 1024


def zero_slots(P, d):
    z = P.sb([128, DM], BF16)
    mset(P, "pool", z[:, :], 0.0, ["z"])
    for i in range(NSLOT_T):
        P.dma("pool", d["xs"][i * 128:(i + 1) * 128, :], z[:, :], reads=["z"], semkey="zst")


def phase_D(nc, l, d, last):
    phase_Da(nc, l, d)
    phase_Db(nc, l, d, last)


def phase_Da(nc, l, d):
    P = Phase(nc, f"D{l}", d.get("pool"))
    NT = TOK // 128
    NTB = BLK // 128
    identf = P.sb([128, 128], F32)
    ident = P.sb([128, 128], BF16)
    epsl = P.sb([128, 1], F32)
    wout = P.sb([128, 8, DM], BF16)
    wrt = P.sb([128, 8, 36], BF16)
    brt = P.sb([128, 36], F32)
    lnp = P.sb([128, 2, DM], F32)
    sortc = P.sb([128, 128 + NSLOT_T + 1], F32)
    ltri = P.sb([128, 128], BF16)
    ones = P.sb([128, 128], BF16)
    stg = Rot([P.sb([128, 2048], F32) for _ in range(1)], "stg")
    mset(P, "pool", epsl[:], LN_EPS, ["eps"])
    mset(P, "pool", ones[:], 1.0, ["ones"])
    P.dma("sp", identf[:], d["ident"][:, :], writes=["identf"])
    cpy(P, "dve", ident[:], identf[:], ["identf"], ["ident"])
    P.dma("sp", sortc[:], d["sortc"][:, :], writes=["sortc"])
    cpy(P, "dve", ltri[:], sortc[:, 0:128], ["sortc"], ["ltri"])
    tstart = sortc[:, 128:128 + NSLOT_T]
    pidx = sortc[:, 128 + NSLOT_T:128 + NSLOT_T + 1]
    P.dma("sp", brt[:], d["b_rt"][l, :, :], writes=["brt"])
    for i in range(2):
        P.dma("sp", lnp[:, i, :], d["lnp"][2 + 4 * l + i, :, :], writes=[("lnp", i)])
    for kc in range(8):
        s, sk = stg.next()
        P.dma("sp", s[:, 0:1024], d["w_out"][l, kc * 128:(kc + 1) * 128, :], writes=[sk])
        P.dma("sp", s[:, 1024:1060], d["w_rt"][l, kc * 128:(kc + 1) * 128, :], writes=[sk])
        cpy(P, "dve" if kc % 2 else "pool", wout[:, kc, :], s[:, 0:1024], [sk], [("wout", kc)])
        cpy(P, "dve", wrt[:, kc, :], s[:, 1024:1060], [sk], [("wrt", kc)])

    om = P.sb([128, 8, BLK], BF16)
    hts = Rot([P.sb([128, DM], F32) for _ in range(4)], "ht")
    res = Rot([P.sb([128, DM], F32) for _ in range(4)], "res")
    h1s = Rot([P.sb([128, DM], F32) for _ in range(3)], "h1")
    h1b = Rot([P.sb([128, DM], BF16) for _ in range(6)], "h1b")
    h1Ts = Rot([P.sb([128, 8, 128], BF16) for _ in range(2)], "h1T")
    lts = [ln_tmp(P), ln_tmp(P)]
    pb = Rot([P.ps([128, 512], F32) for _ in range(5)], "pb")
    lgps = Rot([P.ps([128, 512], F32) for _ in range(1)], "lgp")
    trp = Rot([P.ps([128, 1024], BF16) for _ in range(2)], "trp")
    M1 = P.sb([128, NT, NE], F32)
    M2 = P.sb([128, NT, NE], F32)
    W1, W2, posi, idxw = d["sbW1"], d["sbW2"], d["sbposi"], d["sbidxw"]
    R = {k: P.sb(shape, F32) for k, shape in dict(
        lg=[128, NTB, 36], m4=[128, NTB], d4=[128, NTB, 4], e4=[128, NTB, 4], s4=[128, NTB], pg=[128, NTB],
        oh=[128, NTB, 4], t48=[128, NTB, 4, 8], el=[128, NTB, 8], m1=[128, NTB], k1=[128, NTB, 8], el2=[128, NTB, 8],
        m2=[128, NTB], k2=[128, NTB, 8], dd=[128, NTB], ee=[128, NTB], p1=[128, NTB], p2=[128, NTB]).items()}

    def rk(n):
        return ("R", n)

    g1, b1 = lnp[:, 0, :], lnp[:, 1, :]
    bc3 = lambda ap, n: ap.unsqueeze(2).broadcast_to([128, NTB, n])
    for nb in range(TOK // BLK):
        tb = nb * BLK
        ts_ = slice(nb * NTB, (nb + 1) * NTB)
        for mc in range(8):
            P.dma("sp", om[:, mc, :], d["omixT"][mc * 128:(mc + 1) * 128, tb:tb + BLK], writes=[("om", mc)])
        lgp, lgk = lgps.next()
        st1 = {}

        def d1_a(t):
            ht, hk = hts.next()
            P.dma("sp", ht[:, :], d["hbuf"][tb + t * 128:tb + (t + 1) * 128, :], writes=[hk])
            r_, rk_ = res.next()
            def mix_half(half):
                p, pk = pb.next()
                for mc in range(8):
                    mm(P, p[:, :], om[:, mc, t * 128:(t + 1) * 128], wout[:, mc, half * 512:(half + 1) * 512], mc == 0, mc == 7,
                       [("om", mc), ("wout", mc)], [pk])
                stt(P, "dve", r_[:, half * 512:(half + 1) * 512], ht[:, half * 512:(half + 1) * 512], ALPHA, p[:, :],
                    ALU.mult, ALU.add, [hk, pk], [rk_])

            P.interleave([lambda: mix_half(0), lambda: mix_half(1)])
            st1[t] = (r_, rk_)

        def d1_b(t):
            r_, rk_ = st1.pop(t)
            h1, h1k = h1s.next()
            layer_norm_tile(P, r_[:, :], rk_, h1[:, :], h1k, g1, b1, [("lnp", 0), ("lnp", 1)], lts[t % 2], epsl[:, 0:1], ("lnD", t % 2))
            P.dma("act", d["h1buf"][tb + t * 128:tb + (t + 1) * 128, :], h1[:, :], reads=[h1k], writes=[("dram", "h1")],
                  semkey="h1st")
            hb, hbk = h1b.next()
            cpy(P, "act", hb[:, :], h1[:, :], [h1k], [hbk])
            P.dma("act", d["h1b"][tb + t * 128:tb + (t + 1) * 128, :], hb[:, :], reads=[hbk], writes=[("dram", "h1b")],
                  semkey="h1bst")
            tp, tk = trp.next()
            for kc in range(8):
                trn(P, tp[:, kc * 128:(kc + 1) * 128], hb[:, kc * 128:(kc + 1) * 128], ident[:], [hbk, "ident"], [tk])
            hT, hTk = h1Ts.next()
            cpy(P, "dve", hT[:, :, :], tp[:, :].rearrange("p (k t) -> p k t", k=8), [tk], [hTk])
            st1[("hT", t)] = (hT, hTk)

        def d1_c(t):
            hT, hTk = st1.pop(("hT", t))
            for kc in range(8):
                mm(P, lgp[:, t * 36:(t + 1) * 36], hT[:, kc, :], wrt[:, kc, :], kc == 0, kc == 7, [hTk, ("wrt", kc)], [lgk])

        d1_a(0)
        d1_a(1)
        for t in range(0, NTB, 2):
            if t + 2 < NTB:
                d1_a(t + 2)
                d1_a(t + 3)
            P.interleave([lambda t=t: d1_b(t), lambda t=t: d1_b(t + 1)])
            d1_c(t)
            d1_c(t + 1)
        lg = R["lg"]
        tt(P, "dve", lg[:, :, :], lgp[:, 0:NTB * 36].rearrange("p (t c) -> p t c", c=36),
           brt[:, :].unsqueeze(1).broadcast_to([128, NTB, 36]), ALU.add, [lgk, "brt"], [rk("lg")])
        P.add("dve", lambda e: e.reduce_max(out=R["m4"][:, :], in_=lg[:, :, 0:4], axis=AX.X), [rk("lg")], [rk("m4")])
        tt(P, "dve", R["d4"][:, :, :], lg[:, :, 0:4], bc3(R["m4"][:, :], 4), ALU.subtract, [rk("lg"), rk("m4")], [rk("d4")])
        act(P, R["e4"][:, :, :], R["d4"][:, :, :], AF.Exp, [rk("d4")], [rk("e4")])
        P.add("dve", lambda e: e.reduce_sum(out=R["s4"][:, :], in_=R["e4"][:, :, :], axis=AX.X), [rk("e4")], [rk("s4")])
        P.add("dve", lambda e: e.reciprocal(out=R["pg"][:, :], in_=R["s4"][:, :]), [rk("s4")], [rk("pg")])
        ts(P, "dve", R["oh"][:, :, :], R["d4"][:, :, :], 0.0, None, ALU.is_equal, None, [rk("d4")], [rk("oh")])
        tt(P, "dve", R["t48"][:, :, :, :], lg[:, :, 4:36].rearrange("p t (g e) -> p t g e", e=8),
           R["oh"][:, :, :].unsqueeze(3).broadcast_to([128, NTB, 4, 8]), ALU.mult, [rk("lg"), rk("oh")], [rk("t48")])
        P.add("dve", lambda e: e.reduce_sum(out=R["el"][:, :, :], in_=R["t48"][:, :, :, :].rearrange("p t g e -> p t e g"),
                                            axis=AX.X), [rk("t48")], [rk("el")])
        P.add("dve", lambda e: e.reduce_max(out=R["m1"][:, :], in_=R["el"][:, :, :], axis=AX.X), [rk("el")], [rk("m1")])
        tt(P, "dve", R["k1"][:, :, :], R["el"][:, :, :], bc3(R["m1"][:, :], 8), ALU.is_equal, [rk("el"), rk("m1")], [rk("k1")])
        stt(P, "dve", R["el2"][:, :, :], R["k1"][:, :, :], -1.0e30, R["el"][:, :, :], ALU.mult, ALU.add, [rk("k1"), rk("el")],
            [rk("el2")])
        P.add("dve", lambda e: e.reduce_max(out=R["m2"][:, :], in_=R["el2"][:, :, :], axis=AX.X), [rk("el2")], [rk("m2")])
        tt(P, "dve", R["k2"][:, :, :], R["el2"][:, :, :], bc3(R["m2"][:, :], 8), ALU.is_equal, [rk("el2"), rk("m2")], [rk("k2")])
        tt(P, "dve", R["dd"][:, :], R["m2"][:, :], R["m1"][:, :], ALU.subtract, [rk("m1"), rk("m2")], [rk("dd")])
        act(P, R["ee"][:, :], R["dd"][:, :], AF.Exp, [rk("dd")], [rk("ee")])
        ts(P, "dve", R["p2"][:, :], R["ee"][:, :], 1.0, None, ALU.add, None, [rk("ee")], [rk("p2")])
        P.add("dve", lambda e: e.reciprocal(out=R["p1"][:, :], in_=R["p2"][:, :]), [rk("p2")], [rk("p1")])
        tt(P, "dve", W1[:, ts_], R["p1"][:, :], R["pg"][:, :], ALU.mult, [rk("p1"), rk("pg")], [("W1", nb)])
        tt(P, "dve", W2[:, ts_], W1[:, ts_], R["ee"][:, :], ALU.mult, [("W1", nb), rk("ee")], [("W2", nb)])
        ohb = R["oh"][:, :, :].unsqueeze(3).broadcast_to([128, NTB, 4, 8])
        tt(P, "dve", M1[:, ts_, :].rearrange("p t (g e) -> p t g e", e=8), ohb,
           R["k1"][:, :, :].unsqueeze(2).broadcast_to([128, NTB, 4, 8]), ALU.mult, [rk("oh"), rk("k1")], [("M1", nb)])
        tt(P, "dve", M2[:, ts_, :].rearrange("p t (g e) -> p t g e", e=8), ohb,
           R["k2"][:, :, :].unsqueeze(2).broadcast_to([128, NTB, 4, 8]), ALU.mult, [rk("oh"), rk("k2")], [("M2", nb)])
    NBK = TOK // BLK
    mkeys = [("M1", nb) for nb in range(NBK)] + [("M2", nb) for nb in range(NBK)]
    wkeys = [("W1", nb) for nb in range(NBK)] + [("W2", nb) for nb in range(NBK)]

    M12 = P.sb([128, NT * NE], BF16)
    Wn = P.sb([128, NT, NE], F32)
    TA = P.sb([128, NT, NE], F32)
    TB = P.sb([128, NT, NE], F32)
    T0 = P.sb([128, NT, NE], F32)
    SL = P.sb([128, NT, NE], F32)
    ea = P.sb([128, NE], F32)
    eb = P.sb([128, NE], F32)
    pcf = P.sb([128, NE], F32)
    pci = P.sb([128, NE], I32)
    bexc = P.sb([128, NE], F32)
    posf = P.sb([128, 2, NT], F32)
    cmp_ = P.sb([128, NSLOT_T, NE], F32)
    tef = P.sb([128, NSLOT_T], F32)
    PR1 = P.sb([128, NT, NE], F32)
    PR2 = P.sb([128, NT, NE], F32)
    tt(P, "dve", M12[:, :].rearrange("p (t e) -> p t e", e=NE), M1[:, :, :], M2[:, :, :], ALU.add, mkeys, ["M12"])
    for half in range(2):
        p, pk = pb.next()
        mm(P, p[:, :], ltri[:, :], M12[:, half * 512:(half + 1) * 512], True, True, ["ltri", "M12"], [pk])
        cpy(P, "act", Wn[:, half * 16:(half + 1) * 16, :].rearrange("p t e -> p (t e)"), p[:, :], [pk], [("Wn", half)])
        p, pk = pb.next()
        mm(P, p[:, :], ones[:, :], M12[:, half * 512:(half + 1) * 512], True, True, ["ones", "M12"], [pk])
        cpy(P, "dve", T0[:, half * 16:(half + 1) * 16, :].rearrange("p t e -> p (t e)"), p[:, :], [pk], [("T0", half)])
    P.add("dve", lambda e: e.tensor_copy(out=TA[:, :, :], in_=T0[:, :, :]), [("T0", 0), ("T0", 1)], ["TA"])
    cur, curk, oth, othk = TA, "TA", TB, "TB"
    for sft in (1, 2, 4, 8, 16):
        cpy(P, "dve", oth[:, 0:sft, :], cur[:, 0:sft, :], [curk], [othk])
        tt(P, "dve", oth[:, sft:NT, :], cur[:, sft:NT, :], cur[:, 0:NT - sft, :], ALU.add, [curk, othk], [othk])
        cur, curk, oth, othk = oth, othk, cur, curk
    incl, inclk = cur, curk
    cpy(P, "dve", pci[:, :], incl[:, NT - 1, :], [inclk], ["pci"])
    P.add("dve", lambda e: e.tensor_single_scalar(out=pci[:, :], in_=pci[:, :], scalar=WPAD - 1, op=ALU.add), ["pci"], ["pci"])
    P.add("dve", lambda e: e.tensor_single_scalar(out=pci[:, :], in_=pci[:, :], scalar=8, op=ALU.arith_shift_right), ["pci"], ["pci"])
    P.add("dve", lambda e: e.tensor_single_scalar(out=pci[:, :], in_=pci[:, :], scalar=8, op=ALU.logical_shift_left), ["pci"], ["pci"])
    cpy(P, "dve", pcf[:, :], pci[:, :], ["pci"], ["pcf"])
    cpy(P, "dve", ea[:, :], pcf[:, :], ["pcf"], ["ea"])
    c2, c2k, o2, o2k = ea, "ea", eb, "eb"
    for sft in (1, 2, 4, 8, 16):
        cpy(P, "dve", o2[:, 0:sft], c2[:, 0:sft], [c2k], [o2k])
        tt(P, "dve", o2[:, sft:NE], c2[:, sft:NE], c2[:, 0:NE - sft], ALU.add, [c2k, o2k], [o2k])
        c2, c2k, o2, o2k = o2, o2k, c2, c2k
    pend, pendk = c2, c2k
    tt(P, "dve", bexc[:, :], pend[:, :], pcf[:, :], ALU.subtract, [pendk, "pcf"], ["bexc"])
    tt(P, "dve", SL[:, :, :], incl[:, :, :], T0[:, :, :], ALU.subtract, [inclk, ("T0", 0), ("T0", 1)], ["SL"])
    tt(P, "dve", SL[:, :, :], SL[:, :, :], Wn[:, :, :], ALU.add, ["SL", ("Wn", 0), ("Wn", 1)], ["SL"])
    tt(P, "dve", SL[:, :, :], SL[:, :, :], bexc[:, :].unsqueeze(1).broadcast_to([128, NT, NE]), ALU.add, ["SL", "bexc"], ["SL"])
    tt(P, "dve", PR1[:, :, :], SL[:, :, :], M1[:, :, :], ALU.mult, ["SL"] + mkeys, ["PR1"])
    P.add("dve", lambda e: e.reduce_sum(out=posf[:, 0, :], in_=PR1[:, :, :], axis=AX.X), ["PR1"], [("posf", 0)])
    tt(P, "dve", PR2[:, :, :], SL[:, :, :], M2[:, :, :], ALU.mult, ["SL"] + mkeys, ["PR2"])
    P.add("dve", lambda e: e.reduce_sum(out=posf[:, 1, :], in_=PR2[:, :, :], axis=AX.X), ["PR2"], [("posf", 1)])
    cpy(P, "dve", posi[:, :, :], posf[:, :, :], [("posf", 0), ("posf", 1)], ["posi"])
    tt(P, "dve", cmp_[:, :, :], pend[:, :].unsqueeze(1).broadcast_to([128, NSLOT_T, NE]),
       tstart.unsqueeze(2).broadcast_to([128, NSLOT_T, NE]), ALU.is_le, [pendk, "sortc"], ["cmp"])
    P.add("dve", lambda e: e.reduce_sum(out=tef[:, :], in_=cmp_[:, :, :], axis=AX.X), ["cmp"], ["tef"])
    ts(P, "dve", tef[:, :], tef[:, :], float(NE - 1), 128.0, ALU.min, ALU.mult, ["tef"], ["tef"])
    ts(P, "dve", tef[:, :], tef[:, :], pidx, None, ALU.add, None, ["tef", "sortc"], ["tef"])
    cpy(P, "dve", idxw[:, :], tef[:, :], ["tef"], ["idxw"])

    for t in range(0 if "no_scatter" not in d else NT, NT):
        hb, hbk = h1b.next()
        P.dma("sp", hb[:, :], d["h1b"][t * 128:(t + 1) * 128, :], reads=[("dram", "h1b")], writes=[hbk])
        for k in range(2):
            P.add("pool", lambda e, hb=hb, k=k, t=t: e.indirect_dma_start(
                out=d["xs"][:, :], out_offset=bass.IndirectOffsetOnAxis(ap=posi[:, k, t:t + 1], axis=0), in_=hb[:, :], in_offset=None),
                [hbk, "posi"], [("dram", "xs")], dma=True, semkey="xs_sc")
    if "dbg_posi" in d:
        P.dma("sp", d["dbg_posi"][:, :], posi[:, :, :].rearrange("p a b -> p (a b)"), reads=["posi"], semkey="dbg1")
        P.dma("sp", d["dbg_idxw"][:, :], idxw[:, :], reads=["idxw"], semkey="dbg2")
        P.dma("sp", d["dbg_W"][:, 0:NT], W1[:, :], reads=wkeys, semkey="dbg3")
        P.dma("sp", d["dbg_W"][:, NT:2 * NT], W2[:, :], reads=wkeys, semkey="dbg3")
        P.dma("sp", d["dbg_M1"][:, :], M1[:, :, :].rearrange("p a b -> p (a b)"), reads=mkeys, semkey="dbg4")
        P.dma("sp", d["dbg_M2"][:, :], M2[:, :, :].rearrange("p a b -> p (a b)"), reads=mkeys, semkey="dbg4")
    P.emit()


def phase_Db(nc, l, d, last):
    P = Phase(nc, f"E{l}", d.get("pool"))
    NT = TOK // 128
    identf = P.sb([128, 128], F32)
    ident = P.sb([128, 128], BF16)
    epsl = P.sb([128, 1], F32)
    lnp = P.sb([128, 2, DM], F32)
    mset(P, "pool", epsl[:], LN_EPS, ["eps"])
    P.dma("sp", identf[:], d["ident"][:, :], writes=["identf"])
    cpy(P, "dve", ident[:], identf[:], ["identf"], ["ident"])
    for i in range(2):
        P.dma("sp", lnp[:, i, :], d["lnp"][2 + 4 * l + 2 + i, :, :], writes=[("lnp", 2 + i)])
    g2, b2 = lnp[:, 0, :], lnp[:, 1, :]
    W1, W2, posi, idxw = d["sbW1"], d["sbW2"], d["sbposi"], d["sbidxw"]
    wkeys = []
    hts = Rot([P.sb([128, DM], F32) for _ in range(3)], "ht")
    res = Rot([P.sb([128, DM], F32) for _ in range(4)], "res")
    h1s = Rot([P.sb([128, DM], F32) for _ in range(4)], "h1")
    lts = [ln_tmp(P), ln_tmp(P)]
    pb = Rot([P.ps([128, 512], F32) for _ in range(6)], "pb")
    trp = Rot([P.ps([128, 1024], BF16) for _ in range(1)], "trp")
    tp2 = Rot([P.ps([128, 256], BF16) for _ in range(1)], "tp2")

    wsb = Rot([P.sb([128, 6144], BF16) for _ in range(3)], "wsb")
    xts = Rot([P.sb([128, DM], BF16) for _ in range(4)], "xst")
    xTs = Rot([P.sb([128, 8, 128], BF16) for _ in range(3)], "xT")
    sgs = Rot([P.sb([128, 256], F32) for _ in range(3)], "sg")
    hids = Rot([P.sb([128, 256], BF16) for _ in range(3)], "hid")
    hTs = Rot([P.sb([128, 2, 128], BF16) for _ in range(2)], "hidT")
    yos = Rot([P.sb([128, DM], BF16) for _ in range(2)], "yo")
    wcur = {}
    stX = {}

    def stage_x(i):
        if i % (WPAD // 128) == 0:
            w_, wk = wsb.next()
            P.add("pool", lambda e, w_=w_, i=i: e.indirect_dma_start(
                out=w_[:, :], out_offset=None, in_=d["wbf"][:, :], in_offset=bass.IndirectOffsetOnAxis(ap=idxw[:, i:i + 1], axis=0)),
                [], [wk], dma=True)
            wcur["w"] = (w_, wk)
        w_, wk = wcur["w"]
        x_, xk = xts.next()
        P.dma("sp", x_[:, :], d["xs"][i * 128:(i + 1) * 128, :], reads=[("dram", "xs")], writes=[xk])
        tp, tk = trp.next()
        for kc in range(8):
            trn(P, tp[:, kc * 128:(kc + 1) * 128], x_[:, kc * 128:(kc + 1) * 128], ident[:], [xk, "ident"], [tk])
        xT, xTk = xTs.next()
        cpy(P, "dve" if i % 2 else "act", xT[:, :, :], tp[:, :].rearrange("p (k t) -> p k t", k=8), [tk], [xTk])
        pg, pgk = pb.next()
        wgu = w_[:, 0:4096].rearrange("p (g k f) -> p k g f", g=2, k=8)
        for kc in range(8):
            mm(P, pg[:, :].rearrange("p (g f) -> p g f", g=2), xT[:, kc, :], wgu[:, kc, :, :], kc == 0, kc == 7, [xTk, wk], [pgk])
        sg, sgk = sgs.next()
        act(P, sg[:, :], pg[:, 0:256], AF.Silu, [pgk], [sgk])
        hid, hidk = hids.next()
        tt(P, "dve", hid[:, :], sg[:, :], pg[:, 256:512], ALU.mult, [sgk, pgk], [hidk])
        stX[i] = (w_, wk, hid, hidk)

    def stage_y(i):
        w_, wk, hid, hidk = stX.pop(i)
        t2, t2k = tp2.next()
        for fc in range(2):
            trn(P, t2[:, fc * 128:(fc + 1) * 128], hid[:, fc * 128:(fc + 1) * 128], ident[:], [hidk, "ident"], [t2k])
        hT_, hTk_ = hTs.next()
        cpy(P, "act", hT_[:, :, :], t2[:, :].rearrange("p (k t) -> p k t", k=2), [t2k], [hTk_])
        yo, yok = yos.next()
        for half in range(2):
            pd, pdk = pb.next()
            for fc in range(2):
                mm(P, pd[:, :], hT_[:, fc, :], w_[:, 4096 + fc * 1024 + half * 512:4096 + fc * 1024 + (half + 1) * 512], fc == 0, fc == 1,
                   [hTk_, wk], [pdk])
            cpy(P, "dve" if half else "act", yo[:, half * 512:(half + 1) * 512], pd[:, :], [pdk], [(yok, half)])
        P.dma("act", d["ys"][i * 128:(i + 1) * 128, :], yo[:, :], reads=[(yok, 0), (yok, 1)], writes=[("dram", "ys")], semkey="ys_st")

    stage_x(0)
    for i in range(NSLOT_T):
        if i + 1 < NSLOT_T:
            stage_x(i + 1)
        stage_y(i)

    y1s = Rot([P.sb([128, DM], BF16) for _ in range(3)], "y1")
    y2s = Rot([P.sb([128, DM], BF16) for _ in range(3)], "y2")
    fs_ = Rot([P.sb([128, DM], F32) for _ in range(2)], "ff")
    st3 = {}

    def d3_a(t):
        ys_ = []
        for k, rot in ((0, y1s), (1, y2s)):
            y_, yk = rot.next()
            P.add("pool", lambda e, y_=y_, k=k, t=t: e.indirect_dma_start(
                out=y_[:, :], out_offset=None, in_=d["ys"][:, :], in_offset=bass.IndirectOffsetOnAxis(ap=posi[:, k, t:t + 1], axis=0)),
                [("dram", "ys")], [yk], dma=True)
            ys_.append((y_, yk))
        h1, h1k = h1s.next()
        P.dma("sp", h1[:, :], d["h1buf"][t * 128:(t + 1) * 128, :], reads=[("dram", "h1")], writes=[h1k])
        f_, fk = fs_.next()
        ts(P, "dve", f_[:, :], ys_[0][0][:, :], W1[:, t:t + 1], None, ALU.mult, None, [ys_[0][1]] + wkeys, [fk])
        stt(P, "dve", f_[:, :], ys_[1][0][:, :], W2[:, t:t + 1], f_[:, :], ALU.mult, ALU.add, [ys_[1][1], fk] + wkeys, [fk])
        r_, rk_ = res.next()
        stt(P, "dve", r_[:, :], h1[:, :], ALPHA, f_[:, :], ALU.mult, ALU.add, [h1k, fk], [rk_])
        st3[t] = (r_, rk_)

    def d3_b(t):
        r_, rk_ = st3.pop(t)
        ht, hk = hts.next()
        layer_norm_tile(P, r_[:, :], rk_, ht[:, :], hk, g2, b2, [("lnp", 2), ("lnp", 3)], lts[t % 2], epsl[:, 0:1], ("lnD", t % 2))
        dst = d["y"] if last else d["hbuf"]
        P.dma("act", dst[t * 128:(t + 1) * 128, :], ht[:, :], reads=[hk])

    d3_a(0)
    d3_a(1)
    for t in range(0, NT, 2):
        if t + 2 < NT:
            d3_a(t + 2)
            d3_a(t + 3)
        P.interleave([lambda t=t: d3_b(t), lambda t=t: d3_b(t + 1)])
    P.emit()


def _rope_tables():
    inv = (1.0 / (10000.0 ** (np.arange(0, DR, 2, dtype=np.float32) / DR))).astype(np.float32)
    ang = np.arange(SEQ, dtype=np.float32)[:, None] * inv[None, :]
    cos = np.cos(ang).astype(np.float32).T
    sin = np.sin(ang).astype(np.float32).T
    c2 = np.concatenate([cos, cos], 0)
    s2 = np.concatenate([-sin, sin], 0)
    out = np.zeros((128, 2, SEQ), np.float32)
    out[0:32, 0], out[0:32, 1] = c2, s2
    out[64:96, 0], out[64:96, 1] = c2 * np.float32(QSCALE), s2 * np.float32(QSCALE)
    return out


def _dft_tables():
    n = np.arange(128, dtype=np.float64)
    ang1 = 2 * np.pi * np.outer(n, n) / 128.0
    dft1 = np.concatenate([np.cos(ang1), -np.sin(ang1)], 1).astype(np.float32)
    c = np.arange(64, dtype=np.float64)
    angc = 2 * np.pi * np.outer(c, c) / 64.0
    sc = 1.0 / np.sqrt(SEQ * 64.0)
    bdc = np.kron(np.eye(2), np.cos(angc)) * sc
    bds = np.kron(np.eye(2), np.sin(angc)) * sc
    dftc = np.concatenate([bdc, bds], 1).astype(np.float32)
    per_core = []
    b = np.arange(128, dtype=np.float64)[:, None, None]
    k1 = np.arange(128, dtype=np.float64)[None, :, None]
    for j in range(4):
        k2 = (32 * j + np.arange(32, dtype=np.float64))[None, None, :]
        ang = 2 * np.pi * ((b * (k1 + 128.0 * k2)) % SEQ) / SEQ
        tr, ti = np.cos(ang), -np.sin(ang)
        per_core.append(np.ascontiguousarray(np.concatenate([-ti, tr, ti], 2).astype(np.float32)))
    return dft1, dftc, per_core


def _prep(inp):
    f = np.float32
    L = DEPTH
    w_in = np.asarray(inp["w_in"], f)
    sp = np.cumsum([0, 384, 256, 32, 256, 256, 256, 256])
    cq, ckv, kpe, bg, cg, hc, ff = [w_in[:, :, sp[i]:sp[i + 1]] for i in range(7)]
    kps = np.concatenate([kpe[:, :, 16:32], kpe[:, :, 0:16]], -1)
    w_in2 = np.ascontiguousarray(np.concatenate([cq, ckv, kpe, kps, bg, cg, hc, ff], -1))
    w_uq = np.asarray(inp["w_uq"], f)
    w_uq_sw = np.concatenate([w_uq[..., 0:64], w_uq[..., 80:96], w_uq[..., 64:80]], -1)
    w_uq2 = np.ascontiguousarray(np.stack([w_uq, w_uq_sw], 2).reshape(L, QL, 2 * NH * 96))
    w_ukv = np.asarray(inp["w_ukv"], f)
    w_k = np.ascontiguousarray(w_ukv[..., 0:64].reshape(L, KVL, NH * 64))
    w_v = np.ascontiguousarray(w_ukv[..., 64:128].reshape(L, KVL, NH * 64))
    g_q = np.ascontiguousarray(np.asarray(inp["g_q"], f).reshape(L, 3, 128).transpose(0, 2, 1))
    g_kv = np.ascontiguousarray(np.asarray(inp["g_kv"], f).reshape(L, 2, 128).transpose(0, 2, 1))
    lnp = np.stack([inp["ln_in_g"], inp["ln_in_b"]] + [inp[k][l] for l in range(L) for k in ("ln1_g", "ln1_b", "ln2_g", "ln2_b")])
    lnp = np.ascontiguousarray(np.broadcast_to(np.asarray(lnp, f)[:, None, :], (2 + 4 * L, 128, DM)))
    rope = _rope_tables()
    dft1, dftc, dft2 = _dft_tables()
    w_conv = np.ascontiguousarray(np.asarray(inp["w_conv"], f).reshape(L, 3, 2, 128).transpose(0, 3, 2, 1))
    w_rt = np.ascontiguousarray(np.concatenate([np.asarray(inp["w_group"], f), np.asarray(inp["w_router"], f).reshape(L, DM, NE)], -1))
    b_rt = np.concatenate([np.asarray(inp["b_group"], f), np.asarray(inp["b_router"], f).reshape(L, NE)], -1)
    b_rt = np.ascontiguousarray(np.broadcast_to(b_rt[:, None, :], (L, 128, 36)))
    sortc = np.zeros((128, 128 + NSLOT_T + 1), f)
    sortc[:, 0:128] = np.triu(np.ones((128, 128), f), 1)
    sortc[:, 128:128 + NSLOT_T] = 128.0 * np.arange(NSLOT_T, dtype=f)[None, :]
    sortc[:, 128 + NSLOT_T] = np.arange(128, dtype=f)
    shared = dict(sortc=sortc, w_out=np.asarray(inp["w_out"], f), w_rt=w_rt, b_rt=b_rt,
                  w_gate=np.asarray(inp["w_gate"], f).reshape(L, NE, DM, DE), w_up=np.asarray(inp["w_up"], f).reshape(L, NE, DM, DE),
                  w_down=np.asarray(inp["w_down"], f).reshape(L, NE, DE, DM),
                  w_in=w_in2, w_uq=w_uq2, w_ukv_k=w_k, w_ukv_v=w_v, g_q=g_q, g_kv=g_kv, lnp=lnp,
                  ident=np.eye(128, dtype=f), dft1=dft1, dftc=dftc, w_conv=w_conv)
    x = np.asarray(inp["x"], f)
    maps = []
    for c in range(NCORES):
        b, j = c // 4, c % 4
        m = dict(shared)
        m["x"] = np.ascontiguousarray(x[b, j * TOK:(j + 1) * TOK])
        m["rope"] = np.ascontiguousarray(rope[:, :, j * TOK:(j + 1) * TOK])
        m["dft2"] = dft2[j]
        hs = np.zeros((128, 2, 4), f)
        if j > 0:
            hs[:, 0, j - 1] = 1.0
        if j < 3:
            hs[:, 1, j + 1] = 1.0
        m["halo_sel"] = hs
        maps.append(m)
    return maps


def build(debug=None, stop_after=None, stop_layers=DEPTH):
    nc = bass.Bass("TRN2", target_bir_lowering=False)
    L = DEPTH
    d = {"pool": SemPool(nc)}

    def inp(name, shape, dt=F32):
        d[name] = nc.dram_tensor(name, list(shape), dt, kind="ExternalInput").ap()

    def scr(name, shape, dt):
        kind = "ExternalOutput" if (debug and name in debug) else None
        t = nc.dram_tensor(name, list(shape), dt, kind=kind) if kind else nc.dram_tensor(name, list(shape), dt)
        d[name + "_t"] = t
        d[name] = t.ap()

    inp("x", [TOK, DM]); inp("rope", [128, 2, TOK]); inp("w_in", [L, DM, WIN_COLS]); inp("w_uq", [L, QL, 2 * NH * 96])
    inp("w_ukv_k", [L, KVL, 512]); inp("w_ukv_v", [L, KVL, 512]); inp("g_q", [L, 128, 3]); inp("g_kv", [L, 128, 2])
    inp("lnp", [2 + 4 * L, 128, DM]); inp("ident", [128, 128])
    inp("dft1", [128, 256]); inp("dftc", [128, 256]); inp("dft2", [128, 128, 96]); inp("w_conv", [L, 128, 2, 3])
    inp("halo_sel", [128, 2, 4]); inp("sortc", [128, 128 + NSLOT_T + 1])
    inp("w_out", [L, DM, DM]); inp("w_rt", [L, DM, 36]); inp("b_rt", [L, 128, 36])
    inp("w_gate", [L, NE, DM, DE]); inp("w_up", [L, NE, DM, DE]); inp("w_down", [L, NE, DE, DM])
    scr("hbuf", [TOK, DM], F32)
    scr("h1buf", [TOK, DM], F32)
    scr("wbf", [NE * 128, 6144], BF16)
    scr("h1b", [TOK, DM], BF16)
    scr("xs", [NSLOT, DM], BF16)
    scr("ys", [NSLOT, DM], BF16)
    scr("qT", [NH, 96, TOK], BF16)
    def scrl(name, n, shape, dt):
        ts_ = [nc.dram_tensor(f"{name}{i}", list(shape), dt) for i in range(n)]
        d[name + "_t"] = ts_
        d[name] = [t.ap() for t in ts_]

    scrl("payK", 4, [128, TOK], BF16); scrl("gK", 4, [4 * 128, TOK], BF16)
    scr("payKr", [32, TOK], BF16); scr("gKr", [4 * 32, TOK], BF16)
    scrl("payV", NH, [128, 32 * 65], BF16); scrl("gV", NH, [4 * 128, 32 * 65], BF16)
    scrl("payF", 2, [TOK, 128], BF16); scrl("gF", 2, [4 * TOK, 128], BF16)
    scr("payH", [256, 16], F32); scr("gH", [4 * 256, 16], F32)
    scr("upT", [256, TOK], F32)
    scr("bgT", [256, TOK], F32)
    scr("omixT", [DM, TOK], BF16)
    d["y"] = nc.dram_tensor("y", [TOK, DM], F32, kind="ExternalOutput").ap()
    pst = contextlib.ExitStack()
    d["_pst"] = pst
    d["sbW1"] = pst.enter_context(nc.sbuf_tensor("p_W1", [128, TOK // 128], F32))
    d["sbW2"] = pst.enter_context(nc.sbuf_tensor("p_W2", [128, TOK // 128], F32))
    d["sbposi"] = pst.enter_context(nc.sbuf_tensor("p_posi", [128, 2, TOK // 128], I32))
    d["sbidxw"] = pst.enter_context(nc.sbuf_tensor("p_idxw", [128, NSLOT_T], I32))
    if debug and "dbgF" in debug:
        d["dbgY1"] = nc.dram_tensor("dbgY1", [128, 256 * 128], BF16, kind="ExternalOutput").ap()
        d["dbgF"] = nc.dram_tensor("dbgF", [128, 128 * 128], BF16, kind="ExternalOutput").ap()
        d["dbgX"] = nc.dram_tensor("dbgX", [128, 128 * 64], BF16, kind="ExternalOutput").ap()
    for l in range(stop_layers):
        phase_A(nc, l, d)
        if stop_after == "A":
            break
        phase_B(nc, l, d)
        if stop_after == "B":
            break
        phase_C1(nc, l, d)
        phase_C2(nc, l, d)
        if stop_after == "C":
            break
        phase_D(nc, l, d, last=(l == DEPTH - 1))
    return nc


def kernel(**inputs):
    maps = _prep(inputs)
    nc = build()
    res = run_bass_kernel_spmd(nc, maps, core_ids=list(range(NCORES)))
    out = np.empty((BATCH, SEQ, DM), np.float32)
    for c in range(NCORES):
        out[c // 4, (c % 4) * TOK:(c % 4 + 1) * TOK] = res.results[c]["y"]
    return out
```
